# Optimizing a Trainium2 kernel written in Bass

```python
import math
import jax, jax.numpy as jnp
from jax import lax
import numpy as np

D_MODEL = 2048
BATCH = 2
SEQ = 4096
DEPTH = 1

CHUNK = 64
Q_BLOCK = 128
ROPE_THETA = 10000.0
NORM_EPS = 1e-6
NEG_INF = -1e30
D_MIX = D_MODEL
DIFF_WIDTH = D_MIX // 2
DIFF_HEADS = 8
DIFF_VDIM = DIFF_WIDTH // DIFF_HEADS
DIFF_QKDIM = DIFF_VDIM // 2
GLA_WIDTH = D_MIX - DIFF_WIDTH
GLA_HEADS = 4
GLA_VDIM = GLA_WIDTH // GLA_HEADS
GLA_KDIM = GLA_VDIM // 2
GLA_GATE_RANK = 16
GLA_TAU = 16.0
N_MEM = 256
CROSS_HEADS = 4
CROSS_HDIM = D_MODEL // CROSS_HEADS
N_GROUPS = 4
EXPERTS_PER_GROUP = 8
N_EXPERTS = N_GROUPS * EXPERTS_PER_GROUP
TOP_K = 2
D_EXPERT = D_MODEL // 4
EXPERT_BLOCK = 128
IN_SIZES = (DIFF_HEADS * 2 * DIFF_QKDIM, DIFF_HEADS * 2 * DIFF_QKDIM, DIFF_HEADS * DIFF_VDIM,
            GLA_HEADS * GLA_KDIM, GLA_HEADS * GLA_KDIM, GLA_HEADS * GLA_VDIM,
            GLA_GATE_RANK, GLA_WIDTH)
IN_COLS = sum(IN_SIZES)

kernel_name = 'hybrid_diffattn_gla_hmoe_block'


def _split_points():
    pts, acc = [], 0
    for s in IN_SIZES[:-1]:
        acc += s
        pts.append(acc)
    return pts


def rmsnorm(x, g):
    xf = x.astype(jnp.float32)
    y = xf * lax.rsqrt(jnp.mean(xf * xf, axis=-1, keepdims=True) + NORM_EPS)
    return (y * g.astype(jnp.float32)).astype(x.dtype)


def rope(t, positions):
    half = t.shape[-1] // 2
    freq = ROPE_THETA ** (-jnp.arange(half, dtype=jnp.float32) / half)
    ang = positions.astype(jnp.float32)[..., None] * freq
    cos = jnp.cos(ang)[:, :, None, None, :]
    sin = jnp.sin(ang)[:, :, None, None, :]
    tf = t.astype(jnp.float32)
    t1, t2 = tf[..., :half], tf[..., half:]
    return jnp.concatenate([t1 * cos - t2 * sin, t2 * cos + t1 * sin], axis=-1).astype(t.dtype)


def diff_attention(q, k, v, lam):
    B, S, H, _, Dh = q.shape
    Dv = v.shape[-1]
    nqb = S // Q_BLOCK
    qb = q.reshape(B, nqb, Q_BLOCK, H, 2, Dh).transpose(1, 0, 2, 3, 4, 5)
    key_chunk = jnp.arange(S) // CHUNK
    scale = Dh ** -0.5

    def block(args):
        qblk, b = args
        s = jnp.einsum('bqhcd,bkhcd->bhcqk', qblk, k).astype(jnp.float32) * scale
        q_chunk = (b * Q_BLOCK + jnp.arange(Q_BLOCK)) // CHUNK
        allowed = key_chunk[None, :] <= q_chunk[:, None]
        p = jax.nn.softmax(jnp.where(allowed, s, NEG_INF), axis=-1)
        a = p[:, :, 0] - lam * p[:, :, 1]
        return jnp.einsum('bhqk,bkhd->bqhd', a.astype(v.dtype), v)

    out = lax.map(block, (qb, jnp.arange(nqb)))
    return out.transpose(1, 0, 2, 3, 4).reshape(B, S, H, Dv)


def gla(q, k, v, log_a):
    B, S, H, Dk = q.shape
    Dv = v.shape[-1]
    NC = S // CHUNK
    qc = q.reshape(B, NC, CHUNK, H, Dk).astype(jnp.float32)
    kc = k.reshape(B, NC, CHUNK, H, Dk).astype(jnp.float32)
    vc = v.reshape(B, NC, CHUNK, H, Dv).astype(jnp.float32)
    cum = jnp.cumsum(log_a.reshape(B, NC, CHUNK, H, Dk).astype(jnp.float32), axis=2)
    cum_end = cum[:, :, -1:]
    k_dec = kc * jnp.exp(cum_end - cum)
    d_state = jnp.einsum('bnchk,bnchv->nbhkv', k_dec, vc)
    decay = jnp.exp(cum_end[:, :, 0]).transpose(1, 0, 2, 3)

    def step(s_prev, inp):
        dcy, ds = inp
        s_new = dcy[..., None] * s_prev + ds
        return s_new, s_new

    _, states = lax.scan(step, jnp.zeros((B, H, Dk, Dv), jnp.float32), (decay, d_state))
    o = jnp.einsum('bnchk,nbhkv->bnchv', qc * (Dk ** -0.5), states)
    return o.reshape(B, S, H, Dv).astype(q.dtype)


def cross_attention(xn, memn, w_cq, w_ckv, qg, kg, w_co):
    B, S, D = xn.shape
    M = memn.shape[1]
    q = rmsnorm((xn @ w_cq).reshape(B, S, CROSS_HEADS, CROSS_HDIM), qg)
    k, v = jnp.split(memn @ w_ckv, 2, axis=-1)
    k = rmsnorm(k.reshape(B, M, CROSS_HEADS, CROSS_HDIM), kg)
    v = v.reshape(B, M, CROSS_HEADS, CROSS_HDIM)
    s = jnp.einsum('bshd,bmhd->bhsm', q, k).astype(jnp.float32) * (CROSS_HDIM ** -0.5)
    p = jax.nn.softmax(s, axis=-1)
    o = jnp.einsum('bhsm,bmhd->bshd', p.astype(v.dtype), v).reshape(B, S, D)
    return o @ w_co


def hmoe(xn, w_rg, b_rg, w_re, b_re, w_gate, w_up, w_down):
    B, S, D = xn.shape
    T = B * S
    xt = xn.reshape(T, D)
    tok = jnp.arange(T)
    grp_logits = (xt @ w_rg).astype(jnp.float32) + b_rg.astype(jnp.float32)
    grp_prob = jax.nn.softmax(grp_logits, axis=-1)
    grp = jnp.argmax(grp_logits, axis=-1)
    grp_w = jnp.max(grp_prob, axis=-1, keepdims=True)
    exp_logits = ((xt @ w_re).astype(jnp.float32) + b_re.astype(jnp.float32)).reshape(T, N_GROUPS, EXPERTS_PER_GROUP)
    in_prob = jax.nn.softmax(exp_logits[tok, grp], axis=-1)
    top_p, top_i = lax.top_k(in_prob, TOP_K)
    gate = grp_w * top_p / jnp.sum(top_p, axis=-1, keepdims=True)
    expert = grp[:, None] * EXPERTS_PER_GROUP + top_i

    TK = T * TOP_K
    flat_e = expert.reshape(-1)
    flat_tok = jnp.repeat(tok.astype(jnp.int32), TOP_K)
    flat_w = gate.reshape(-1)
    order = jnp.argsort(flat_e)
    e_s, tok_s, w_s = flat_e[order], flat_tok[order], flat_w[order]
    counts = jnp.bincount(flat_e, length=N_EXPERTS)
    starts = jnp.cumsum(counts) - counts
    pcounts = ((counts + EXPERT_BLOCK - 1) // EXPERT_BLOCK) * EXPERT_BLOCK
    pends = jnp.cumsum(pcounts)
    pstarts = pends - pcounts
    dest = pstarts[e_s] + (jnp.arange(TK) - starts[e_s])
    NB = (TK + N_EXPERTS * (EXPERT_BLOCK - 1) + EXPERT_BLOCK - 1) // EXPERT_BLOCK
    P = NB * EXPERT_BLOCK
    buf_tok = jnp.full((P,), T, jnp.int32).at[dest].set(tok_s)
    buf_w = jnp.zeros((P,), jnp.float32).at[dest].set(w_s)
    blk_e = jnp.minimum(jnp.searchsorted(pends, jnp.arange(NB) * EXPERT_BLOCK, side='right'), N_EXPERTS - 1)
    x_pad = jnp.concatenate([xt, jnp.zeros((1, D), xt.dtype)], axis=0)
    xb = x_pad[buf_tok].reshape(NB, EXPERT_BLOCK, D)

    def expert_block(args):
        xblk, e = args
        hdn = jax.nn.silu(xblk @ w_gate[e]) * (xblk @ w_up[e])
        return hdn @ w_down[e]

    yb = lax.map(expert_block, (xb, blk_e)).reshape(P, D)
    y = jnp.zeros((T + 1, D), jnp.float32).at[buf_tok].add(yb.astype(jnp.float32) * buf_w[:, None])
    return y[:T].reshape(B, S, D).astype(xn.dtype)


def setup_inputs(seed: int = 0) -> dict:
    key = jax.random.key(seed)
    ks = jax.random.split(key, 32)
    f32 = jnp.float32
    L, D = DEPTH, D_MODEL

    def nrm(k, shape, scale):
        return jax.random.normal(k, shape, f32) * scale

    def gain(k, shape):
        return 1.0 + 0.05 * jax.random.normal(k, shape, f32)

    start = jax.random.randint(ks[2], (BATCH, 1), 0, 64, dtype=jnp.int32) * CHUNK
    positions = (start + jnp.arange(SEQ, dtype=jnp.int32)[None, :]).astype(jnp.int32)
    return {
        'x': nrm(ks[0], (BATCH, SEQ, D), 1.0),
        'mem': nrm(ks[1], (BATCH, N_MEM, D), 1.0),
        'positions': positions,
        'g_attn': gain(ks[3], (L, D)),
        'w_in': nrm(ks[4], (L, D, IN_COLS), D ** -0.5),
        'q_norm_g': gain(ks[5], (L, DIFF_QKDIM)),
        'k_norm_g': gain(ks[6], (L, DIFF_QKDIM)),
        'lambda_q1': nrm(ks[7], (L, DIFF_QKDIM), 0.1),
        'lambda_k1': nrm(ks[8], (L, DIFF_QKDIM), 0.1),
        'lambda_q2': nrm(ks[9], (L, DIFF_QKDIM), 0.1),
        'lambda_k2': nrm(ks[10], (L, DIFF_QKDIM), 0.1),
        'diff_subln_g': gain(ks[11], (L, DIFF_VDIM)),
        'gla_w_a2': nrm(ks[12], (L, GLA_GATE_RANK, GLA_HEADS * GLA_KDIM), GLA_GATE_RANK ** -0.5),
        'gla_b_a': nrm(ks[13], (L, GLA_HEADS * GLA_KDIM), 0.1),
        'gla_out_g': gain(ks[14], (L, GLA_VDIM)),
        'w_out': nrm(ks[15], (L, D_MIX, D), D_MIX ** -0.5),
        'g_cross': gain(ks[16], (L, D)),
        'g_mem': gain(ks[17], (L, D)),
        'w_cq': nrm(ks[18], (L, D, D), D ** -0.5),
        'w_ckv': nrm(ks[19], (L, D, 2 * D), D ** -0.5),
        'cq_norm_g': gain(ks[20], (L, CROSS_HDIM)),
        'ck_norm_g': gain(ks[21], (L, CROSS_HDIM)),
        'w_co': nrm(ks[22], (L, D, D), D ** -0.5),
        'g_ffn': gain(ks[23], (L, D)),
        'w_router_grp': nrm(ks[24], (L, D, N_GROUPS), D ** -0.5),
        'b_router_grp': nrm(ks[25], (L, N_GROUPS), 0.01),
        'w_router_exp': nrm(ks[26], (L, D, N_EXPERTS), D ** -0.5),
        'b_router_exp': nrm(ks[27], (L, N_EXPERTS), 0.01),
        'w_gate': nrm(ks[28], (L, N_EXPERTS, D, D_EXPERT), D ** -0.5),
        'w_up': nrm(ks[29], (L, N_EXPERTS, D, D_EXPERT), D ** -0.5),
        'w_down': nrm(ks[30], (L, N_EXPERTS, D_EXPERT, D), D_EXPERT ** -0.5),
    }


def reference(x, mem, positions, g_attn, w_in, q_norm_g, k_norm_g, lambda_q1, lambda_k1,
              lambda_q2, lambda_k2, diff_subln_g, gla_w_a2, gla_b_a, gla_out_g, w_out,
              g_cross, g_mem, w_cq, w_ckv, cq_norm_g, ck_norm_g, w_co, g_ffn,
              w_router_grp, b_router_grp, w_router_exp, b_router_exp, w_gate, w_up, w_down):
    B, S, D = x.shape
    h = x
    for l in range(DEPTH):
        n = rmsnorm(h, g_attn[l])
        dq, dk, dv, gq, gk, gv, g_lr, g_r = jnp.split(n @ w_in[l], _split_points(), axis=-1)
        dq = rope(rmsnorm(dq.reshape(B, S, DIFF_HEADS, 2, DIFF_QKDIM), q_norm_g[l]), positions)
        dk = rope(rmsnorm(dk.reshape(B, S, DIFF_HEADS, 2, DIFF_QKDIM), k_norm_g[l]), positions)
        dv = dv.reshape(B, S, DIFF_HEADS, DIFF_VDIM)
        lam_init = 0.8 - 0.6 * math.exp(-0.3 * l)
        lam = (jnp.exp(jnp.sum(lambda_q1[l] * lambda_k1[l]).astype(jnp.float32))
               - jnp.exp(jnp.sum(lambda_q2[l] * lambda_k2[l]).astype(jnp.float32)) + lam_init)
        d_out = diff_attention(dq, dk, dv, lam)
        d_out = rmsnorm(d_out, diff_subln_g[l]) * (1.0 - lam_init)

        log_a = jax.nn.log_sigmoid((g_lr @ gla_w_a2[l] + gla_b_a[l]).astype(jnp.float32)) / GLA_TAU
        g_out = gla(gq.reshape(B, S, GLA_HEADS, GLA_KDIM), gk.reshape(B, S, GLA_HEADS, GLA_KDIM),
                    gv.reshape(B, S, GLA_HEADS, GLA_VDIM), log_a.reshape(B, S, GLA_HEADS, GLA_KDIM))
        g_out = rmsnorm(g_out, gla_out_g[l]) * jax.nn.silu(g_r.reshape(B, S, GLA_HEADS, GLA_VDIM))

        mix = jnp.concatenate([d_out.reshape(B, S, DIFF_WIDTH), g_out.reshape(B, S, GLA_WIDTH)], axis=-1)
        h = h + mix @ w_out[l]

        h = h + cross_attention(rmsnorm(h, g_cross[l]), rmsnorm(mem, g_mem[l]),
                                w_cq[l], w_ckv[l], cq_norm_g[l], ck_norm_g[l], w_co[l])

        h = h + hmoe(rmsnorm(h, g_ffn[l]), w_router_grp[l], b_router_grp[l], w_router_exp[l],
                     b_router_exp[l], w_gate[l], w_up[l], w_down[l])
    return h
```

```python
import numpy as np
import ml_dtypes
import concourse.bass as bass
import concourse.mybir as mybir
from concourse.bass_utils import run_bass_kernel_spmd

F32 = mybir.dt.float32
BF16 = mybir.dt.bfloat16
I32 = mybir.dt.int32
AF = mybir.ActivationFunctionType
ALU = mybir.AluOpType
AX = mybir.AxisListType

EPS = 1e-6
NDMA_SLOTS = 16


class V:
    def __init__(self, ap, keys):
        self.ap = ap
        self.keys = tuple(keys) if isinstance(keys, (list, tuple)) else (keys,)

    def __getitem__(self, idx):
        return V(self.ap[idx], self.keys)

    def k(self, *keys):
        return V(self.ap, keys)


class Sched:
    ENG = ("pe", "act", "dve", "pool", "sp")

    def __init__(self, nc):
        self.nc = nc
        self.ops = []
        self.dma_rr = {"sp": 0, "pool": 0, "act": 0}

    def add(self, issue, fn, reads, writes, dma=False):
        if dma:
            s = self.dma_rr[issue]
            self.dma_rr[issue] = (s + 1) % NDMA_SLOTS
            stream = f"dma_{issue}_{s}"
        else:
            stream = issue
        rk, wk = [], []
        for v in reads:
            rk.extend(v.keys)
        for v in writes:
            wk.extend(v.keys)
        self.ops.append(dict(stream=stream, issue=issue, fn=fn, reads=rk, writes=wk, dma=dma))

    def barrier(self):
        self.ops.append(dict(barrier=True))

    def mm(self, out, lhsT, rhs, start=True, stop=True, extra_reads=()):
        self.add("pe", lambda e: e.matmul(out.ap, lhsT.ap, rhs.ap, start=start, stop=stop),
                 [lhsT, rhs, *extra_reads], [out])

    def tr(self, out, in_, ident):
        self.add("pe", lambda e: e.transpose(out.ap, in_.ap, ident.ap), [in_, ident], [out])

    def act(self, out, in_, func, scale=1.0, bias=0.0, accum=None):
        reads = [in_]
        if isinstance(scale, V):
            reads.append(scale)
        if isinstance(bias, V):
            reads.append(bias)
        writes = [out] + ([accum] if accum is not None else [])
        sc = scale.ap if isinstance(scale, V) else scale
        bi = bias.ap if isinstance(bias, V) else bias
        if accum is None:
            self.add("act", lambda e: e.activation(out.ap, in_.ap, func, bias=bi, scale=sc), reads, writes)
        else:
            self.add("act", lambda e: e.activation(out.ap, in_.ap, func, bias=bi, scale=sc, accum_out=accum.ap),
                     reads, writes)

    def tt(self, eng, out, a, b, op):
        self.add(eng, lambda e: e.tensor_tensor(out.ap, a.ap, b.ap, op), [a, b], [out])

    def ts(self, eng, out, a, s1, op0, s2=None, op1=None):
        reads = [a] + [s for s in (s1, s2) if isinstance(s, V)]
        x1 = s1.ap if isinstance(s1, V) else s1
        x2 = s2.ap if isinstance(s2, V) else s2
        if op1 is None:
            self.add(eng, lambda e: e.tensor_scalar(out.ap, a.ap, x1, None, op0), reads, [out])
        else:
            self.add(eng, lambda e: e.tensor_scalar(out.ap, a.ap, x1, x2, op0, op1), reads, [out])

    def stt(self, out, a, s, b, op0, op1):
        reads = [a, b] + ([s] if isinstance(s, V) else [])
        x = s.ap if isinstance(s, V) else s
        self.add("dve", lambda e: e.scalar_tensor_tensor(out.ap, a.ap, x, b.ap, op0, op1), reads, [out])

    def copy(self, eng, out, in_):
        if eng == "act":
            self.add("act", lambda e: e.copy(out.ap, in_.ap), [in_], [out])
        else:
            self.add(eng, lambda e: e.tensor_copy(out.ap, in_.ap), [in_], [out])

    def recip(self, out, in_):
        self.add("dve", lambda e: e.reciprocal(out.ap, in_.ap), [in_], [out])

    def memset(self, eng, out, val):
        self.add(eng, lambda e: e.memset(out.ap, val), [], [out])

    def dma(self, issue, out, in_):
        self.add(issue, lambda e: e.dma_start(out=out.ap, in_=in_.ap), [in_], [out], dma=True)

    def emit(self):
        nc = self.nc
        stream_pos = {}
        fence = {}
        ops = []
        for op in self.ops:
            if op.get("barrier"):
                fence = dict(stream_pos)
                continue
            p = stream_pos.get(op["stream"], 0) + 1
            stream_pos[op["stream"]] = p
            op["pos"] = p
            op["fence"] = fence
            ops.append(op)
        n = len(ops)
        last_w = {}
        readers = {}
        seen = {e: {} for e in self.ENG}
        for i, op in enumerate(ops):
            need = {}

            def want(j):
                o = ops[j]
                st = o["stream"]
                if st == "pe" and op["stream"] == "pe":
                    return
                if o["pos"] > need.get(st, 0):
                    need[st] = o["pos"]

            for k in op["reads"]:
                if k in last_w:
                    want(last_w[k])
            for k in op["writes"]:
                if k in last_w:
                    want(last_w[k])
                for r in readers.get(k, ()):
                    if r != i:
                        want(r)
            for st, p in op["fence"].items():
                if st == "pe" and op["stream"] == "pe":
                    continue
                if p > need.get(st, 0):
                    need[st] = p
            if op["dma"] and op["pos"] > 1:
                if op["pos"] - 1 > need.get(op["stream"], 0):
                    need[op["stream"]] = op["pos"] - 1
            sn = seen[op["issue"]]
            waits = []
            for st, p in need.items():
                if sn.get(st, 0) < p:
                    sn[st] = p
                    waits.append((st, p))
            op["waits"] = waits
            for k in op["reads"]:
                readers.setdefault(k, []).append(i)
            for k in op["writes"]:
                last_w[k] = i
                readers[k] = []
        by_stream = {}
        for i, op in enumerate(ops):
            by_stream.setdefault(op["stream"], []).append(i)
        signal = set()
        for op in ops:
            for st, p in op["waits"]:
                signal.add((st, p))
            if op["dma"]:
                signal.add((op["stream"], op["pos"]))
        for st, lst in by_stream.items():
            signal.add((st, ops[lst[-1]]["pos"]))
        rank = {}
        for st, lst in by_stream.items():
            r = 0
            for i in lst:
                if (st, ops[i]["pos"]) in signal:
                    r += 1
                    rank[(st, ops[i]["pos"])] = r
        finals = {st: max(v for (s, _), v in rank.items() if s == st) for st in by_stream}
        import contextlib
        with contextlib.ExitStack() as es:
            sems = {st: es.enter_context(nc.semaphore(f"s_{st}")) for st in by_stream}
            block = es.enter_context(nc.Block())
            handles = {"pe": nc.tensor, "act": nc.scalar, "dve": nc.vector, "pool": nc.gpsimd, "sp": nc.sync}

            def run_engine(ename):
                def body(_e):
                    eng = handles[ename]
                    for op in ops:
                        if op["issue"] != ename:
                            continue
                        for st, p in op["waits"]:
                            mult = 16 if st.startswith("dma_") else 1
                            eng.wait_ge(sems[st], rank[(st, p)] * mult)
                        ins = op["fn"](eng)
                        key = (op["stream"], op["pos"])
                        if key in rank:
                            ins.then_inc(sems[op["stream"]], 16 if op["dma"] else 1)
                    if ename == "sp":
                        for st, f in finals.items():
                            mult = 16 if st.startswith("dma_") else 1
                            eng.wait_ge(sems[st], f * mult)
                return body

            block.tensor(run_engine("pe"))
            block.scalar(run_engine("act"))
            block.vector(run_engine("dve"))
            block.gpsimd(run_engine("pool"))
            block.sync(run_engine("sp"))


def _alloc(es, nc, name, shape, dt):
    return es.enter_context(nc.sbuf_tensor("sb_" + name, list(shape), dt))


def rmsnorm_fm(S, src_fn, gcol_fn, dst_fn, nk, ntok, D, tmp, pbank, ones_bf, eps_col, f32_fn=None):
    for k in range(nk):
        sq = tmp["sq"][k % 2][:, 0:ntok]
        S.act(sq, src_fn(k), AF.Square)
        S.mm(pbank[:, 0:ntok], ones_bf, sq, start=(k == 0), stop=(k == nk - 1))
    rt = tmp["rt"][:, 0:ntok]
    S.act(rt, pbank[:, 0:ntok], AF.Sqrt, scale=1.0 / D, bias=eps_col)
    rb = tmp["rb"][:, 0:ntok]
    S.recip(rb, rt)
    for k in range(nk):
        if f32_fn is None:
            S.stt(dst_fn(k), src_fn(k), gcol_fn(k), rb, ALU.mult, ALU.mult)
        else:
            f32_fn(k, rb)


def build_phase2(nc, S, t, es_outer, mix_select=False):
    import contextlib
    es = es_outer
    NT = 1024
    hT = _alloc(es, nc, "hT", [128, 16, NT], F32)
    ones_bf = V(_alloc(es, nc, "ones_bf", [128, 128], BF16)[:], "ones_bf")
    ones_f = V(_alloc(es, nc, "ones_f", [128, 128], F32)[:], "ones_f")
    ident = V(_alloc(es, nc, "ident", [128, 128], F32)[:], "ident")
    eps_col = V(_alloc(es, nc, "eps_col", [128, 1], F32)[:], "eps_col")
    gvec = V(_alloc(es, nc, "gvec", [128, 48], F32)[:], "gvec")
    cg = V(_alloc(es, nc, "cg", [128, 8], F32)[:], "cg")
    tmp = {
        "sq": [V(_alloc(es, nc, f"sq{i}", [128, 512], BF16)[:], f"sq{i}") for i in range(2)],
        "rt": V(_alloc(es, nc, "rt", [128, 512], F32)[:], "rt"),
        "rb": V(_alloc(es, nc, "rb", [128, 512], F32)[:], "rb"),
    }
    pb = [V(es.enter_context(nc.psum_tensor(f"pb{i}", [128, 512], F32))[:], f"pb{i}") for i in range(8)]
    wpool = []

    def hv(k, half):
        return V(hT[:, k, half * 512:(half + 1) * 512], f"hT_{k}_{half}")

    S.memset("dve", ones_bf, 1.0)
    S.memset("dve", ones_f, 1.0)
    S.memset("dve", eps_col, EPS)
    S.dma("sp", ident, t["ident"])
    S.dma("sp", gvec, t["gvec"])
    S.dma("sp", cg, t["cg"])

    def wview(i, a, b):
        return V(wpool[i][:].rearrange("p (a b) -> p a b", a=a), f"wp{i}")

    state = {"wi": 0, "pbi": 0}

    def next_w():
        i = state["wi"]
        state["wi"] = (i + 1) % 2
        return i

    def next_pb(lo=4, n=4):
        i = state["pbi"]
        state["pbi"] = (i + 1) % n
        return pb[lo + i]

    def linear_fm(wdram, col0, ncols, in_fn, nk, ntoks, evac):
        for g in range(ncols // 512):
            wi = next_w()
            wv = wview(wi, nk, 512)
            S.dma("pool", wv, V(wdram.ap[:, col0 + g * 512: col0 + (g + 1) * 512].rearrange("(k p) n -> p k n", p=128), wdram.keys))
            for ocl in range(4):
                oc = g * 4 + ocl
                for ti, nt in enumerate(ntoks):
                    ps = next_pb()
                    for k in range(nk):
                        S.mm(ps[:, 0:nt], wv[:, k, ocl * 128:(ocl + 1) * 128], in_fn(k, ti), start=(k == 0), stop=(k == nk - 1))
                    evac(oc, ti, ps[:, 0:nt])

    with contextlib.ExitStack() as esA:
        wpool[:] = [_alloc(esA, nc, f"wpA{i}", [128, 8192], BF16) for i in range(2)]
        mixT = _alloc(esA, nc, "mixT", [128, 16, NT], BF16)
        if mix_select:
            stage = [V(_alloc(esA, nc, f"mstg{q}", [128, 1024], BF16)[:], f"mstg{q}") for q in range(4)]
            sel = V(_alloc(esA, nc, "sel", [128, 4], F32)[:], "sel")
            S.dma("sp", sel, t["sel"])
        for k in range(16):
            if mix_select:
                mk = V(mixT[:, k, :], f"mixT_{k}")
                for q in range(4):
                    S.dma("sp", stage[q], V(t["mixT"].ap[k * 128:(k + 1) * 128, q * 1024:(q + 1) * 1024], t["mixT"].keys))
                S.ts("dve", mk, stage[0], sel[:, 0:1], ALU.mult)
                for q in range(1, 4):
                    S.stt(mk, stage[q], sel[:, q:q + 1], mk, ALU.mult, ALU.add)
            else:
                S.dma("sp", V(mixT[:, k, :], f"mixT_{k}"), V(t["mixT"].ap[k * 128:(k + 1) * 128, :], t["mixT"].keys))
            for half in range(2):
                S.dma("sp", hv(k, half), V(t["xT2"].ap[k * 128:(k + 1) * 128, half * 512:(half + 1) * 512], t["xT2"].keys))

        def evacA(oc, ti, ps):
            S.tt("dve", hv(oc, ti), ps, hv(oc, ti), ALU.add)

        linear_fm(t["w_out"], 0, 2048, lambda k, ti: V(mixT[:, k, ti * 512:(ti + 1) * 512], f"mixT_{k}"), 16, [512, 512], evacA)
    S.barrier()

    with contextlib.ExitStack() as esB:
        wpool[:] = [_alloc(esB, nc, f"wpB{i}", [128, 8192], BF16) for i in range(2)]
        kT = _alloc(esB, nc, "kT", [128, 16, 256], BF16)
        v_sb = _alloc(esB, nc, "v_sb", [128, 2, 2048], BF16)
        with contextlib.ExitStack() as esB1:
            memT = _alloc(esB1, nc, "memT", [128, 16, 256], F32)
            memnT = _alloc(esB1, nc, "memnT", [128, 16, 256], BF16)
            kraw = _alloc(esB1, nc, "kraw", [128, 16, 256], F32)
            for k in range(16):
                S.dma("sp", V(memT[:, k, :], f"memT_{k}"), V(t["memT"].ap[k * 128:(k + 1) * 128, :], t["memT"].keys))
            rmsnorm_fm(S, lambda k: V(memT[:, k, :], f"memT_{k}"), lambda k: gvec[:, 16 + k:17 + k],
                       lambda k: V(memnT[:, k, :], f"memnT_{k}"), 16, 256, 2048.0, tmp, pb[0], ones_bf, eps_col)

            def evacK(oc, ti, ps):
                S.copy("act", V(kraw[:, oc, :], f"kraw_{oc}"), ps)

            linear_fm(t["w_ckv"], 0, 2048, lambda k, ti: V(memnT[:, k, :], f"memnT_{k}"), 16, [256], evacK)
            for h in range(4):
                rmsnorm_fm(S, lambda dc: V(kraw[:, h * 4 + dc, :], f"kraw_{h * 4 + dc}"), lambda dc: cg[:, 4 + dc:5 + dc],
                           lambda dc: V(kT[:, h * 4 + dc, :], f"kT_{h * 4 + dc}"), 4, 256, 512.0, tmp, pb[0], ones_bf, eps_col)
            for g in range(4):
                wi = next_w()
                wv = wview(wi, 16, 512)
                S.dma("pool", wv, V(t["w_ckv"].ap[:, 2048 + g * 512: 2048 + (g + 1) * 512].rearrange("(k p) n -> p k n", p=128), t["w_ckv"].keys))
                for mc in range(2):
                    ps = next_pb()
                    for k in range(16):
                        S.mm(ps, V(memnT[:, k, mc * 128:(mc + 1) * 128], f"memnT_{k}"), wv[:, k, :], start=(k == 0), stop=(k == 15))
                    S.copy("act", V(v_sb[:, mc, g * 512:(g + 1) * 512], f"v_sb_{mc}_{g}"), ps)
        S.barrier()
        xnh = _alloc(esB, nc, "xnh", [128, 16, 512], BF16)
        oTh = _alloc(esB, nc, "oTh", [128, 16, 512], BF16)
        qraw = _alloc(esB, nc, "qraw", [128, 4, 512], F32)
        qT = _alloc(esB, nc, "qT", [128, 4, 512], BF16)
        pT = _alloc(esB, nc, "pT", [128, 2, 512], BF16)
        rden = V(_alloc(esB, nc, "rden", [128, 512], F32)[:], "rden")
        for half in range(2):
            rmsnorm_fm(S, lambda k: hv(k, half), lambda k: gvec[:, k:k + 1],
                       lambda k: V(xnh[:, k, :], f"xnh_{k}"), 16, 512, 2048.0, tmp, pb[0], ones_bf, eps_col)
            for h in range(4):
                wi = next_w()
                wv = wview(wi, 16, 512)
                S.dma("pool", wv, V(t["w_cq"].ap[:, h * 512:(h + 1) * 512].rearrange("(k p) n -> p k n", p=128), t["w_cq"].keys))
                for dc in range(4):
                    ps = next_pb()
                    for k in range(16):
                        S.mm(ps, wv[:, k, dc * 128:(dc + 1) * 128], V(xnh[:, k, :], f"xnh_{k}"), start=(k == 0), stop=(k == 15))
                    S.copy("act", V(qraw[:, dc, :], f"qraw_{dc}"), ps)
                rmsnorm_fm(S, lambda dc: V(qraw[:, dc, :], f"qraw_{dc}"), lambda dc: cg[:, dc:dc + 1],
                           lambda dc: V(qT[:, dc, :], f"qT_{dc}"), 4, 512, 512.0, tmp, pb[0], ones_bf, eps_col)
                for mc in range(2):
                    ps = next_pb()
                    for dc in range(4):
                        S.mm(ps, V(kT[:, h * 4 + dc, mc * 128:(mc + 1) * 128], f"kT_{h * 4 + dc}"), V(qT[:, dc, :], f"qT_{dc}"),
                             start=(dc == 0), stop=(dc == 3))
                    S.act(V(pT[:, mc, :], f"pT_{mc}"), ps, AF.Exp, scale=512.0 ** -0.5)
                ps = pb[1]
                for mc in range(2):
                    S.mm(ps, ones_bf, V(pT[:, mc, :], f"pT_{mc}"), start=(mc == 0), stop=(mc == 1))
                S.recip(rden, ps)
                for dvc in range(4):
                    ps = next_pb()
                    for mc in range(2):
                        S.mm(ps, V(v_sb[:, mc, h * 512 + dvc * 128: h * 512 + (dvc + 1) * 128], f"v_sb_{mc}_{h}"), V(pT[:, mc, :], f"pT_{mc}"),
                             start=(mc == 0), stop=(mc == 1))
                    S.tt("dve", V(oTh[:, h * 4 + dvc, :], f"oTh_{h * 4 + dvc}"), ps, rden, ALU.mult)

            def evacO(oc, ti, ps):
                S.tt("dve", hv(oc, half), ps, hv(oc, half), ALU.add)

            linear_fm(t["w_co"], 0, 2048, lambda k, ti: V(oTh[:, k, :], f"oTh_{k}"), 16, [512], evacO)
    S.barrier()

    with contextlib.ExitStack() as esC:
        wpool[:] = [_alloc(esC, nc, f"wpC{i}", [128, 8192], BF16) for i in range(5)]
        xnT = _alloc(esC, nc, "xnT", [128, 16, NT], BF16)
        hid = _alloc(esC, nc, "hid", [128, 4, NT], BF16)
        xf = [V(_alloc(esC, nc, f"xf{i}", [128, 512], F32)[:], f"xf{i}") for i in range(2)]
        w_r = V(_alloc(esC, nc, "w_r", [128, 16, 36], F32)[:], "w_r")
        b_r = V(_alloc(esC, nc, "b_r", [1, 36], F32)[:], "b_r")
        GT = _alloc(esC, nc, "GT", [32, NT], F32)
        L = V(_alloc(esC, nc, "L", [128, 36], F32)[:], "L")
        sm = V(_alloc(esC, nc, "sm", [128, 16], F32)[:], "sm")
        gm = V(_alloc(esC, nc, "gm", [128, 4], F32)[:], "gm")
        pen = V(_alloc(esC, nc, "pen", [128, 4], F32)[:], "pen")
        ge = V(_alloc(esC, nc, "ge", [128, 4], F32)[:], "ge")
        elm = V(_alloc(esC, nc, "elm", [128, 32], F32)[:], "elm")
        elm2 = V(_alloc(esC, nc, "elm2", [128, 32], F32)[:], "elm2")
        mk1 = V(_alloc(esC, nc, "mk1", [128, 32], F32)[:], "mk1")
        mk2 = V(_alloc(esC, nc, "mk2", [128, 32], F32)[:], "mk2")
        G = V(_alloc(esC, nc, "G", [128, 32], F32)[:], "G")
        sg_t = xf
        gbc = [tmp["rt"], tmp["rb"]]
        S.dma("sp", w_r, V(t["w_r"].ap.rearrange("(k p) n -> p k n", p=128), t["w_r"].keys))
        S.dma("sp", b_r, t["b_r"])
        BIG = 1.0e30
        for half in range(2):
            def f32_fn(k, rb, half=half):
                x = xf[k % 2]
                S.stt(x, hv(k, half), gvec[:, 32 + k:33 + k], rb, ALU.mult, ALU.mult)
                for tb in range(4):
                    S.mm(pb[4 + tb][:, 0:36], x[:, tb * 128:(tb + 1) * 128], w_r[:, k, :], start=(k == 0), stop=False)
                S.copy("act", V(xnT[:, k, half * 512:(half + 1) * 512], f"xnT_{k}_{half}"), x)

            rmsnorm_fm(S, lambda k: hv(k, half), None, None, 16, 512, 2048.0, tmp, pb[0], ones_bf, eps_col, f32_fn=f32_fn)
            for tb in range(4):
                S.mm(pb[4 + tb][:, 0:36], ones_f[0:1, 0:128], b_r, start=False, stop=True)
                S.copy("dve", L, pb[4 + tb][:, 0:36])
                gmax, gsum, grw, m1, m2, d12, sgm, g1, g2, ngmax = [sm[:, i:i + 1] for i in range(10)]
                S.add("dve", lambda e, o=gmax, i=L: e.reduce_max(o.ap, i.ap[:, 0:4], AX.X), [L], [gmax])
                S.ts("dve", gm, L[:, 0:4], gmax, ALU.is_equal)
                S.ts("dve", ngmax, gmax, -1.0, ALU.mult)
                S.act(ge, L[:, 0:4], AF.Exp, bias=ngmax, accum=gsum)
                S.recip(grw, gsum)
                S.ts("dve", pen, gm, BIG, ALU.mult, -BIG, ALU.add)
                for g in range(4):
                    S.ts("dve", elm[:, g * 8:(g + 1) * 8], L[:, 4 + g * 8: 4 + (g + 1) * 8], pen[:, g:g + 1], ALU.add)
                S.add("dve", lambda e, o=m1, i=elm: e.reduce_max(o.ap, i.ap, AX.X), [elm], [m1])
                S.ts("dve", mk1, elm, m1, ALU.is_equal)
                S.stt(elm2, mk1, -BIG, elm, ALU.mult, ALU.add)
                S.add("dve", lambda e, o=m2, i=elm2: e.reduce_max(o.ap, i.ap, AX.X), [elm2], [m2])
                S.ts("dve", mk2, elm2, m2, ALU.is_equal)
                S.tt("dve", d12, m1, m2, ALU.subtract)
                S.act(sgm, d12, AF.Sigmoid)
                S.tt("dve", g1, sgm, grw, ALU.mult)
                S.tt("dve", g2, grw, g1, ALU.subtract)
                S.ts("dve", G, mk1, g1, ALU.mult)
                S.stt(G, mk2, g2, G, ALU.mult, ALU.add)
                S.tr(pb[3][0:32, 0:128], G, ident)
                col = half * 512 + tb * 128
                S.copy("dve", V(GT[:, col:col + 128], f"GT_{half}"), pb[3][0:32, 0:128])
        for e in range(32):
            wg = V(wpool[e % 2][:].rearrange("p (a b) -> p a b", a=16), f"wp{e % 2}")
            wu = V(wpool[2 + e % 2][:].rearrange("p (a b) -> p a b", a=16), f"wp{2 + e % 2}")
            wd = V(wpool[4][:].rearrange("p (a b) -> p a b", a=4), "wp4")
            S.dma("pool", wg, V(t["w_gate"].ap[e].rearrange("(k p) n -> p k n", p=128), t["w_gate"].keys))
            S.dma("pool", wu, V(t["w_up"].ap[e].rearrange("(k p) n -> p k n", p=128), t["w_up"].keys))
            S.dma("pool", wd, V(t["w_down"].ap[e].rearrange("(k p) n -> p k n", p=128), t["w_down"].keys))
            for half in range(2):
                gb = gbc[half]
                S.mm(pb[1], V(ident.ap[0:32, e:e + 1].broadcast_to([32, 128]), ident.keys), V(GT[:, half * 512:(half + 1) * 512], f"GT_{half}"))
                S.copy("act", gb, pb[1])
                for fc in range(4):
                    pg, pu = pb[2], pb[3]
                    for k in range(16):
                        S.mm(pg, wg[:, k, fc * 128:(fc + 1) * 128], V(xnT[:, k, half * 512:(half + 1) * 512], f"xnT_{k}_{half}"),
                             start=(k == 0), stop=(k == 15))
                    for k in range(16):
                        S.mm(pu, wu[:, k, fc * 128:(fc + 1) * 128], V(xnT[:, k, half * 512:(half + 1) * 512], f"xnT_{k}_{half}"),
                             start=(k == 0), stop=(k == 15))
                    sg = sg_t[fc % 2]
                    S.act(sg, pg, AF.Silu)
                    S.tt("pool", sg, sg, gb, ALU.mult)
                    S.tt("dve", V(hid[:, fc, half * 512:(half + 1) * 512], f"hid_{fc}_{half}"), pu, sg, ALU.mult)
                for oc in range(16):
                    ps = next_pb()
                    for fc in range(4):
                        S.mm(ps, wd[:, fc, oc * 128:(oc + 1) * 128], V(hid[:, fc, half * 512:(half + 1) * 512], f"hid_{fc}_{half}"),
                             start=(fc == 0), stop=(fc == 3))
                    S.tt("dve", hv(oc, half), ps, hv(oc, half), ALU.add)
        for k in range(16):
            for half in range(2):
                S.dma("sp", V(t["outT"].ap[k * 128:(k + 1) * 128, half * 512:(half + 1) * 512], t["outT"].keys), hv(k, half))


P2_INPUTS = [
    ("mixT", [2048, 1024], BF16), ("xT2", [2048, 1024], F32), ("memT", [2048, 256], F32),
    ("w_out", [2048, 2048], F32), ("w_cq", [2048, 2048], F32), ("w_co", [2048, 2048], F32),
    ("w_ckv", [2048, 4096], F32), ("gvec", [128, 48], F32), ("cg", [128, 8], F32), ("ident", [128, 128], F32),
    ("w_r", [2048, 36], F32), ("b_r", [1, 36], F32),
    ("w_gate", [32, 2048, 512], F32), ("w_up", [32, 2048, 512], F32), ("w_down", [32, 512, 2048], F32),
]


def host_phase2_inputs(inp, c):
    b, j = c // 4, c % 4
    tok = own_tokens(c)

    def pcol(v):
        return np.ascontiguousarray(v.reshape(-1, 128).T)

    gvec = np.concatenate([pcol(inp["g_cross"][0]), pcol(inp["g_mem"][0]), pcol(inp["g_ffn"][0])], axis=1)
    cg = np.concatenate([pcol(inp["cq_norm_g"][0]), pcol(inp["ck_norm_g"][0])], axis=1)
    return {
        "xT2": np.ascontiguousarray(inp["x"][b, tok, :].T),
        "memT": np.ascontiguousarray(inp["mem"][b].T),
        "w_out": inp["w_out"][0], "w_cq": inp["w_cq"][0], "w_co": inp["w_co"][0], "w_ckv": inp["w_ckv"][0],
        "gvec": np.ascontiguousarray(gvec, dtype=np.float32), "cg": np.ascontiguousarray(cg, dtype=np.float32),
        "ident": np.eye(128, dtype=np.float32),
        "w_r": np.ascontiguousarray(np.concatenate([inp["w_router_grp"][0], inp["w_router_exp"][0]], axis=1)),
        "b_r": np.ascontiguousarray(np.concatenate([inp["b_router_grp"][0], inp["b_router_exp"][0]])[None, :]),
        "w_gate": inp["w_gate"][0], "w_up": inp["w_up"][0], "w_down": inp["w_down"][0],
    }


def build_nc_phase2_only():
    import contextlib
    nc = bass.Bass("TRN2", target_bir_lowering=False)
    t = {}
    for name, shape, dt in P2_INPUTS:
        t[name] = V(nc.dram_tensor(name, shape, dt, kind="ExternalInput").ap(), "dram_" + name)
    t["outT"] = V(nc.dram_tensor("outT", [2048, 1024], F32, kind="ExternalOutput").ap(), "dram_outT")
    S = Sched(nc)
    with contextlib.ExitStack() as es:
        build_phase2(nc, S, t, es)
        S.emit()
    return nc


P1_INPUTS = [
    ("xT1", [2048, 4096], F32), ("w1", [1, 2048, 1552], F32), ("pp1", [128, 8], F32), ("pos", [1, 4096], I32),
    ("g1", [128, 16], F32), ("lam4", [1, 256], F32), ("subg", [1, 128], F32), ("gog", [1, 256], F32),
    ("wa2", [1, 16, 128], F32), ("ba", [1, 1, 128], F32), ("ident1", [128, 128], F32), ("mstrict", [128, 128], F32),
    ("chunkind", [128, 2], F32), ("protT", [128, 128], F32), ("sel4", [128, 4], F32), ("maskT", [128, 512], F32),
]
_DBG_STOP = [99]
C_QA, C_QB, C_KA, C_KB, C_GQ, C_LR, C_TM1, C_TM2, NW1 = 0, 128, 256, 384, 512, 640, 656, 1168, 1552


def host_w1(inp, j):
    w = inp["w_in"][0]
    hA, hB = 2 * j, 2 * j + 1
    cols = []
    cols += [w[:, hA * 128:(hA + 1) * 128], w[:, hB * 128:(hB + 1) * 128]]
    cols += [w[:, 1024 + hA * 128:1024 + (hA + 1) * 128], w[:, 1024 + hB * 128:1024 + (hB + 1) * 128]]
    cols += [w[:, 3072 + j * 128:3072 + (j + 1) * 128]]
    cols += [w[:, 5120:5136]]
    cols += [w[:, 2048 + hA * 128:2048 + (hA + 1) * 128], w[:, 2048 + hB * 128:2048 + (hB + 1) * 128]]
    cols += [w[:, 5136 + j * 256:5136 + (j + 1) * 256]]
    cols += [w[:, 3584 + j * 128:3584 + (j + 1) * 128]]
    cols += [w[:, 4096 + j * 256:4096 + (j + 1) * 256]]
    w1 = np.ascontiguousarray(np.concatenate(cols, axis=1))
    assert w1.shape == (2048, NW1)
    return w1


def host_phase1_inputs(inp, c, groups=None):
    b, j = c // 4, c % 4
    groups = [j] if groups is None else groups
    w1 = np.stack([host_w1(inp, g) for g in groups], axis=0)
    pp1 = np.zeros((128, 8), np.float32)
    pp1[:, 0] = np.tile(inp["q_norm_g"][0], 2)
    pp1[:, 2] = np.tile(inp["k_norm_g"][0], 2)
    half = 32
    freq = (np.float32(10000.0) ** (-np.arange(half, dtype=np.float32) / np.float32(half))).astype(np.float32)
    pp1[:, 4] = np.tile(freq, 4)
    lam4 = np.concatenate([inp["lambda_q1"][0], inp["lambda_k1"][0], inp["lambda_q2"][0], inp["lambda_k2"][0]])[None, :]
    l = np.arange(128)
    mstrict = ((l[:, None] > l[None, :]) & ((l[:, None] // 64) == (l[None, :] // 64))).astype(np.float32)
    chunkind = np.stack([(l < 64), (l >= 64)], axis=1).astype(np.float32)
    protT = np.zeros((128, 128), np.float32)
    for m in range(128):
        if (m % 64) < 32:
            protT[m + 32, m] = -1.0
        else:
            protT[m - 32, m] = 1.0
    return {
        "xT1": np.ascontiguousarray(inp["x"][b].T), "w1": w1, "pp1": pp1,
        "pos": np.ascontiguousarray(inp["positions"][b][None, :].astype(np.int32)),
        "g1": np.ascontiguousarray(inp["g_attn"][0].reshape(16, 128).T),
        "lam4": np.ascontiguousarray(lam4.astype(np.float32)),
        "subg": np.ascontiguousarray(inp["diff_subln_g"][0][None, :]),
        "gog": np.ascontiguousarray(inp["gla_out_g"][0][None, :]),
        "wa2": np.ascontiguousarray(np.stack([inp["gla_w_a2"][0][:, g * 128:(g + 1) * 128] for g in groups], axis=0)),
        "ba": np.ascontiguousarray(np.stack([inp["gla_b_a"][0][None, g * 128:(g + 1) * 128] for g in groups], axis=0)),
        "ident1": np.eye(128, dtype=np.float32), "mstrict": mstrict, "chunkind": chunkind, "protT": protT,
        "sel4": own_sel(c), "maskT": own_mask(c),
    }


def own_sel(c):
    sel = np.zeros((128, 4), np.float32)
    sel[:, c % 4] = 1.0
    return sel


def own_mask(c):
    j = c % 4
    M = np.zeros((128, 4, 128), np.float32)
    for m in range(4):
        if m < j:
            M[:, m, :] = 1.0
        elif m == j:
            M[:, m, :] = 1.0
            M[64:128, m, 0:64] = 0.0
    return np.ascontiguousarray(M.reshape(128, 512))


def own_tokens(c):
    j = c % 4
    return np.concatenate([np.arange((4 * i + j) * 128, (4 * i + j + 1) * 128) for i in range(8)])


def build_phase1(nc, S, t, es, ngroups=8, npass=1, row_of=None):
    import math
    NTOK = 4096
    LAM_INIT = 0.8 - 0.6 * math.exp(-0.3 * 0)
    PI = math.pi

    def A(name, shape, dt):
        return _alloc(es, nc, "p1" + name, shape, dt)

    w1s = A("w1s", [128, 16, NW1], BF16)
    KT = A("KT", [128, 2, NTOK], BF16)
    QTo = A("QTo", [128, 2, 128], BF16)
    Vaug = A("Vaug", [128, 32, 2, 130], BF16)
    cosT = A("cosT", [128, NTOK], BF16)
    sinT = A("sinT", [128, NTOK], BF16)
    gqo = V(A("gqo", [128, 128], BF16)[:], "gqo")
    xno = A("xno", [128, 16, 128], BF16)
    cs2all = A("cs2all", [128, 8, 256], BF16)
    sn2all = A("sn2all", [128, 8, 256], BF16)
    maskT = V(A("maskT", [128, 512], BF16)[:], "maskT")
    sel4 = V(A("sel4", [128, 4], F32)[:], "sel4")
    osel = V(A("osel", [128, 256], F32)[:], "osel")
    xg = A("xg", [128, 16, 512], F32)
    xn = A("xn", [128, 16, 512], BF16)
    ones_bf = V(A("ones_bf", [128, 128], BF16)[:], "ones_bf")
    onesblk = V(A("onesblk", [128, 128], BF16)[:], "onesblk")
    protT = V(A("protT", [128, 128], BF16)[:], "protT")
    ident = V(A("ident", [128, 128], F32)[:], "ident")
    mstrict = V(A("mstrict", [128, 128], F32)[:], "mstrict")
    chunkind = V(A("chunkind", [128, 2], F32)[:], "chunkind")
    ones_f = V(A("ones_f", [128, 128], F32)[:], "ones_f")
    eps_col = V(A("eps_col", [128, 1], F32)[:], "eps_col")
    pp1 = V(A("pp1", [128, 8], F32)[:], "pp1")
    g1 = V(A("g1", [128, 16], F32)[:], "g1")
    lamt = V(A("lamt", [128, 256], F32)[:], "lamt")
    lamv = V(A("lamv", [128, 8], F32)[:], "lamv")
    subg_bc = V(A("subg_bc", [128, 128], F32)[:], "subg_bc")
    gog_bc = V(A("gog_bc", [128, 256], F32)[:], "gog_bc")
    wa2 = V(A("wa2", [16, 128], F32)[:], "wa2")
    ba = V(A("ba", [1, 128], F32)[:], "ba")
    tmp = {
        "sq": [V(A(f"sq{i}", [128, 512], BF16)[:], f"sq{i}") for i in range(2)],
        "rt": V(A("rt", [128, 512], F32)[:], "rt"),
        "rb": V(A("rb", [128, 512], F32)[:], "rb"),
    }
    qn_bf = V(A("qn_bf", [128, 512], BF16)[:], "qn_bf")
    t1 = V(A("t1", [128, 512], F32)[:], "t1")
    t2 = V(A("t2", [128, 512], F32)[:], "t2")
    rtB = V(A("rtB", [128, 512], F32)[:], "rtB")
    rbB = V(A("rbB", [128, 512], F32)[:], "rbB")
    qnB = V(A("qnB", [128, 512], BF16)[:], "qnB")
    t1B = V(A("t1B", [128, 512], F32)[:], "t1B")
    t2B = V(A("t2B", [128, 512], F32)[:], "t2B")
    pexp = [V(A(f"pexp{i}", [128, 512], BF16)[:], f"pexp{i}") for i in range(2)]
    glrT = V(A("glrT", [16, 512], F32)[:], "glrT")
    pexp = pexp + [V(A(f"pexp{i}", [128, 512], BF16)[:], f"pexp{i}") for i in (2, 3)]
    ez = [V(t1.ap[:, i * 128:(i + 1) * 128], "t1") for i in range(4)]
    wexp = ez
    lsp = [V(t2.ap[:, i * 128:(i + 1) * 128], "t2") for i in range(4)]
    k_sb = [V(tmp["rt"].ap[:, i * 128:(i + 1) * 128], "rt") for i in range(4)]
    kdec = [V(tmp["sq"][0].ap[:, i * 128:(i + 1) * 128], "sq0") for i in range(4)]
    vg = [V(tmp["sq"][1].ap[:, 0:256], "sq1"), V(tmp["sq"][1].ap[:, 256:512], "sq1"),
          V(qn_bf.ap[:, 0:256], "qn_bf"), V(qn_bf.ap[:, 256:512], "qn_bf")]
    go = [V(tmp["rb"].ap[:, 0:256], "rb"), V(tmp["rb"].ap[:, 256:512], "rb")]
    sgr = V(A("sgr", [128, 256], F32)[:], "sgr")
    Sst2 = [V(A(f"Sst{i}", [128, 256], F32)[:], f"Sst{i}") for i in range(2)]
    Sst = Sst2[0]
    Sbf = [V(A(f"Sbf{i}", [128, 256], BF16)[:], f"Sbf{i}") for i in range(8)]
    dec = V(A("dec", [128, 8], F32)[:], "dec")
    junk = V(t2.ap[:, 0:256], "t2")
    junk2 = V(t2.ap[:, 256:384], "t2")
    gsm = V(A("gsm", [128, 16], F32)[:], "gsm")
    o1 = V(A("o1", [128, 128], F32)[:], "o1")
    dd = V(A("dd", [128, 128], F32)[:], "dd")
    dn = [V(A(f"dn{i}", [128, 128], F32)[:], f"dn{i}") for i in range(2)]
    asm = V(A("asm", [128, 8], F32)[:], "asm")
    mstage = [V(A(f"mstage{r}", [128, 512], BF16)[:], f"mstage{r}") for r in range(4)]
    pb = [V(es.enter_context(nc.psum_tensor(f"p1pb{i}", [128, 512], F32))[:], f"pb{i}") for i in range(8)]

    if row_of is None:
        row_of = lambda hg, r: r * 128
    S.dma("pool", protT, t["protT"])
    S.dma("pool", maskT, t["maskT"])
    for dst, src in [(ident, "ident1"), (mstrict, "mstrict"), (chunkind, "chunkind"), (pp1, "pp1"), (g1, "g1"), (sel4, "sel4")]:
        S.dma("sp", dst, t[src])
    S.dma("sp", lamt, V(t["lam4"].ap.partition_broadcast(128), t["lam4"].keys))
    S.dma("sp", subg_bc, V(t["subg"].ap.partition_broadcast(128), t["subg"].keys))
    S.dma("sp", gog_bc, V(t["gog"].ap.partition_broadcast(128), t["gog"].keys))
    S.memset("dve", ones_bf, 1.0)
    S.memset("dve", ones_f, 1.0)
    S.memset("dve", eps_col, EPS)
    S.memset("dve", onesblk, 0.0)
    S.memset("dve", onesblk[0:64, 0:64], 1.0)
    S.memset("dve", onesblk[64:128, 64:128], 1.0)
    S.memset("pool", V(Vaug[:], [f"Vaug_{g}" for g in range(8)]), 1.0)
    for i in range(2):
        S.tt("dve", junk2[:, 0:64], lamt[:, i * 128:i * 128 + 64], lamt[:, i * 128 + 64:i * 128 + 128], ALU.mult)
        S.add("dve", lambda e, o=lamv[:, i:i + 1], x=junk2: e.reduce_sum(o.ap, x.ap[:, 0:64], AX.X), [junk2], [lamv])
        S.act(lamv[:, 2 + i:3 + i], lamv[:, i:i + 1], AF.Exp)
    S.tt("dve", lamv[:, 4:5], lamv[:, 2:3], lamv[:, 3:4], ALU.subtract)
    S.ts("dve", lamv[:, 5:6], lamv[:, 4:5], LAM_INIT, ALU.add, -1.0, ALU.mult)
    neg_lam = lamv[:, 5:6]
    S.ts("dve", subg_bc, subg_bc, 1.0 - LAM_INIT, ALU.mult)
    xgf = xg[:].rearrange("p a b -> p (a b)")
    posi = V(xgf[:, 0:4096].bitcast(I32), "xg_tab0")
    angf = V(xgf[:, 4096:8192], "xg_tab1")
    kf = V(xgf[:, 0:4096], "xg_tab0")
    ki = V(xgf[:, 0:4096].bitcast(I32), "xg_tab0")
    S.dma("sp", posi, V(t["pos"].ap.partition_broadcast(128), t["pos"].keys))
    S.copy("dve", angf, posi)
    S.ts("dve", angf, angf, pp1[:, 4:5], ALU.mult)
    S.ts("dve", t1.k("xg_tab0")[:, 0:1], angf[:, 0:1], 1.0, ALU.mult)
    for c0 in range(0, 4096, 2048):
        sl = slice(c0, c0 + 2048)
        S.ts("dve", kf[:, sl], angf[:, sl], 1.0 / (2 * PI), ALU.mult)
        S.copy("dve", ki[:, sl], kf[:, sl])
        S.copy("dve", kf[:, sl], ki[:, sl])
        C1 = 6.28125
        C2 = 2 * PI - C1
        S.stt(angf[:, sl], kf[:, sl], -C1, angf[:, sl], ALU.mult, ALU.add)
        S.stt(angf[:, sl], kf[:, sl], -C2, angf[:, sl], ALU.mult, ALU.add)
        msk = V(xn[:].rearrange("p a b -> p (a b)").bitcast(F32)[:, 0:2048], "xn_tab")
        PIS = 3.1415925

        def wrap(dst, src, shift):
            S.ts("dve", dst, src, shift, ALU.add)
            S.ts("dve", msk, dst, PI, ALU.is_gt)
            S.stt(dst, msk, -2 * PI, dst, ALU.mult, ALU.add)
            S.ts("dve", msk, dst, -PI, ALU.is_lt)
            S.stt(dst, msk, 2 * PI, dst, ALU.mult, ALU.add)
            S.ts("dve", dst, dst, PIS, ALU.min, -PIS, ALU.max)

        wrap(kf[:, sl], angf[:, sl], 0.0)
        S.act(V(sinT[:, sl], "sinT"), kf[:, sl], AF.Sin)
        wrap(kf[:, sl], angf[:, sl], PI / 2)
        S.act(V(cosT[:, sl], "cosT"), kf[:, sl], AF.Sin)
    S.barrier()

    loaded = set()

    def issue_loads(hg, tg):
        if tg >= ngroups:
            hg, tg = hg + 1, 0
        if hg >= npass or (hg, tg) in loaded:
            return
        loaded.add((hg, tg))
        c_ = slice(tg * 512, (tg + 1) * 512)
        if hg == 0:
            for k in range(16):
                S.dma("sp", V(xg[:, k, :], f"xg_{k}"), V(t["xT1"].ap[k * 128:(k + 1) * 128, c_], t["xT1"].keys))
        else:
            S.dma("sp", V(xno[:], "xno"), V(t["xnoscr"].ap[:, tg], f"xnoscr_{tg}"))
            for k in range(16):
                S.dma("sp", V(xn[:, k, :], f"xn_{k}"), V(t["xnscr"].ap[k * 128:(k + 1) * 128, c_], f"xnscr_{tg}"))

    for hg, tg in [(a, b_) for a in range(npass) for b_ in range(ngroups)]:
        cols = slice(tg * 512, (tg + 1) * 512)
        if tg == 0:
            for k in range(16):
                S.dma("pool", V(w1s[:, k, :], f"w1s_{k}"), V(t["w1"].ap[hg, k * 128:(k + 1) * 128, :], t["w1"].keys))
            S.dma("sp", wa2, V(t["wa2"].ap[hg], t["wa2"].keys))
            S.dma("sp", ba, V(t["ba"].ap[hg], t["ba"].keys))
            S.memset("dve", Sst, 0.0)
        def pick(dst, srcs):
            S.ts("dve", dst, srcs[0], sel4[:, 0:1], ALU.mult)
            for q in range(1, 4):
                S.stt(dst, srcs[q], sel4[:, q:q + 1], dst, ALU.mult, ALU.add)

        xn_all = [f"xn_{k}" for k in range(16)]
        gc0 = tg * 512
        cs2 = V(cs2all[:, tg, :], f"cs2_{tg}")
        sn2 = V(sn2all[:, tg, :], f"sn2_{tg}")
        issue_loads(hg, tg)
        if hg == 0:
            rmsnorm_fm(S, lambda k: V(xg[:, k, :], f"xg_{k}"), lambda k: g1[:, k:k + 1],
                       lambda k: V(xn[:, k, :], f"xn_{k}"), 16, 512, 2048.0, tmp, pb[0], ones_bf, eps_col)
            if tg + 1 < ngroups:
                issue_loads(0, tg + 1)
            pick(V(xno[:], "xno"), [V(xn[:, :, q * 128:(q + 1) * 128], xn_all) for q in range(4)])
            for tab, dst2 in ((cosT, cs2), (sinT, sn2)):
                nm = "cosT" if tab is cosT else "sinT"
                pick(dst2[:, 0:128], [V(tab[:, gc0 + q * 128:gc0 + (q + 1) * 128], nm) for q in range(4)])
                S.copy("pool", dst2[:, 128:256], dst2[:, 0:128])
            if npass > 1:
                for k in range(16):
                    S.dma("sp", V(t["xnscr"].ap[k * 128:(k + 1) * 128, cols], f"xnscr_{tg}"), V(xn[:, k, :], f"xn_{k}"))
                S.dma("sp", V(t["xnoscr"].ap[:, tg], f"xnoscr_{tg}"), V(xno[:], "xno"))

        def qk_post_multi(items):
            for ps, gcol, dst, cos_v, sin_v, w, T, nb in items:
                S.act(T["sq"][:, 0:w], ps, AF.Square)
            for ps, gcol, dst, cos_v, sin_v, w, T, nb in items:
                S.mm(nb[:, 0:w], onesblk, T["sq"][:, 0:w])
            for ps, gcol, dst, cos_v, sin_v, w, T, nb in items:
                S.act(T["rt"][:, 0:w], nb[:, 0:w], AF.Sqrt, scale=1.0 / 64, bias=eps_col)
            for ps, gcol, dst, cos_v, sin_v, w, T, nb in items:
                S.recip(T["rb"][:, 0:w], T["rt"][:, 0:w])
            for ps, gcol, dst, cos_v, sin_v, w, T, nb in items:
                S.stt(T["qn"][:, 0:w], ps, gcol, T["rb"][:, 0:w], ALU.mult, ALU.mult)
            for ps, gcol, dst, cos_v, sin_v, w, T, nb in items:
                S.mm(nb[:, 0:w], protT, T["qn"][:, 0:w])
            for ps, gcol, dst, cos_v, sin_v, w, T, nb in items:
                S.tt("dve", T["t2"][:, 0:w], nb[:, 0:w], sin_v, ALU.mult)
                S.tt("pool", T["t1"][:, 0:w], T["qn"][:, 0:w], cos_v, ALU.mult)
            for ps, gcol, dst, cos_v, sin_v, w, T, nb in items:
                S.tt("pool", dst, T["t1"][:, 0:w], T["t2"][:, 0:w], ALU.add)

        TA = {"sq": tmp["sq"][0], "rt": tmp["rt"], "rb": tmp["rb"], "qn": qn_bf, "t1": t1, "t2": t2}
        TB = {"sq": tmp["sq"][1], "rt": rtB, "rb": rbB, "qn": qnB, "t1": t1B, "t2": t2B}
        items = []
        for i, (c0, hd) in enumerate([(C_KA, 0), (C_KB, 1)]):
            ps = pb[1 + (i % 2)]
            for k in range(16):
                S.mm(ps, V(w1s[:, k, c0:c0 + 128], f"w1s_{k}"), V(xn[:, k, :], f"xn_{k}"), start=(k == 0), stop=(k == 15))
            items.append((ps, pp1[:, 2:3], V(KT[:, hd, cols], f"KT_{hd}_{tg}"), V(cosT[:, cols], "cosT"), V(sinT[:, cols], "sinT"), 512,
                          TA if i == 0 else TB, pb[0] if i == 0 else pb[3]))
        qk_post_multi(items)
        ps = pb[1]
        for hd, c0 in enumerate((C_QA, C_QB)):
            for k in range(16):
                S.mm(ps[:, hd * 128:(hd + 1) * 128], V(w1s[:, k, c0:c0 + 128], f"w1s_{k}"), V(xno[:, k, :], "xno"), start=(k == 0), stop=(k == 15))
        qk_post_multi([(ps[:, 0:256], pp1[:, 0:1], V(QTo[:].rearrange("p a b -> p (a b)"), "QTo"), cs2, sn2, 256, TA, pb[0])])
        ps = pb[2]
        for k in range(16):
            S.mm(ps[:, 0:128], V(w1s[:, k, C_GQ:C_GQ + 128], f"w1s_{k}"), V(xno[:, k, :], "xno"), start=(k == 0), stop=(k == 15))
        S.act(gqo, ps[:, 0:128], AF.Copy, scale=128.0 ** -0.5)
        for k in range(16):
            S.mm(ps[0:16, :], V(w1s[:, k, C_LR:C_LR + 16], f"w1s_{k}"), V(xn[:, k, :], f"xn_{k}"), start=(k == 0), stop=(k == 15))
        S.copy("dve", glrT, ps[0:16, :])
        ps = pb[3]
        for k in range(16):
            S.mm(ps[:, 0:256], V(xno[:, k, :], "xno"), V(w1s[:, k, C_TM1 + 256:C_TM1 + 512], f"w1s_{k}"), start=(k == 0), stop=(k == 15))
        S.act(sgr, ps[:, 0:256], AF.Silu)
        for tb in range(4):
            blk = tg * 4 + tb
            tsl = slice(tb * 128, (tb + 1) * 128)
            ps = pb[3]
            for k in range(16):
                S.mm(ps[:, 0:256], V(xn[:, k, tsl], f"xn_{k}"), V(w1s[:, k, C_TM1:C_TM1 + 256], f"w1s_{k}"), start=(k == 0), stop=(k == 15))
            for h2 in range(2):
                S.copy("act", V(Vaug[:, blk, h2, 0:128], f"Vaug_{tg}"), ps[:, h2 * 128:(h2 + 1) * 128])
            ps = pb[7]
            for k in range(16):
                S.mm(ps[:, 0:384], V(xn[:, k, tsl], f"xn_{k}"), V(w1s[:, k, C_TM2:C_TM2 + 384], f"w1s_{k}"), start=(k == 0), stop=(k == 15))
            S.copy("dve", k_sb[tb], ps[:, 0:128])
            S.copy("dve", vg[tb], ps[:, 128:384])
        if hg >= 1 or tg == ngroups - 1:
            issue_loads(hg, tg + 1)
        zb = [pb[7][:, tb * 128:(tb + 1) * 128] for tb in range(4)]
        rvb = [pb[1][:, tb * 128:(tb + 1) * 128] for tb in range(4)]
        for tb in range(4):
            tsl = slice(tb * 128, (tb + 1) * 128)
            S.mm(zb[tb], glrT[0:16, tsl], wa2, start=True, stop=False)
            S.mm(zb[tb], ones_f[0:1, 0:128], ba, start=False, stop=True)
        for tb in range(4):
            S.act(ez[tb], zb[tb], AF.Exp, scale=-1.0)
        for tb in range(4):
            S.act(lsp[tb], ez[tb], AF.Ln, bias=ones_f[:, 0:1])
        for tb in range(4):
            S.mm(rvb[tb], mstrict, lsp[tb])
        for tb in range(4):
            S.mm(pb[2][:, tb * 2:tb * 2 + 2], lsp[tb], chunkind)
        for tb in range(4):
            S.act(wexp[tb], rvb[tb], AF.Exp, scale=-1.0 / 16)
        S.act(dec, pb[2][:, 0:8], AF.Exp, scale=-1.0 / 16)
        for tb in range(4):
            S.tt("dve", kdec[tb], k_sb[tb], wexp[tb], ALU.mult)
        dslot = [V(pb[7].ap[:, 0:256], ["pb7a", "pb7"]), V(pb[3].ap[:, 0:256], ["pb3a", "pb3"]),
                 V(pb[7].ap[:, 256:512], ["pb7b", "pb7"]), V(pb[3].ap[:, 256:512], ["pb3b", "pb3"])]
        opsb = [pb[4][:, 0:256], pb[4][:, 256:512], pb[5][:, 0:256], pb[5][:, 256:512]]
        for c8 in range(8):
            tb, cc = c8 // 2, c8 % 2
            psl = slice(64 * cc, 64 * cc + 64)
            ds_ps = dslot[c8 % 4]
            S.mm(ds_ps, kdec[tb][psl, :], vg[tb][psl, :])
            S.stt(Sst2[(c8 + 1) % 2], Sst2[c8 % 2], dec[:, c8:c8 + 1], ds_ps, ALU.mult, ALU.add)
            S.copy("pool", Sbf[c8], Sst2[(c8 + 1) % 2])
            S.mm(opsb[tb][psl, :], gqo[:, 64 * cc:64 * cc + 64], Sbf[c8])
        pick(osel, opsb)
        S.act(junk, osel, AF.Square, accum=gsm[:, 0:1])
        S.act(gsm[:, 1:2], gsm[:, 0:1], AF.Sqrt, scale=1.0 / 256, bias=eps_col)
        S.recip(gsm[:, 2:3], gsm[:, 1:2])
        S.stt(osel, osel, gsm[:, 2:3], gog_bc, ALU.mult, ALU.mult)
        S.tt("pool", osel, osel, sgr, ALU.mult)
        for r in range(2):
            S.tr(pb[6][:, r * 128:(r + 1) * 128], osel[:, r * 128:(r + 1) * 128], ident)
            S.copy("act", mstage[2 + r][:, 0:128], pb[6][:, r * 128:(r + 1) * 128])
        sbanks = [[pb[4], pb[2]], [pb[3], pb[7]]]
        rounds = []
        for hd in range(2):
            for r0 in range(0, 4 * tg + 4, 4):
                rounds.append((hd, list(range(r0, r0 + 4))))

        def emit_qk(R, sset):
            hd, kbs = R
            for i, kb in enumerate(kbs):
                for c in range(2):
                    csl = slice(64 * c, 64 * c + 64)
                    S.mm(sbanks[sset][c][:, i * 128:(i + 1) * 128], V(KT[csl, hd, kb * 128:(kb + 1) * 128], f"KT_{hd}_{kb // 4}"),
                         V(QTo[csl, hd, :], "QTo"))

        pending = []

        def flush_pending():
            while pending:
                hd, dnb = pending.pop(0)
                S.tr(pb[1][:, 384:512], dnb, ident)
                S.copy("act", mstage[hd][:, 0:128], pb[1][:, 384:512])

        def emit_rest(R, sset, n):
            hd, kbs = R
            last = kbs[-1] == 4 * tg + 3
            pe2 = [pexp[sset * 2], pexp[sset * 2 + 1]]
            for c in range(2):
                S.act(pe2[c], sbanks[sset][c], AF.Exp, scale=0.125)
            if last:
                for c in range(2):
                    S.tt("pool", pe2[c], pe2[c], maskT, ALU.mult)
            for i, kb in enumerate(kbs):
                for c in range(2):
                    S.mm(pb[5 + c][:, 0:129], pe2[c][:, i * 128:(i + 1) * 128], V(Vaug[:, kb, hd, 0:129], f"Vaug_{kb // 4}"),
                         start=(kb == 0), stop=(kb == 4 * tg + 3))
            if last:
                flush_pending()
                dnb = dn[hd]
                S.recip(asm[:, 0:1], pb[5][:, 128:129])
                S.recip(asm[:, 1:2], pb[6][:, 128:129])
                S.tt("dve", asm[:, 2:3], asm[:, 1:2], neg_lam, ALU.mult)
                S.ts("dve", o1, pb[5][:, 0:128], asm[:, 0:1], ALU.mult)
                S.stt(dd, pb[6][:, 0:128], asm[:, 2:3], o1, ALU.mult, ALU.add)
                S.act(junk2, dd, AF.Square, accum=asm[:, 3:4])
                S.act(asm[:, 4:5], asm[:, 3:4], AF.Sqrt, scale=1.0 / 128, bias=eps_col)
                S.recip(asm[:, 5:6], asm[:, 4:5])
                S.stt(dnb, dd, asm[:, 5:6], subg_bc, ALU.mult, ALU.mult)
                pending.append((hd, dnb))

        emit_qk(rounds[0], 0)
        for n, R in enumerate(rounds):
            if n + 1 < len(rounds):
                emit_qk(rounds[n + 1], (n + 1) % 2)
            emit_rest(R, n % 2, n)
        flush_pending()
        ocols = slice(tg * 128, (tg + 1) * 128)
        for r in range(4):
            r0 = row_of(hg, r)
            S.dma("sp", V(t["mixT1"].ap[r0:r0 + 128, ocols], t["mixT1"].keys), mstage[r][:, 0:128])


def build_nc_phase1_only(ngroups=8):
    import contextlib
    nc = bass.Bass("TRN2", target_bir_lowering=False)
    t = {}
    for name, shape, dt in P1_INPUTS:
        t[name] = V(nc.dram_tensor(name, shape, dt, kind="ExternalInput").ap(), "dram_" + name)
    t["mixT1"] = V(nc.dram_tensor("mixT1", [512, 1024], BF16, kind="ExternalOutput").ap(), "dram_mixT1")
    S = Sched(nc)
    with contextlib.ExitStack() as es:
        build_phase1(nc, S, t, es, ngroups=ngroups)
        S.emit()
    return nc


_NC_CACHE = {}


def build_nc_fused(ngroups=8):
    import contextlib
    nc = bass.Bass("TRN2", target_bir_lowering=False)
    t1, t2 = {}, {}
    for name, shape, dt in P1_INPUTS:
        shape = list(shape)
        if name in ("w1", "wa2", "ba"):
            shape[0] = 4
        t1[name] = V(nc.dram_tensor(name, shape, dt, kind="ExternalInput").ap(), "dram_" + name)
    mixscr = V(nc.dram_tensor("mixscr", [2048, 1024], BF16, kind="Internal").ap(), "dram_mixscr")
    t1["mixT1"] = mixscr
    t1["xnscr"] = V(nc.dram_tensor("xnscr", [2048, 4096], BF16, kind="Internal").ap(), "dram_xnscr")
    t1["xnoscr"] = V(nc.dram_tensor("xnoscr", [128, 8, 16, 128], BF16, kind="Internal").ap(), "dram_xnoscr")
    for name, shape, dt in P2_INPUTS:
        if name == "mixT":
            continue
        t2[name] = V(nc.dram_tensor(name, shape, dt, kind="ExternalInput").ap(), "dram_" + name)
    t2["mixT"] = mixscr
    t2["outT"] = V(nc.dram_tensor("outT", [2048, 1024], F32, kind="ExternalOutput").ap(), "dram_outT")
    S = Sched(nc)

    def row_of(hg, r):
        return hg * 256 + r * 128 if r < 2 else 1024 + hg * 256 + (r - 2) * 128

    with contextlib.ExitStack() as es1:
        build_phase1(nc, S, t1, es1, ngroups=ngroups, npass=4, row_of=row_of)
    S.barrier()
    with contextlib.ExitStack() as es2:
        build_phase2(nc, S, t2, es2, mix_select=False)
    S.emit()
    return nc


def host_fused_inputs(inp, c):
    m = host_phase1_inputs(inp, c, groups=[0, 1, 2, 3])
    m.update(host_phase2_inputs(inp, c))
    return m


def kernel(**inp):
    inp = {k: np.asarray(v) for k, v in inp.items()}
    if "fused" not in _NC_CACHE:
        _NC_CACHE["fused"] = build_nc_fused()
    cores = list(range(8))
    res = run_bass_kernel_spmd(_NC_CACHE["fused"], [host_fused_inputs(inp, c) for c in cores], core_ids=cores)
    out = np.empty((2, 4096, 2048), np.float32)
    for c in cores:
        b, j = c // 4, c % 4
        out[b, own_tokens(c), :] = np.asarray(res.results[c]["outT"]).T
    return out
```

```python
import numpy as np
import ml_dtypes
import concourse.bass as bass
import concourse.mybir as mybir
from concourse.bass_utils import run_bass_kernel_spmd

F32 = mybir.dt.float32
BF16 = mybir.dt.bfloat16
I32 = mybir.dt.int32
AF = mybir.ActivationFunctionType
ALU = mybir.AluOpType
AX = mybir.AxisListType

EPS = 1e-6
NDMA_SLOTS = 16


class V:
    def __init__(self, ap, keys):
        self.ap = ap
        self.keys = tuple(keys) if isinstance(keys, (list, tuple)) else (keys,)

    def __getitem__(self, idx):
        return V(self.ap[idx], self.keys)

    def k(self, *keys):
        return V(self.ap, keys)


class Sched:
    ENG = ("pe", "act", "dve", "pool", "sp")

    def __init__(self, nc):
        self.nc = nc
        self.ops = []
        self.dma_rr = {"sp": 0, "pool": 0, "act": 0}

    def add(self, issue, fn, reads, writes, dma=False):
        if dma:
            s = self.dma_rr[issue]
            self.dma_rr[issue] = (s + 1) % NDMA_SLOTS
            stream = f"dma_{issue}_{s}"
        else:
            stream = issue
        rk, wk = [], []
        for v in reads:
            rk.extend(v.keys)
        for v in writes:
            wk.extend(v.keys)
        self.ops.append(dict(stream=stream, issue=issue, fn=fn, reads=rk, writes=wk, dma=dma))

    def barrier(self):
        self.ops.append(dict(barrier=True))

    def mm(self, out, lhsT, rhs, start=True, stop=True, extra_reads=()):
        self.add("pe", lambda e: e.matmul(out.ap, lhsT.ap, rhs.ap, start=start, stop=stop),
                 [lhsT, rhs, *extra_reads], [out])

    def tr(self, out, in_, ident):
        self.add("pe", lambda e: e.transpose(out.ap, in_.ap, ident.ap), [in_, ident], [out])

    def act(self, out, in_, func, scale=1.0, bias=0.0, accum=None):
        reads = [in_]
        if isinstance(scale, V):
            reads.append(scale)
        if isinstance(bias, V):
            reads.append(bias)
        writes = [out] + ([accum] if accum is not None else [])
        sc = scale.ap if isinstance(scale, V) else scale
        bi = bias.ap if isinstance(bias, V) else bias
        if accum is None:
            self.add("act", lambda e: e.activation(out.ap, in_.ap, func, bias=bi, scale=sc), reads, writes)
        else:
            self.add("act", lambda e: e.activation(out.ap, in_.ap, func, bias=bi, scale=sc, accum_out=accum.ap),
                     reads, writes)

    def tt(self, eng, out, a, b, op):
        self.add(eng, lambda e: e.tensor_tensor(out.ap, a.ap, b.ap, op), [a, b], [out])

    def ts(self, eng, out, a, s1, op0, s2=None, op1=None):
        reads = [a] + [s for s in (s1, s2) if isinstance(s, V)]
        x1 = s1.ap if isinstance(s1, V) else s1
        x2 = s2.ap if isinstance(s2, V) else s2
        if op1 is None:
            self.add(eng, lambda e: e.tensor_scalar(out.ap, a.ap, x1, None, op0), reads, [out])
        else:
            self.add(eng, lambda e: e.tensor_scalar(out.ap, a.ap, x1, x2, op0, op1), reads, [out])

    def stt(self, out, a, s, b, op0, op1):
        reads = [a, b] + ([s] if isinstance(s, V) else [])
        x = s.ap if isinstance(s, V) else s
        self.add("dve", lambda e: e.scalar_tensor_tensor(out.ap, a.ap, x, b.ap, op0, op1), reads, [out])

    def copy(self, eng, out, in_):
        if eng == "act":
            self.add("act", lambda e: e.copy(out.ap, in_.ap), [in_], [out])
        else:
            self.add(eng, lambda e: e.tensor_copy(out.ap, in_.ap), [in_], [out])

    def recip(self, out, in_):
        self.add("dve", lambda e: e.reciprocal(out.ap, in_.ap), [in_], [out])

    def memset(self, eng, out, val):
        self.add(eng, lambda e: e.memset(out.ap, val), [], [out])

    def dma(self, issue, out, in_):
        self.add(issue, lambda e: e.dma_start(out=out.ap, in_=in_.ap), [in_], [out], dma=True)

    def emit(self):
        nc = self.nc
        stream_pos = {}
        fence = {}
        ops = []
        for op in self.ops:
            if op.get("barrier"):
                fence = dict(stream_pos)
                continue
            p = stream_pos.get(op["stream"], 0) + 1
            stream_pos[op["stream"]] = p
            op["pos"] = p
            op["fence"] = fence
            ops.append(op)
        n = len(ops)
        last_w = {}
        readers = {}
        seen = {e: {} for e in self.ENG}
        for i, op in enumerate(ops):
            need = {}

            def want(j):
                o = ops[j]
                st = o["stream"]
                if st == "pe" and op["stream"] == "pe":
                    return
                if o["pos"] > need.get(st, 0):
                    need[st] = o["pos"]

            for k in op["reads"]:
                if k in last_w:
                    want(last_w[k])
            for k in op["writes"]:
                if k in last_w:
                    want(last_w[k])
                for r in readers.get(k, ()):
                    if r != i:
                        want(r)
            for st, p in op["fence"].items():
                if st == "pe" and op["stream"] == "pe":
                    continue
                if p > need.get(st, 0):
                    need[st] = p
            if op["dma"] and op["pos"] > 1:
                if op["pos"] - 1 > need.get(op["stream"], 0):
                    need[op["stream"]] = op["pos"] - 1
            sn = seen[op["issue"]]
            waits = []
            for st, p in need.items():
                if sn.get(st, 0) < p:
                    sn[st] = p
                    waits.append((st, p))
            op["waits"] = waits
            for k in op["reads"]:
                readers.setdefault(k, []).append(i)
            for k in op["writes"]:
                last_w[k] = i
                readers[k] = []
        by_stream = {}
        for i, op in enumerate(ops):
            by_stream.setdefault(op["stream"], []).append(i)
        signal = set()
        for op in ops:
            for st, p in op["waits"]:
                signal.add((st, p))
            if op["dma"]:
                signal.add((op["stream"], op["pos"]))
        for st, lst in by_stream.items():
            signal.add((st, ops[lst[-1]]["pos"]))
        rank = {}
        for st, lst in by_stream.items():
            r = 0
            for i in lst:
                if (st, ops[i]["pos"]) in signal:
                    r += 1
                    rank[(st, ops[i]["pos"])] = r
        finals = {st: max(v for (s, _), v in rank.items() if s == st) for st in by_stream}
        import contextlib
        with contextlib.ExitStack() as es:
            sems = {st: es.enter_context(nc.semaphore(f"s_{st}")) for st in by_stream}
            block = es.enter_context(nc.Block())
            handles = {"pe": nc.tensor, "act": nc.scalar, "dve": nc.vector, "pool": nc.gpsimd, "sp": nc.sync}

            def run_engine(ename):
                def body(_e):
                    eng = handles[ename]
                    for op in ops:
                        if op["issue"] != ename:
                            continue
                        for st, p in op["waits"]:
                            mult = 16 if st.startswith("dma_") else 1
                            eng.wait_ge(sems[st], rank[(st, p)] * mult)
                        ins = op["fn"](eng)
                        key = (op["stream"], op["pos"])
                        if key in rank:
                            ins.then_inc(sems[op["stream"]], 16 if op["dma"] else 1)
                    if ename == "sp":
                        for st, f in finals.items():
                            mult = 16 if st.startswith("dma_") else 1
                            eng.wait_ge(sems[st], f * mult)
                return body

            block.tensor(run_engine("pe"))
            block.scalar(run_engine("act"))
            block.vector(run_engine("dve"))
            block.gpsimd(run_engine("pool"))
            block.sync(run_engine("sp"))


def _alloc(es, nc, name, shape, dt):
    return es.enter_context(nc.sbuf_tensor("sb_" + name, list(shape), dt))


def rmsnorm_fm(S, src_fn, gcol_fn, dst_fn, nk, ntok, D, tmp, pbank, ones_bf, eps_col, f32_fn=None):
    for k in range(nk):
        sq = tmp["sq"][k % 2][:, 0:ntok]
        S.act(sq, src_fn(k), AF.Square)
        S.mm(pbank[:, 0:ntok], ones_bf, sq, start=(k == 0), stop=(k == nk - 1))
    rt = tmp["rt"][:, 0:ntok]
    S.act(rt, pbank[:, 0:ntok], AF.Sqrt, scale=1.0 / D, bias=eps_col)
    rb = tmp["rb"][:, 0:ntok]
    S.recip(rb, rt)
    for k in range(nk):
        if f32_fn is None:
            S.stt(dst_fn(k), src_fn(k), gcol_fn(k), rb, ALU.mult, ALU.mult)
        else:
            f32_fn(k, rb)


def build_phase2(nc, S, t, es_outer, mix_select=False):
    import contextlib
    es = es_outer
    NT = 1024
    hT = _alloc(es, nc, "hT", [128, 16, NT], F32)
    ones_bf = V(_alloc(es, nc, "ones_bf", [128, 128], BF16)[:], "ones_bf")
    ones_f = V(_alloc(es, nc, "ones_f", [128, 128], F32)[:], "ones_f")
    ident = V(_alloc(es, nc, "ident", [128, 128], F32)[:], "ident")
    eps_col = V(_alloc(es, nc, "eps_col", [128, 1], F32)[:], "eps_col")
    gvec = V(_alloc(es, nc, "gvec", [128, 48], F32)[:], "gvec")
    cg = V(_alloc(es, nc, "cg", [128, 8], F32)[:], "cg")
    tmp = {
        "sq": [V(_alloc(es, nc, f"sq{i}", [128, 512], BF16)[:], f"sq{i}") for i in range(2)],
        "rt": V(_alloc(es, nc, "rt", [128, 512], F32)[:], "rt"),
        "rb": V(_alloc(es, nc, "rb", [128, 512], F32)[:], "rb"),
    }
    pb = [V(es.enter_context(nc.psum_tensor(f"pb{i}", [128, 512], F32))[:], f"pb{i}") for i in range(8)]
    wpool = []

    def hv(k, half):
        return V(hT[:, k, half * 512:(half + 1) * 512], f"hT_{k}_{half}")

    S.memset("dve", ones_bf, 1.0)
    S.memset("dve", ones_f, 1.0)
    S.memset("dve", eps_col, EPS)
    S.dma("sp", ident, t["ident"])
    S.dma("sp", gvec, t["gvec"])
    S.dma("sp", cg, t["cg"])

    def wview(i, a, b):
        return V(wpool[i][:].rearrange("p (a b) -> p a b", a=a), f"wp{i}")

    state = {"wi": 0, "pbi": 0}

    def next_w():
        i = state["wi"]
        state["wi"] = (i + 1) % 2
        return i

    def next_pb(lo=4, n=4):
        i = state["pbi"]
        state["pbi"] = (i + 1) % n
        return pb[lo + i]

    def linear_fm(wdram, col0, ncols, in_fn, nk, ntoks, evac):
        for g in range(ncols // 512):
            wi = next_w()
            wv = wview(wi, nk, 512)
            S.dma("pool", wv, V(wdram.ap[:, col0 + g * 512: col0 + (g + 1) * 512].rearrange("(k p) n -> p k n", p=128), wdram.keys))
            for ocl in range(4):
                oc = g * 4 + ocl
                for ti, nt in enumerate(ntoks):
                    ps = next_pb()
                    for k in range(nk):
                        S.mm(ps[:, 0:nt], wv[:, k, ocl * 128:(ocl + 1) * 128], in_fn(k, ti), start=(k == 0), stop=(k == nk - 1))
                    evac(oc, ti, ps[:, 0:nt])

    with contextlib.ExitStack() as esA:
        wpool[:] = [_alloc(esA, nc, f"wpA{i}", [128, 8192], BF16) for i in range(2)]
        mixT = _alloc(esA, nc, "mixT", [128, 16, NT], BF16)
        if mix_select:
            stage = [V(_alloc(esA, nc, f"mstg{q}", [128, 1024], BF16)[:], f"mstg{q}") for q in range(4)]
            sel = V(_alloc(esA, nc, "sel", [128, 4], F32)[:], "sel")
            S.dma("sp", sel, t["sel"])
        for k in range(16):
            if mix_select:
                mk = V(mixT[:, k, :], f"mixT_{k}")
                for q in range(4):
                    S.dma("sp", stage[q], V(t["mixT"].ap[k * 128:(k + 1) * 128, q * 1024:(q + 1) * 1024], t["mixT"].keys))
                S.ts("dve", mk, stage[0], sel[:, 0:1], ALU.mult)
                for q in range(1, 4):
                    S.stt(mk, stage[q], sel[:, q:q + 1], mk, ALU.mult, ALU.add)
            else:
                S.dma("sp", V(mixT[:, k, :], f"mixT_{k}"), V(t["mixT"].ap[k * 128:(k + 1) * 128, :], t["mixT"].keys))
            for half in range(2):
                S.dma("sp", hv(k, half), V(t["xT2"].ap[k * 128:(k + 1) * 128, half * 512:(half + 1) * 512], t["xT2"].keys))

        def evacA(oc, ti, ps):
            S.tt("dve", hv(oc, ti), ps, hv(oc, ti), ALU.add)

        linear_fm(t["w_out"], 0, 2048, lambda k, ti: V(mixT[:, k, ti * 512:(ti + 1) * 512], f"mixT_{k}"), 16, [512, 512], evacA)
    S.barrier()

    with contextlib.ExitStack() as esB:
        wpool[:] = [_alloc(esB, nc, f"wpB{i}", [128, 8192], BF16) for i in range(2)]
        kT = _alloc(esB, nc, "kT", [128, 16, 256], BF16)
        v_sb = _alloc(esB, nc, "v_sb", [128, 2, 2048], BF16)
        with contextlib.ExitStack() as esB1:
            memT = _alloc(esB1, nc, "memT", [128, 16, 256], F32)
            memnT = _alloc(esB1, nc, "memnT", [128, 16, 256], BF16)
            kraw = _alloc(esB1, nc, "kraw", [128, 16, 256], F32)
            for k in range(16):
                S.dma("sp", V(memT[:, k, :], f"memT_{k}"), V(t["memT"].ap[k * 128:(k + 1) * 128, :], t["memT"].keys))
            rmsnorm_fm(S, lambda k: V(memT[:, k, :], f"memT_{k}"), lambda k: gvec[:, 16 + k:17 + k],
                       lambda k: V(memnT[:, k, :], f"memnT_{k}"), 16, 256, 2048.0, tmp, pb[0], ones_bf, eps_col)

            def evacK(oc, ti, ps):
                S.copy("act", V(kraw[:, oc, :], f"kraw_{oc}"), ps)

            linear_fm(t["w_ckv"], 0, 2048, lambda k, ti: V(memnT[:, k, :], f"memnT_{k}"), 16, [256], evacK)
            for h in range(4):
                rmsnorm_fm(S, lambda dc: V(kraw[:, h * 4 + dc, :], f"kraw_{h * 4 + dc}"), lambda dc: cg[:, 4 + dc:5 + dc],
                           lambda dc: V(kT[:, h * 4 + dc, :], f"kT_{h * 4 + dc}"), 4, 256, 512.0, tmp, pb[0], ones_bf, eps_col)
            for g in range(4):
                wi = next_w()
                wv = wview(wi, 16, 512)
                S.dma("pool", wv, V(t["w_ckv"].ap[:, 2048 + g * 512: 2048 + (g + 1) * 512].rearrange("(k p) n -> p k n", p=128), t["w_ckv"].keys))
                for mc in range(2):
                    ps = next_pb()
                    for k in range(16):
                        S.mm(ps, V(memnT[:, k, mc * 128:(mc + 1) * 128], f"memnT_{k}"), wv[:, k, :], start=(k == 0), stop=(k == 15))
                    S.copy("act", V(v_sb[:, mc, g * 512:(g + 1) * 512], f"v_sb_{mc}_{g}"), ps)
        S.barrier()
        xnh = _alloc(esB, nc, "xnh", [128, 16, 512], BF16)
        oTh = _alloc(esB, nc, "oTh", [128, 16, 512], BF16)
        qraw = _alloc(esB, nc, "qraw", [128, 4, 512], F32)
        qT = _alloc(esB, nc, "qT", [128, 4, 512], BF16)
        pT = _alloc(esB, nc, "pT", [128, 2, 512], BF16)
        rden = V(_alloc(esB, nc, "rden", [128, 512], F32)[:], "rden")
        for half in range(2):
            rmsnorm_fm(S, lambda k: hv(k, half), lambda k: gvec[:, k:k + 1],
                       lambda k: V(xnh[:, k, :], f"xnh_{k}"), 16, 512, 2048.0, tmp, pb[0], ones_bf, eps_col)
            for h in range(4):
                wi = next_w()
                wv = wview(wi, 16, 512)
                S.dma("pool", wv, V(t["w_cq"].ap[:, h * 512:(h + 1) * 512].rearrange("(k p) n -> p k n", p=128), t["w_cq"].keys))
                for dc in range(4):
                    ps = next_pb()
                    for k in range(16):
                        S.mm(ps, wv[:, k, dc * 128:(dc + 1) * 128], V(xnh[:, k, :], f"xnh_{k}"), start=(k == 0), stop=(k == 15))
                    S.copy("act", V(qraw[:, dc, :], f"qraw_{dc}"), ps)
                rmsnorm_fm(S, lambda dc: V(qraw[:, dc, :], f"qraw_{dc}"), lambda dc: cg[:, dc:dc + 1],
                           lambda dc: V(qT[:, dc, :], f"qT_{dc}"), 4, 512, 512.0, tmp, pb[0], ones_bf, eps_col)
                for mc in range(2):
                    ps = next_pb()
                    for dc in range(4):
                        S.mm(ps, V(kT[:, h * 4 + dc, mc * 128:(mc + 1) * 128], f"kT_{h * 4 + dc}"), V(qT[:, dc, :], f"qT_{dc}"),
                             start=(dc == 0), stop=(dc == 3))
                    S.act(V(pT[:, mc, :], f"pT_{mc}"), ps, AF.Exp, scale=512.0 ** -0.5)
                ps = pb[1]
                for mc in range(2):
                    S.mm(ps, ones_bf, V(pT[:, mc, :], f"pT_{mc}"), start=(mc == 0), stop=(mc == 1))
                S.recip(rden, ps)
                for dvc in range(4):
                    ps = next_pb()
                    for mc in range(2):
                        S.mm(ps, V(v_sb[:, mc, h * 512 + dvc * 128: h * 512 + (dvc + 1) * 128], f"v_sb_{mc}_{h}"), V(pT[:, mc, :], f"pT_{mc}"),
                             start=(mc == 0), stop=(mc == 1))
                    S.tt("dve", V(oTh[:, h * 4 + dvc, :], f"oTh_{h * 4 + dvc}"), ps, rden, ALU.mult)

            def evacO(oc, ti, ps):
                S.tt("dve", hv(oc, half), ps, hv(oc, half), ALU.add)

            linear_fm(t["w_co"], 0, 2048, lambda k, ti: V(oTh[:, k, :], f"oTh_{k}"), 16, [512], evacO)
    S.barrier()

    with contextlib.ExitStack() as esC:
        wpool[:] = [_alloc(esC, nc, f"wpC{i}", [128, 8192], BF16) for i in range(5)]
        xnT = _alloc(esC, nc, "xnT", [128, 16, NT], BF16)
        hid = _alloc(esC, nc, "hid", [128, 4, NT], BF16)
        xf = [V(_alloc(esC, nc, f"xf{i}", [128, 512], F32)[:], f"xf{i}") for i in range(2)]
        w_r = V(_alloc(esC, nc, "w_r", [128, 16, 36], F32)[:], "w_r")
        b_r = V(_alloc(esC, nc, "b_r", [1, 36], F32)[:], "b_r")
        GT = _alloc(esC, nc, "GT", [32, NT], F32)
        L = V(_alloc(esC, nc, "L", [128, 36], F32)[:], "L")
        sm = V(_alloc(esC, nc, "sm", [128, 16], F32)[:], "sm")
        gm = V(_alloc(esC, nc, "gm", [128, 4], F32)[:], "gm")
        pen = V(_alloc(esC, nc, "pen", [128, 4], F32)[:], "pen")
        ge = V(_alloc(esC, nc, "ge", [128, 4], F32)[:], "ge")
        elm = V(_alloc(esC, nc, "elm", [128, 32], F32)[:], "elm")
        elm2 = V(_alloc(esC, nc, "elm2", [128, 32], F32)[:], "elm2")
        mk1 = V(_alloc(esC, nc, "mk1", [128, 32], F32)[:], "mk1")
        mk2 = V(_alloc(esC, nc, "mk2", [128, 32], F32)[:], "mk2")
        G = V(_alloc(esC, nc, "G", [128, 32], F32)[:], "G")
        sg_t = xf
        gbc = [tmp["rt"], tmp["rb"]]
        S.dma("sp", w_r, V(t["w_r"].ap.rearrange("(k p) n -> p k n", p=128), t["w_r"].keys))
        S.dma("sp", b_r, t["b_r"])
        BIG = 1.0e30
        for half in range(2):
            def f32_fn(k, rb, half=half):
                x = xf[k % 2]
                S.stt(x, hv(k, half), gvec[:, 32 + k:33 + k], rb, ALU.mult, ALU.mult)
                for tb in range(4):
                    S.mm(pb[4 + tb][:, 0:36], x[:, tb * 128:(tb + 1) * 128], w_r[:, k, :], start=(k == 0), stop=False)
                S.copy("act", V(xnT[:, k, half * 512:(half + 1) * 512], f"xnT_{k}_{half}"), x)

            rmsnorm_fm(S, lambda k: hv(k, half), None, None, 16, 512, 2048.0, tmp, pb[0], ones_bf, eps_col, f32_fn=f32_fn)
            for tb in range(4):
                S.mm(pb[4 + tb][:, 0:36], ones_f[0:1, 0:128], b_r, start=False, stop=True)
                S.copy("dve", L, pb[4 + tb][:, 0:36])
                gmax, gsum, grw, m1, m2, d12, sgm, g1, g2, ngmax = [sm[:, i:i + 1] for i in range(10)]
                S.add("dve", lambda e, o=gmax, i=L: e.reduce_max(o.ap, i.ap[:, 0:4], AX.X), [L], [gmax])
                S.ts("dve", gm, L[:, 0:4], gmax, ALU.is_equal)
                S.ts("dve", ngmax, gmax, -1.0, ALU.mult)
                S.act(ge, L[:, 0:4], AF.Exp, bias=ngmax, accum=gsum)
                S.recip(grw, gsum)
                S.ts("dve", pen, gm, BIG, ALU.mult, -BIG, ALU.add)
                for g in range(4):
                    S.ts("dve", elm[:, g * 8:(g + 1) * 8], L[:, 4 + g * 8: 4 + (g + 1) * 8], pen[:, g:g + 1], ALU.add)
                S.add("dve", lambda e, o=m1, i=elm: e.reduce_max(o.ap, i.ap, AX.X), [elm], [m1])
                S.ts("dve", mk1, elm, m1, ALU.is_equal)
                S.stt(elm2, mk1, -BIG, elm, ALU.mult, ALU.add)
                S.add("dve", lambda e, o=m2, i=elm2: e.reduce_max(o.ap, i.ap, AX.X), [elm2], [m2])
                S.ts("dve", mk2, elm2, m2, ALU.is_equal)
                S.tt("dve", d12, m1, m2, ALU.subtract)
                S.act(sgm, d12, AF.Sigmoid)
                S.tt("dve", g1, sgm, grw, ALU.mult)
                S.tt("dve", g2, grw, g1, ALU.subtract)
                S.ts("dve", G, mk1, g1, ALU.mult)
                S.stt(G, mk2, g2, G, ALU.mult, ALU.add)
                S.tr(pb[3][0:32, 0:128], G, ident)
                col = half * 512 + tb * 128
                S.copy("dve", V(GT[:, col:col + 128], f"GT_{half}"), pb[3][0:32, 0:128])
        for e in range(32):
            wg = V(wpool[e % 2][:].rearrange("p (a b) -> p a b", a=16), f"wp{e % 2}")
            wu = V(wpool[2 + e % 2][:].rearrange("p (a b) -> p a b", a=16), f"wp{2 + e % 2}")
            wd = V(wpool[4][:].rearrange("p (a b) -> p a b", a=4), "wp4")
            S.dma("pool", wg, V(t["w_gate"].ap[e].rearrange("(k p) n -> p k n", p=128), t["w_gate"].keys))
            S.dma("pool", wu, V(t["w_up"].ap[e].rearrange("(k p) n -> p k n", p=128), t["w_up"].keys))
            S.dma("pool", wd, V(t["w_down"].ap[e].rearrange("(k p) n -> p k n", p=128), t["w_down"].keys))
            for half in range(2):
                gb = gbc[half]
                S.mm(pb[1], V(ident.ap[0:32, e:e + 1].broadcast_to([32, 128]), ident.keys), V(GT[:, half * 512:(half + 1) * 512], f"GT_{half}"))
                S.copy("act", gb, pb[1])
                for fc in range(4):
                    pg, pu = pb[2], pb[3]
                    for k in range(16):
                        S.mm(pg, wg[:, k, fc * 128:(fc + 1) * 128], V(xnT[:, k, half * 512:(half + 1) * 512], f"xnT_{k}_{half}"),
                             start=(k == 0), stop=(k == 15))
                    for k in range(16):
                        S.mm(pu, wu[:, k, fc * 128:(fc + 1) * 128], V(xnT[:, k, half * 512:(half + 1) * 512], f"xnT_{k}_{half}"),
                             start=(k == 0), stop=(k == 15))
                    sg = sg_t[fc % 2]
                    S.act(sg, pg, AF.Silu)
                    S.tt("pool", sg, sg, gb, ALU.mult)
                    S.tt("dve", V(hid[:, fc, half * 512:(half + 1) * 512], f"hid_{fc}_{half}"), pu, sg, ALU.mult)
                for oc in range(16):
                    ps = next_pb()
                    for fc in range(4):
                        S.mm(ps, wd[:, fc, oc * 128:(oc + 1) * 128], V(hid[:, fc, half * 512:(half + 1) * 512], f"hid_{fc}_{half}"),
                             start=(fc == 0), stop=(fc == 3))
                    S.tt("dve", hv(oc, half), ps, hv(oc, half), ALU.add)
        for k in range(16):
            for half in range(2):
                S.dma("sp", V(t["outT"].ap[k * 128:(k + 1) * 128, half * 512:(half + 1) * 512], t["outT"].keys), hv(k, half))


P2_INPUTS = [
    ("mixT", [2048, 1024], BF16), ("xT2", [2048, 1024], F32), ("memT", [2048, 256], F32),
    ("w_out", [2048, 2048], F32), ("w_cq", [2048, 2048], F32), ("w_co", [2048, 2048], F32),
    ("w_ckv", [2048, 4096], F32), ("gvec", [128, 48], F32), ("cg", [128, 8], F32), ("ident", [128, 128], F32),
    ("w_r", [2048, 36], F32), ("b_r", [1, 36], F32),
    ("w_gate", [32, 2048, 512], F32), ("w_up", [32, 2048, 512], F32), ("w_down", [32, 512, 2048], F32),
]


def host_phase2_inputs(inp, c):
    b, j = c // 4, c % 4
    tok = own_tokens(c)

    def pcol(v):
        return np.ascontiguousarray(v.reshape(-1, 128).T)

    gvec = np.concatenate([pcol(inp["g_cross"][0]), pcol(inp["g_mem"][0]), pcol(inp["g_ffn"][0])], axis=1)
    cg = np.concatenate([pcol(inp["cq_norm_g"][0]), pcol(inp["ck_norm_g"][0])], axis=1)
    return {
        "xT2": np.ascontiguousarray(inp["x"][b, tok, :].T),
        "memT": np.ascontiguousarray(inp["mem"][b].T),
        "w_out": inp["w_out"][0], "w_cq": inp["w_cq"][0], "w_co": inp["w_co"][0], "w_ckv": inp["w_ckv"][0],
        "gvec": np.ascontiguousarray(gvec, dtype=np.float32), "cg": np.ascontiguousarray(cg, dtype=np.float32),
        "ident": np.eye(128, dtype=np.float32),
        "w_r": np.ascontiguousarray(np.concatenate([inp["w_router_grp"][0], inp["w_router_exp"][0]], axis=1)),
        "b_r": np.ascontiguousarray(np.concatenate([inp["b_router_grp"][0], inp["b_router_exp"][0]])[None, :]),
        "w_gate": inp["w_gate"][0], "w_up": inp["w_up"][0], "w_down": inp["w_down"][0],
    }


def build_nc_phase2_only():
    import contextlib
    nc = bass.Bass("TRN2", target_bir_lowering=False)
    t = {}
    for name, shape, dt in P2_INPUTS:
        t[name] = V(nc.dram_tensor(name, shape, dt, kind="ExternalInput").ap(), "dram_" + name)
    t["outT"] = V(nc.dram_tensor("outT", [2048, 1024], F32, kind="ExternalOutput").ap(), "dram_outT")
    S = Sched(nc)
    with contextlib.ExitStack() as es:
        build_phase2(nc, S, t, es)
        S.emit()
    return nc


P1_INPUTS = [
    ("xT1", [2048, 4096], F32), ("w1", [1, 2048, 1552], F32), ("pp1", [128, 8], F32), ("pos", [1, 4096], I32),
    ("g1", [128, 16], F32), ("lam4", [1, 256], F32), ("subg", [1, 128], F32), ("gog", [1, 256], F32),
    ("wa2", [1, 16, 128], F32), ("ba", [1, 1, 128], F32), ("ident1", [128, 128], F32), ("mstrict", [128, 128], F32),
    ("chunkind", [128, 2], F32), ("protT", [128, 128], F32), ("sel4", [128, 4], F32), ("maskT", [128, 512], F32),
]
_DBG_STOP = [99]
C_QA, C_QB, C_KA, C_KB, C_GQ, C_LR, C_TM1, C_TM2, NW1 = 0, 128, 256, 384, 512, 640, 656, 1168, 1552


def host_w1(inp, j):
    w = inp["w_in"][0]
    hA, hB = 2 * j, 2 * j + 1
    cols = []
    cols += [w[:, hA * 128:(hA + 1) * 128], w[:, hB * 128:(hB + 1) * 128]]
    cols += [w[:, 1024 + hA * 128:1024 + (hA + 1) * 128], w[:, 1024 + hB * 128:1024 + (hB + 1) * 128]]
    cols += [w[:, 3072 + j * 128:3072 + (j + 1) * 128]]
    cols += [w[:, 5120:5136]]
    cols += [w[:, 2048 + hA * 128:2048 + (hA + 1) * 128], w[:, 2048 + hB * 128:2048 + (hB + 1) * 128]]
    cols += [w[:, 5136 + j * 256:5136 + (j + 1) * 256]]
    cols += [w[:, 3584 + j * 128:3584 + (j + 1) * 128]]
    cols += [w[:, 4096 + j * 256:4096 + (j + 1) * 256]]
    w1 = np.ascontiguousarray(np.concatenate(cols, axis=1))
    assert w1.shape == (2048, NW1)
    return w1


def host_phase1_inputs(inp, c, groups=None):
    b, j = c // 4, c % 4
    groups = [j] if groups is None else groups
    w1 = np.stack([host_w1(inp, g) for g in groups], axis=0)
    pp1 = np.zeros((128, 8), np.float32)
    pp1[:, 0] = np.tile(inp["q_norm_g"][0], 2)
    pp1[:, 2] = np.tile(inp["k_norm_g"][0], 2)
    half = 32
    freq = (np.float32(10000.0) ** (-np.arange(half, dtype=np.float32) / np.float32(half))).astype(np.float32)
    pp1[:, 4] = np.tile(freq, 4)
    lam4 = np.concatenate([inp["lambda_q1"][0], inp["lambda_k1"][0], inp["lambda_q2"][0], inp["lambda_k2"][0]])[None, :]
    l = np.arange(128)
    mstrict = ((l[:, None] > l[None, :]) & ((l[:, None] // 64) == (l[None, :] // 64))).astype(np.float32)
    chunkind = np.stack([(l < 64), (l >= 64)], axis=1).astype(np.float32)
    protT = np.zeros((128, 128), np.float32)
    for m in range(128):
        if (m % 64) < 32:
            protT[m + 32, m] = -1.0
        else:
            protT[m - 32, m] = 1.0
    return {
        "xT1": np.ascontiguousarray(inp["x"][b].T), "w1": w1, "pp1": pp1,
        "pos": np.ascontiguousarray(inp["positions"][b][None, :].astype(np.int32)),
        "g1": np.ascontiguousarray(inp["g_attn"][0].reshape(16, 128).T),
        "lam4": np.ascontiguousarray(lam4.astype(np.float32)),
        "subg": np.ascontiguousarray(inp["diff_subln_g"][0][None, :]),
        "gog": np.ascontiguousarray(inp["gla_out_g"][0][None, :]),
        "wa2": np.ascontiguousarray(np.stack([inp["gla_w_a2"][0][:, g * 128:(g + 1) * 128] for g in groups], axis=0)),
        "ba": np.ascontiguousarray(np.stack([inp["gla_b_a"][0][None, g * 128:(g + 1) * 128] for g in groups], axis=0)),
        "ident1": np.eye(128, dtype=np.float32), "mstrict": mstrict, "chunkind": chunkind, "protT": protT,
        "sel4": own_sel(c), "maskT": own_mask(c),
    }


def own_sel(c):
    sel = np.zeros((128, 4), np.float32)
    sel[:, c % 4] = 1.0
    return sel


def own_mask(c):
    j = c % 4
    M = np.zeros((128, 4, 128), np.float32)
    for m in range(4):
        if m < j:
            M[:, m, :] = 1.0
        elif m == j:
            M[:, m, :] = 1.0
            M[64:128, m, 0:64] = 0.0
    return np.ascontiguousarray(M.reshape(128, 512))


def own_tokens(c):
    j = c % 4
    return np.concatenate([np.arange((4 * i + j) * 128, (4 * i + j + 1) * 128) for i in range(8)])


def build_phase1(nc, S, t, es, ngroups=8, npass=1, row_of=None):
    import math
    NTOK = 4096
    LAM_INIT = 0.8 - 0.6 * math.exp(-0.3 * 0)
    PI = math.pi

    def A(name, shape, dt):
        return _alloc(es, nc, "p1" + name, shape, dt)

    w1s = A("w1s", [128, 16, NW1], BF16)
    KT = A("KT", [128, 2, NTOK], BF16)
    QTo = A("QTo", [128, 2, 128], BF16)
    Vaug = A("Vaug", [128, 32, 2, 130], BF16)
    cosT = A("cosT", [128, NTOK], BF16)
    sinT = A("sinT", [128, NTOK], BF16)
    gqo = V(A("gqo", [128, 128], BF16)[:], "gqo")
    xno = A("xno", [128, 16, 128], BF16)
    cs2all = A("cs2all", [128, 8, 256], BF16)
    sn2all = A("sn2all", [128, 8, 256], BF16)
    maskT = V(A("maskT", [128, 512], BF16)[:], "maskT")
    sel4 = V(A("sel4", [128, 4], F32)[:], "sel4")
    osel = V(A("osel", [128, 256], F32)[:], "osel")
    xg = A("xg", [128, 16, 512], F32)
    xn = A("xn", [128, 16, 512], BF16)
    ones_bf = V(A("ones_bf", [128, 128], BF16)[:], "ones_bf")
    onesblk = V(A("onesblk", [128, 128], BF16)[:], "onesblk")
    protT = V(A("protT", [128, 128], BF16)[:], "protT")
    ident = V(A("ident", [128, 128], F32)[:], "ident")
    mstrict = V(A("mstrict", [128, 128], F32)[:], "mstrict")
    chunkind = V(A("chunkind", [128, 2], F32)[:], "chunkind")
    ones_f = V(A("ones_f", [128, 128], F32)[:], "ones_f")
    eps_col = V(A("eps_col", [128, 1], F32)[:], "eps_col")
    pp1 = V(A("pp1", [128, 8], F32)[:], "pp1")
    g1 = V(A("g1", [128, 16], F32)[:], "g1")
    lamt = V(A("lamt", [128, 256], F32)[:], "lamt")
    lamv = V(A("lamv", [128, 8], F32)[:], "lamv")
    subg_bc = V(A("subg_bc", [128, 128], F32)[:], "subg_bc")
    gog_bc = V(A("gog_bc", [128, 256], F32)[:], "gog_bc")
    wa2 = V(A("wa2", [16, 128], F32)[:], "wa2")
    ba = V(A("ba", [1, 128], F32)[:], "ba")
    tmp = {
        "sq": [V(A(f"sq{i}", [128, 512], BF16)[:], f"sq{i}") for i in range(2)],
        "rt": V(A("rt", [128, 512], F32)[:], "rt"),
        "rb": V(A("rb", [128, 512], F32)[:], "rb"),
    }
    qn_bf = V(A("qn_bf", [128, 512], BF16)[:], "qn_bf")
    t1 = V(A("t1", [128, 512], F32)[:], "t1")
    t2 = V(A("t2", [128, 512], F32)[:], "t2")
    rtB = V(A("rtB", [128, 512], F32)[:], "rtB")
    rbB = V(A("rbB", [128, 512], F32)[:], "rbB")
    qnB = V(A("qnB", [128, 512], BF16)[:], "qnB")
    t1B = V(A("t1B", [128, 512], F32)[:], "t1B")
    t2B = V(A("t2B", [128, 512], F32)[:], "t2B")
    pexp = [V(A(f"pexp{i}", [128, 512], BF16)[:], f"pexp{i}") for i in range(2)]
    glrT = V(A("glrT", [16, 512], F32)[:], "glrT")
    pexp = pexp + [V(A(f"pexp{i}", [128, 512], BF16)[:], f"pexp{i}") for i in (2, 3)]
    ez = [V(t1.ap[:, i * 128:(i + 1) * 128], "t1") for i in range(4)]
    wexp = ez
    lsp = [V(t2.ap[:, i * 128:(i + 1) * 128], "t2") for i in range(4)]
    k_sb = [V(A(f"k_sb{i}", [128, 128], F32)[:], f"k_sb{i}") for i in range(2)] + \
           [V(rtB.ap[:, i * 128:(i + 1) * 128], "rtB") for i in range(2)]
    kdec = [V(tmp["sq"][0].ap[:, i * 128:(i + 1) * 128], "sq0") for i in range(4)]
    vg = [V(A(f"vg{i}", [128, 256], BF16)[:], f"vg{i}") for i in range(2)] + \
         [V(qnB.ap[:, 0:256], "qnB"), V(qnB.ap[:, 256:512], "qnB")]
    go = [V(tmp["rb"].ap[:, 0:256], "rb"), V(tmp["rb"].ap[:, 256:512], "rb")]
    sgr = V(A("sgr", [128, 256], F32)[:], "sgr")
    Sst2 = [V(A(f"Sst{i}", [128, 256], F32)[:], f"Sst{i}") for i in range(2)]
    Sst = Sst2[0]
    Sbf = [V(A(f"Sbf{i}", [128, 256], BF16)[:], f"Sbf{i}") for i in range(8)]
    dec = V(A("dec", [128, 8], F32)[:], "dec")
    junk = V(t2.ap[:, 0:256], "t2")
    junk2 = V(t2.ap[:, 256:384], "t2")
    gsm = V(A("gsm", [128, 16], F32)[:], "gsm")
    o1 = V(A("o1", [128, 128], F32)[:], "o1")
    dd = V(A("dd", [128, 128], F32)[:], "dd")
    dn = [V(A(f"dn{i}", [128, 128], F32)[:], f"dn{i}") for i in range(2)]
    asm = V(A("asm", [128, 8], F32)[:], "asm")
    mstage = [V(A(f"mstage{r}", [128, 512], BF16)[:], f"mstage{r}") for r in range(4)]
    pb = [V(es.enter_context(nc.psum_tensor(f"p1pb{i}", [128, 512], F32))[:], f"pb{i}") for i in range(8)]

    if row_of is None:
        row_of = lambda hg, r: r * 128
    S.dma("pool", protT, t["protT"])
    S.dma("pool", maskT, t["maskT"])
    for dst, src in [(ident, "ident1"), (mstrict, "mstrict"), (chunkind, "chunkind"), (pp1, "pp1"), (g1, "g1"), (sel4, "sel4")]:
        S.dma("sp", dst, t[src])
    S.dma("sp", lamt, V(t["lam4"].ap.partition_broadcast(128), t["lam4"].keys))
    S.dma("sp", subg_bc, V(t["subg"].ap.partition_broadcast(128), t["subg"].keys))
    S.dma("sp", gog_bc, V(t["gog"].ap.partition_broadcast(128), t["gog"].keys))
    S.memset("dve", ones_bf, 1.0)
    S.memset("dve", ones_f, 1.0)
    S.memset("dve", eps_col, EPS)
    S.memset("dve", onesblk, 0.0)
    S.memset("dve", onesblk[0:64, 0:64], 1.0)
    S.memset("dve", onesblk[64:128, 64:128], 1.0)
    S.memset("pool", V(Vaug[:], [f"Vaug_{g}" for g in range(8)]), 1.0)
    for i in range(2):
        S.tt("dve", junk2[:, 0:64], lamt[:, i * 128:i * 128 + 64], lamt[:, i * 128 + 64:i * 128 + 128], ALU.mult)
        S.add("dve", lambda e, o=lamv[:, i:i + 1], x=junk2: e.reduce_sum(o.ap, x.ap[:, 0:64], AX.X), [junk2], [lamv])
        S.act(lamv[:, 2 + i:3 + i], lamv[:, i:i + 1], AF.Exp)
    S.tt("dve", lamv[:, 4:5], lamv[:, 2:3], lamv[:, 3:4], ALU.subtract)
    S.ts("dve", lamv[:, 5:6], lamv[:, 4:5], LAM_INIT, ALU.add, -1.0, ALU.mult)
    neg_lam = lamv[:, 5:6]
    S.ts("dve", subg_bc, subg_bc, 1.0 - LAM_INIT, ALU.mult)
    xgf = xg[:].rearrange("p a b -> p (a b)")
    posi = V(xgf[:, 0:4096].bitcast(I32), "xg_tab0")
    angf = V(xgf[:, 4096:8192], "xg_tab1")
    kf = V(xgf[:, 0:4096], "xg_tab0")
    ki = V(xgf[:, 0:4096].bitcast(I32), "xg_tab0")
    S.dma("sp", posi, V(t["pos"].ap.partition_broadcast(128), t["pos"].keys))
    S.copy("dve", angf, posi)
    S.ts("dve", angf, angf, pp1[:, 4:5], ALU.mult)
    S.ts("dve", t1.k("xg_tab0")[:, 0:1], angf[:, 0:1], 1.0, ALU.mult)
    for c0 in range(0, 4096, 2048):
        sl = slice(c0, c0 + 2048)
        S.ts("dve", kf[:, sl], angf[:, sl], 1.0 / (2 * PI), ALU.mult)
        S.copy("dve", ki[:, sl], kf[:, sl])
        S.copy("dve", kf[:, sl], ki[:, sl])
        C1 = 6.28125
        C2 = 2 * PI - C1
        S.stt(angf[:, sl], kf[:, sl], -C1, angf[:, sl], ALU.mult, ALU.add)
        S.stt(angf[:, sl], kf[:, sl], -C2, angf[:, sl], ALU.mult, ALU.add)
        msk = V(xn[:].rearrange("p a b -> p (a b)").bitcast(F32)[:, 0:2048], "xn_tab")
        PIS = 3.1415925

        def wrap(dst, src, shift):
            S.ts("dve", dst, src, shift, ALU.add)
            S.ts("dve", msk, dst, PI, ALU.is_gt)
            S.stt(dst, msk, -2 * PI, dst, ALU.mult, ALU.add)
            S.ts("dve", msk, dst, -PI, ALU.is_lt)
            S.stt(dst, msk, 2 * PI, dst, ALU.mult, ALU.add)
            S.ts("dve", dst, dst, PIS, ALU.min, -PIS, ALU.max)

        wrap(kf[:, sl], angf[:, sl], 0.0)
        S.act(V(sinT[:, sl], "sinT"), kf[:, sl], AF.Sin)
        wrap(kf[:, sl], angf[:, sl], PI / 2)
        S.act(V(cosT[:, sl], "cosT"), kf[:, sl], AF.Sin)
    S.barrier()

    loaded = set()

    def issue_loads(hg, tg):
        if tg >= ngroups:
            hg, tg = hg + 1, 0
        if hg >= npass or (hg, tg) in loaded:
            return
        loaded.add((hg, tg))
        c_ = slice(tg * 512, (tg + 1) * 512)
        if hg == 0:
            for k in range(16):
                S.dma("sp", V(xg[:, k, :], f"xg_{k}"), V(t["xT1"].ap[k * 128:(k + 1) * 128, c_], t["xT1"].keys))
        else:
            S.dma("sp", V(xno[:], "xno"), V(t["xnoscr"].ap[:, tg], f"xnoscr_{tg}"))
            for k in range(16):
                S.dma("sp", V(xn[:, k, :], f"xn_{k}"), V(t["xnscr"].ap[k * 128:(k + 1) * 128, c_], f"xnscr_{tg}"))

    for hg, tg in [(a, b_) for a in range(npass) for b_ in range(ngroups)]:
        cols = slice(tg * 512, (tg + 1) * 512)
        if tg == 0:
            for k in range(16):
                S.dma("pool", V(w1s[:, k, :], f"w1s_{k}"), V(t["w1"].ap[hg, k * 128:(k + 1) * 128, :], t["w1"].keys))
            S.dma("sp", wa2, V(t["wa2"].ap[hg], t["wa2"].keys))
            S.dma("sp", ba, V(t["ba"].ap[hg], t["ba"].keys))
            S.memset("dve", Sst, 0.0)
        def pick(dst, srcs):
            S.ts("dve", dst, srcs[0], sel4[:, 0:1], ALU.mult)
            for q in range(1, 4):
                S.stt(dst, srcs[q], sel4[:, q:q + 1], dst, ALU.mult, ALU.add)

        xn_all = [f"xn_{k}" for k in range(16)]
        gc0 = tg * 512
        cs2 = V(cs2all[:, tg, :], f"cs2_{tg}")
        sn2 = V(sn2all[:, tg, :], f"sn2_{tg}")
        issue_loads(hg, tg)
        if hg == 0:
            rmsnorm_fm(S, lambda k: V(xg[:, k, :], f"xg_{k}"), lambda k: g1[:, k:k + 1],
                       lambda k: V(xn[:, k, :], f"xn_{k}"), 16, 512, 2048.0, tmp, pb[0], ones_bf, eps_col)
            if tg + 1 < ngroups:
                issue_loads(0, tg + 1)
            pick(V(xno[:], "xno"), [V(xn[:, :, q * 128:(q + 1) * 128], xn_all) for q in range(4)])
            for tab, dst2 in ((cosT, cs2), (sinT, sn2)):
                nm = "cosT" if tab is cosT else "sinT"
                pick(dst2[:, 0:128], [V(tab[:, gc0 + q * 128:gc0 + (q + 1) * 128], nm) for q in range(4)])
                S.copy("pool", dst2[:, 128:256], dst2[:, 0:128])
            if npass > 1:
                for k in range(16):
                    S.dma("sp", V(t["xnscr"].ap[k * 128:(k + 1) * 128, cols], f"xnscr_{tg}"), V(xn[:, k, :], f"xn_{k}"))
                S.dma("sp", V(t["xnoscr"].ap[:, tg], f"xnoscr_{tg}"), V(xno[:], "xno"))

        def qk_post_multi(items, fillers=()):
            fillers = list(fillers)

            def fill():
                if fillers:
                    fillers.pop(0)()

            for ps, gcol, dst, cos_v, sin_v, w, T, nb in items:
                S.act(T["sq"][:, 0:w], ps, AF.Square)
            fill()
            for ps, gcol, dst, cos_v, sin_v, w, T, nb in items:
                S.mm(nb[:, 0:w], onesblk, T["sq"][:, 0:w])
            for ps, gcol, dst, cos_v, sin_v, w, T, nb in items:
                S.act(T["rt"][:, 0:w], nb[:, 0:w], AF.Sqrt, scale=1.0 / 64, bias=eps_col)
            for ps, gcol, dst, cos_v, sin_v, w, T, nb in items:
                S.recip(T["rb"][:, 0:w], T["rt"][:, 0:w])
            for ps, gcol, dst, cos_v, sin_v, w, T, nb in items:
                S.stt(T["qn"][:, 0:w], ps, gcol, T["rb"][:, 0:w], ALU.mult, ALU.mult)
            fill()
            for ps, gcol, dst, cos_v, sin_v, w, T, nb in items:
                S.mm(nb[:, 0:w], protT, T["qn"][:, 0:w])
            for ps, gcol, dst, cos_v, sin_v, w, T, nb in items:
                S.tt("dve", T["t2"][:, 0:w], nb[:, 0:w], sin_v, ALU.mult)
                S.tt("pool", T["t1"][:, 0:w], T["qn"][:, 0:w], cos_v, ALU.mult)
            for ps, gcol, dst, cos_v, sin_v, w, T, nb in items:
                S.tt("pool", dst, T["t1"][:, 0:w], T["t2"][:, 0:w], ALU.add)

        def tm_block(tb):
            blk = tg * 4 + tb
            tsl = slice(tb * 128, (tb + 1) * 128)
            ps = pb[3]
            for k in range(16):
                S.mm(ps[:, 0:256], V(xn[:, k, tsl], f"xn_{k}"), V(w1s[:, k, C_TM1:C_TM1 + 256], f"w1s_{k}"), start=(k == 0), stop=(k == 15))
            for h2 in range(2):
                S.copy("act", V(Vaug[:, blk, h2, 0:128], f"Vaug_{tg}"), ps[:, h2 * 128:(h2 + 1) * 128])
            ps = pb[7]
            for k in range(16):
                S.mm(ps[:, 0:384], V(xn[:, k, tsl], f"xn_{k}"), V(w1s[:, k, C_TM2:C_TM2 + 384], f"w1s_{k}"), start=(k == 0), stop=(k == 15))
            S.copy("dve", k_sb[tb], ps[:, 0:128])
            S.copy("dve", vg[tb], ps[:, 128:384])

        TA = {"sq": tmp["sq"][0], "rt": tmp["rt"], "rb": tmp["rb"], "qn": qn_bf, "t1": t1, "t2": t2}
        TB = {"sq": tmp["sq"][1], "rt": rtB, "rb": rbB, "qn": qnB, "t1": t1B, "t2": t2B}
        items = []
        for i, (c0, hd) in enumerate([(C_KA, 0), (C_KB, 1)]):
            ps = pb[1 + (i % 2)]
            for k in range(16):
                S.mm(ps, V(w1s[:, k, c0:c0 + 128], f"w1s_{k}"), V(xn[:, k, :], f"xn_{k}"), start=(k == 0), stop=(k == 15))
            items.append((ps, pp1[:, 2:3], V(KT[:, hd, cols], f"KT_{hd}_{tg}"), V(cosT[:, cols], "cosT"), V(sinT[:, cols], "sinT"), 512,
                          TA if i == 0 else TB, pb[0] if i == 0 else pb[4]))
        qk_post_multi(items, fillers=[lambda: tm_block(0), lambda: tm_block(1)])
        ps = pb[1]
        for hd, c0 in enumerate((C_QA, C_QB)):
            for k in range(16):
                S.mm(ps[:, hd * 128:(hd + 1) * 128], V(w1s[:, k, c0:c0 + 128], f"w1s_{k}"), V(xno[:, k, :], "xno"), start=(k == 0), stop=(k == 15))
        qk_post_multi([(ps[:, 0:256], pp1[:, 0:1], V(QTo[:].rearrange("p a b -> p (a b)"), "QTo"), cs2, sn2, 256, TA, pb[0])],
                      fillers=[lambda: tm_block(2), lambda: tm_block(3)])
        ps = pb[2]
        for k in range(16):
            S.mm(ps[:, 0:128], V(w1s[:, k, C_GQ:C_GQ + 128], f"w1s_{k}"), V(xno[:, k, :], "xno"), start=(k == 0), stop=(k == 15))
        S.act(gqo, ps[:, 0:128], AF.Copy, scale=128.0 ** -0.5)
        for k in range(16):
            S.mm(ps[0:16, :], V(w1s[:, k, C_LR:C_LR + 16], f"w1s_{k}"), V(xn[:, k, :], f"xn_{k}"), start=(k == 0), stop=(k == 15))
        S.copy("dve", glrT, ps[0:16, :])
        ps = pb[3]
        for k in range(16):
            S.mm(ps[:, 0:256], V(xno[:, k, :], "xno"), V(w1s[:, k, C_TM1 + 256:C_TM1 + 512], f"w1s_{k}"), start=(k == 0), stop=(k == 15))
        S.act(sgr, ps[:, 0:256], AF.Silu)
        if hg >= 1 or tg == ngroups - 1:
            issue_loads(hg, tg + 1)
        zb = [pb[7][:, tb * 128:(tb + 1) * 128] for tb in range(4)]
        rvb = [pb[1][:, tb * 128:(tb + 1) * 128] for tb in range(4)]
        for tb in range(4):
            tsl = slice(tb * 128, (tb + 1) * 128)
            S.mm(zb[tb], glrT[0:16, tsl], wa2, start=True, stop=False)
            S.mm(zb[tb], ones_f[0:1, 0:128], ba, start=False, stop=True)
        for tb in range(4):
            S.act(ez[tb], zb[tb], AF.Exp, scale=-1.0)
        for tb in range(4):
            S.act(lsp[tb], ez[tb], AF.Ln, bias=ones_f[:, 0:1])
        for tb in range(4):
            S.mm(rvb[tb], mstrict, lsp[tb])
        for tb in range(4):
            S.mm(pb[2][:, tb * 2:tb * 2 + 2], lsp[tb], chunkind)
        for tb in range(4):
            S.act(wexp[tb], rvb[tb], AF.Exp, scale=-1.0 / 16)
        S.act(dec, pb[2][:, 0:8], AF.Exp, scale=-1.0 / 16)
        for tb in range(4):
            S.tt("dve", kdec[tb], k_sb[tb], wexp[tb], ALU.mult)
        dslot = [V(pb[7].ap[:, 0:256], ["pb7a", "pb7"]), V(pb[3].ap[:, 0:256], ["pb3a", "pb3"]),
                 V(pb[7].ap[:, 256:512], ["pb7b", "pb7"]), V(pb[3].ap[:, 256:512], ["pb3b", "pb3"])]
        opsb = [pb[4][:, 0:256], pb[4][:, 256:512], pb[5][:, 0:256], pb[5][:, 256:512]]
        for c8 in range(8):
            tb, cc = c8 // 2, c8 % 2
            psl = slice(64 * cc, 64 * cc + 64)
            ds_ps = dslot[c8 % 4]
            S.mm(ds_ps, kdec[tb][psl, :], vg[tb][psl, :])
            S.stt(Sst2[(c8 + 1) % 2], Sst2[c8 % 2], dec[:, c8:c8 + 1], ds_ps, ALU.mult, ALU.add)
            S.copy("pool", Sbf[c8], Sst2[(c8 + 1) % 2])
            S.mm(opsb[tb][psl, :], gqo[:, 64 * cc:64 * cc + 64], Sbf[c8])
        pick(osel, opsb)
        S.act(junk, osel, AF.Square, accum=gsm[:, 0:1])
        S.act(gsm[:, 1:2], gsm[:, 0:1], AF.Sqrt, scale=1.0 / 256, bias=eps_col)
        S.recip(gsm[:, 2:3], gsm[:, 1:2])
        S.stt(osel, osel, gsm[:, 2:3], gog_bc, ALU.mult, ALU.mult)
        S.tt("pool", osel, osel, sgr, ALU.mult)
        for r in range(2):
            S.tr(pb[6][:, r * 128:(r + 1) * 128], osel[:, r * 128:(r + 1) * 128], ident)
            S.copy("act", mstage[2 + r][:, 0:128], pb[6][:, r * 128:(r + 1) * 128])
        sbanks = [[pb[4], pb[2]], [pb[3], pb[7]]]
        rounds = []
        for hd in range(2):
            for r0 in range(0, 4 * tg + 4, 4):
                rounds.append((hd, list(range(r0, r0 + 4))))

        def emit_qk(R, sset):
            hd, kbs = R
            for i, kb in enumerate(kbs):
                for c in range(2):
                    csl = slice(64 * c, 64 * c + 64)
                    S.mm(sbanks[sset][c][:, i * 128:(i + 1) * 128], V(KT[csl, hd, kb * 128:(kb + 1) * 128], f"KT_{hd}_{kb // 4}"),
                         V(QTo[csl, hd, :], "QTo"))

        pending = []

        def flush_pending():
            while pending:
                hd, dnb = pending.pop(0)
                S.tr(pb[1][:, 384:512], dnb, ident)
                S.copy("act", mstage[hd][:, 0:128], pb[1][:, 384:512])

        def emit_rest(R, sset, n):
            hd, kbs = R
            last = kbs[-1] == 4 * tg + 3
            pe2 = [pexp[sset * 2], pexp[sset * 2 + 1]]
            for c in range(2):
                S.act(pe2[c], sbanks[sset][c], AF.Exp, scale=0.125)
            if last:
                for c in range(2):
                    S.tt("pool", pe2[c], pe2[c], maskT, ALU.mult)
            for i, kb in enumerate(kbs):
                for c in range(2):
                    S.mm(pb[5 + c][:, 0:129], pe2[c][:, i * 128:(i + 1) * 128], V(Vaug[:, kb, hd, 0:129], f"Vaug_{kb // 4}"),
                         start=(kb == 0), stop=(kb == 4 * tg + 3))
            if last:
                flush_pending()
                dnb = dn[hd]
                S.recip(asm[:, 0:1], pb[5][:, 128:129])
                S.recip(asm[:, 1:2], pb[6][:, 128:129])
                S.tt("dve", asm[:, 2:3], asm[:, 1:2], neg_lam, ALU.mult)
                S.ts("dve", o1, pb[5][:, 0:128], asm[:, 0:1], ALU.mult)
                S.stt(dd, pb[6][:, 0:128], asm[:, 2:3], o1, ALU.mult, ALU.add)
                S.act(junk2, dd, AF.Square, accum=asm[:, 3:4])
                S.act(asm[:, 4:5], asm[:, 3:4], AF.Sqrt, scale=1.0 / 128, bias=eps_col)
                S.recip(asm[:, 5:6], asm[:, 4:5])
                S.stt(dnb, dd, asm[:, 5:6], subg_bc, ALU.mult, ALU.mult)
                pending.append((hd, dnb))

        emit_qk(rounds[0], 0)
        for n, R in enumerate(rounds):
            if n + 1 < len(rounds):
                emit_qk(rounds[n + 1], (n + 1) % 2)
            emit_rest(R, n % 2, n)
        flush_pending()
        ocols = slice(tg * 128, (tg + 1) * 128)
        for r in range(4):
            r0 = row_of(hg, r)
            S.dma("sp", V(t["mixT1"].ap[r0:r0 + 128, ocols], t["mixT1"].keys), mstage[r][:, 0:128])


def build_nc_phase1_only(ngroups=8):
    import contextlib
    nc = bass.Bass("TRN2", target_bir_lowering=False)
    t = {}
    for name, shape, dt in P1_INPUTS:
        t[name] = V(nc.dram_tensor(name, shape, dt, kind="ExternalInput").ap(), "dram_" + name)
    t["mixT1"] = V(nc.dram_tensor("mixT1", [512, 1024], BF16, kind="ExternalOutput").ap(), "dram_mixT1")
    S = Sched(nc)
    with contextlib.ExitStack() as es:
        build_phase1(nc, S, t, es, ngroups=ngroups)
        S.emit()
    return nc


_NC_CACHE = {}


def build_nc_fused(ngroups=8):
    import contextlib
    nc = bass.Bass("TRN2", target_bir_lowering=False)
    t1, t2 = {}, {}
    for name, shape, dt in P1_INPUTS:
        shape = list(shape)
        if name in ("w1", "wa2", "ba"):
            shape[0] = 4
        t1[name] = V(nc.dram_tensor(name, shape, dt, kind="ExternalInput").ap(), "dram_" + name)
    mixscr = V(nc.dram_tensor("mixscr", [2048, 1024], BF16, kind="Internal").ap(), "dram_mixscr")
    t1["mixT1"] = mixscr
    t1["xnscr"] = V(nc.dram_tensor("xnscr", [2048, 4096], BF16, kind="Internal").ap(), "dram_xnscr")
    t1["xnoscr"] = V(nc.dram_tensor("xnoscr", [128, 8, 16, 128], BF16, kind="Internal").ap(), "dram_xnoscr")
    for name, shape, dt in P2_INPUTS:
        if name == "mixT":
            continue
        t2[name] = V(nc.dram_tensor(name, shape, dt, kind="ExternalInput").ap(), "dram_" + name)
    t2["mixT"] = mixscr
    t2["outT"] = V(nc.dram_tensor("outT", [2048, 1024], F32, kind="ExternalOutput").ap(), "dram_outT")
    S = Sched(nc)

    def row_of(hg, r):
        return hg * 256 + r * 128 if r < 2 else 1024 + hg * 256 + (r - 2) * 128

    with contextlib.ExitStack() as es1:
        build_phase1(nc, S, t1, es1, ngroups=ngroups, npass=4, row_of=row_of)
    S.barrier()
    with contextlib.ExitStack() as es2:
        build_phase2(nc, S, t2, es2, mix_select=False)
    S.emit()
    return nc


def host_fused_inputs(inp, c):
    m = host_phase1_inputs(inp, c, groups=[0, 1, 2, 3])
    m.update(host_phase2_inputs(inp, c))
    return m


def kernel(**inp):
    inp = {k: np.asarray(v) for k, v in inp.items()}
    if "fused" not in _NC_CACHE:
        _NC_CACHE["fused"] = build_nc_fused()
    cores = list(range(8))
    res = run_bass_kernel_spmd(_NC_CACHE["fused"], [host_fused_inputs(inp, c) for c in cores], core_ids=cores)
    out = np.empty((2, 4096, 2048), np.float32)
    for c in cores:
        b, j = c // 4, c % 4
        out[b, own_tokens(c), :] = np.asarray(res.results[c]["outT"]).T
    return out
```

```python
import numpy as np
import ml_dtypes
import concourse.bass as bass
import concourse.mybir as mybir
from concourse.bass_utils import run_bass_kernel_spmd

F32 = mybir.dt.float32
BF16 = mybir.dt.bfloat16
I32 = mybir.dt.int32
AF = mybir.ActivationFunctionType
ALU = mybir.AluOpType
AX = mybir.AxisListType

EPS = 1e-6
NDMA_SLOTS = 16


class V:
    def __init__(self, ap, keys):
        self.ap = ap
        self.keys = tuple(keys) if isinstance(keys, (list, tuple)) else (keys,)

    def __getitem__(self, idx):
        return V(self.ap[idx], self.keys)

    def k(self, *keys):
        return V(self.ap, keys)


class Sched:
    ENG = ("pe", "act", "dve", "pool", "sp")

    def __init__(self, nc):
        self.nc = nc
        self.ops = []
        self.dma_rr = {"sp": 0, "pool": 0, "act": 0}

    def add(self, issue, fn, reads, writes, dma=False):
        if dma:
            s = self.dma_rr[issue]
            self.dma_rr[issue] = (s + 1) % NDMA_SLOTS
            stream = f"dma_{issue}_{s}"
        else:
            stream = issue
        rk, wk = [], []
        for v in reads:
            rk.extend(v.keys)
        for v in writes:
            wk.extend(v.keys)
        self.ops.append(dict(stream=stream, issue=issue, fn=fn, reads=rk, writes=wk, dma=dma))

    def barrier(self):
        self.ops.append(dict(barrier=True))

    def mm(self, out, lhsT, rhs, start=True, stop=True, extra_reads=()):
        self.add("pe", lambda e: e.matmul(out.ap, lhsT.ap, rhs.ap, start=start, stop=stop),
                 [lhsT, rhs, *extra_reads], [out])

    def tr(self, out, in_, ident):
        self.add("pe", lambda e: e.transpose(out.ap, in_.ap, ident.ap), [in_, ident], [out])

    def act(self, out, in_, func, scale=1.0, bias=0.0, accum=None):
        reads = [in_]
        if isinstance(scale, V):
            reads.append(scale)
        if isinstance(bias, V):
            reads.append(bias)
        writes = [out] + ([accum] if accum is not None else [])
        sc = scale.ap if isinstance(scale, V) else scale
        bi = bias.ap if isinstance(bias, V) else bias
        if accum is None:
            self.add("act", lambda e: e.activation(out.ap, in_.ap, func, bias=bi, scale=sc), reads, writes)
        else:
            self.add("act", lambda e: e.activation(out.ap, in_.ap, func, bias=bi, scale=sc, accum_out=accum.ap),
                     reads, writes)

    def tt(self, eng, out, a, b, op):
        self.add(eng, lambda e: e.tensor_tensor(out.ap, a.ap, b.ap, op), [a, b], [out])

    def ts(self, eng, out, a, s1, op0, s2=None, op1=None):
        reads = [a] + [s for s in (s1, s2) if isinstance(s, V)]
        x1 = s1.ap if isinstance(s1, V) else s1
        x2 = s2.ap if isinstance(s2, V) else s2
        if op1 is None:
            self.add(eng, lambda e: e.tensor_scalar(out.ap, a.ap, x1, None, op0), reads, [out])
        else:
            self.add(eng, lambda e: e.tensor_scalar(out.ap, a.ap, x1, x2, op0, op1), reads, [out])

    def stt(self, out, a, s, b, op0, op1):
        reads = [a, b] + ([s] if isinstance(s, V) else [])
        x = s.ap if isinstance(s, V) else s
        self.add("dve", lambda e: e.scalar_tensor_tensor(out.ap, a.ap, x, b.ap, op0, op1), reads, [out])

    def copy(self, eng, out, in_):
        if eng == "act":
            self.add("act", lambda e: e.copy(out.ap, in_.ap), [in_], [out])
        else:
            self.add(eng, lambda e: e.tensor_copy(out.ap, in_.ap), [in_], [out])

    def recip(self, out, in_):
        self.add("dve", lambda e: e.reciprocal(out.ap, in_.ap), [in_], [out])

    def memset(self, eng, out, val):
        self.add(eng, lambda e: e.memset(out.ap, val), [], [out])

    def dma(self, issue, out, in_):
        self.add(issue, lambda e: e.dma_start(out=out.ap, in_=in_.ap), [in_], [out], dma=True)

    def emit(self):
        nc = self.nc
        stream_pos = {}
        fence = {}
        ops = []
        for op in self.ops:
            if op.get("barrier"):
                fence = dict(stream_pos)
                continue
            p = stream_pos.get(op["stream"], 0) + 1
            stream_pos[op["stream"]] = p
            op["pos"] = p
            op["fence"] = fence
            ops.append(op)
        n = len(ops)
        last_w = {}
        readers = {}
        seen = {e: {} for e in self.ENG}
        for i, op in enumerate(ops):
            need = {}

            def want(j):
                o = ops[j]
                st = o["stream"]
                if st == "pe" and op["stream"] == "pe":
                    return
                if o["pos"] > need.get(st, 0):
                    need[st] = o["pos"]

            for k in op["reads"]:
                if k in last_w:
                    want(last_w[k])
            for k in op["writes"]:
                if k in last_w:
                    want(last_w[k])
                for r in readers.get(k, ()):
                    if r != i:
                        want(r)
            for st, p in op["fence"].items():
                if st == "pe" and op["stream"] == "pe":
                    continue
                if p > need.get(st, 0):
                    need[st] = p
            if op["dma"] and op["pos"] > 1:
                if op["pos"] - 1 > need.get(op["stream"], 0):
                    need[op["stream"]] = op["pos"] - 1
            sn = seen[op["issue"]]
            waits = []
            for st, p in need.items():
                if sn.get(st, 0) < p:
                    sn[st] = p
                    waits.append((st, p))
            op["waits"] = waits
            for k in op["reads"]:
                readers.setdefault(k, []).append(i)
            for k in op["writes"]:
                last_w[k] = i
                readers[k] = []
        by_stream = {}
        for i, op in enumerate(ops):
            by_stream.setdefault(op["stream"], []).append(i)
        signal = set()
        for op in ops:
            for st, p in op["waits"]:
                signal.add((st, p))
            if op["dma"]:
                signal.add((op["stream"], op["pos"]))
        for st, lst in by_stream.items():
            signal.add((st, ops[lst[-1]]["pos"]))
        rank = {}
        for st, lst in by_stream.items():
            r = 0
            for i in lst:
                if (st, ops[i]["pos"]) in signal:
                    r += 1
                    rank[(st, ops[i]["pos"])] = r
        finals = {st: max(v for (s, _), v in rank.items() if s == st) for st in by_stream}
        import contextlib
        with contextlib.ExitStack() as es:
            sems = {st: es.enter_context(nc.semaphore(f"s_{st}")) for st in by_stream}
            block = es.enter_context(nc.Block())
            handles = {"pe": nc.tensor, "act": nc.scalar, "dve": nc.vector, "pool": nc.gpsimd, "sp": nc.sync}

            def run_engine(ename):
                def body(_e):
                    eng = handles[ename]
                    for op in ops:
                        if op["issue"] != ename:
                            continue
                        for st, p in op["waits"]:
                            mult = 16 if st.startswith("dma_") else 1
                            eng.wait_ge(sems[st], rank[(st, p)] * mult)
                        ins = op["fn"](eng)
                        key = (op["stream"], op["pos"])
                        if key in rank:
                            ins.then_inc(sems[op["stream"]], 16 if op["dma"] else 1)
                    if ename == "sp":
                        for st, f in finals.items():
                            mult = 16 if st.startswith("dma_") else 1
                            eng.wait_ge(sems[st], f * mult)
                return body

            block.tensor(run_engine("pe"))
            block.scalar(run_engine("act"))
            block.vector(run_engine("dve"))
            block.gpsimd(run_engine("pool"))
            block.sync(run_engine("sp"))


def _alloc(es, nc, name, shape, dt):
    return es.enter_context(nc.sbuf_tensor("sb_" + name, list(shape), dt))


def rmsnorm_fm(S, src_fn, gcol_fn, dst_fn, nk, ntok, D, tmp, pbank, ones_bf, eps_col, f32_fn=None):
    for k in range(nk):
        sq = tmp["sq"][k % 2][:, 0:ntok]
        S.act(sq, src_fn(k), AF.Square)
        S.mm(pbank[:, 0:ntok], ones_bf, sq, start=(k == 0), stop=(k == nk - 1))
    rt = tmp["rt"][:, 0:ntok]
    S.act(rt, pbank[:, 0:ntok], AF.Sqrt, scale=1.0 / D, bias=eps_col)
    rb = tmp["rb"][:, 0:ntok]
    S.recip(rb, rt)
    for k in range(nk):
        if f32_fn is None:
            S.stt(dst_fn(k), src_fn(k), gcol_fn(k), rb, ALU.mult, ALU.mult)
        else:
            f32_fn(k, rb)


def build_phase2(nc, S, t, es_outer, mix_select=False):
    import contextlib
    es = es_outer
    NT = 1024
    hT = _alloc(es, nc, "hT", [128, 16, NT], F32)
    ones_bf = V(_alloc(es, nc, "ones_bf", [128, 128], BF16)[:], "ones_bf")
    ones_f = V(_alloc(es, nc, "ones_f", [128, 128], F32)[:], "ones_f")
    ident = V(_alloc(es, nc, "ident", [128, 128], F32)[:], "ident")
    eps_col = V(_alloc(es, nc, "eps_col", [128, 1], F32)[:], "eps_col")
    gvec = V(_alloc(es, nc, "gvec", [128, 48], F32)[:], "gvec")
    cg = V(_alloc(es, nc, "cg", [128, 8], F32)[:], "cg")
    tmp = {
        "sq": [V(_alloc(es, nc, f"sq{i}", [128, 512], BF16)[:], f"sq{i}") for i in range(2)],
        "rt": V(_alloc(es, nc, "rt", [128, 512], F32)[:], "rt"),
        "rb": V(_alloc(es, nc, "rb", [128, 512], F32)[:], "rb"),
    }
    pb = [V(es.enter_context(nc.psum_tensor(f"pb{i}", [128, 512], F32))[:], f"pb{i}") for i in range(8)]
    wpool = []

    def hv(k, half):
        return V(hT[:, k, half * 512:(half + 1) * 512], f"hT_{k}_{half}")

    S.memset("dve", ones_bf, 1.0)
    S.memset("dve", ones_f, 1.0)
    S.memset("dve", eps_col, EPS)
    S.dma("sp", ident, t["ident"])
    S.dma("sp", gvec, t["gvec"])
    S.dma("sp", cg, t["cg"])

    def wview(i, a, b):
        return V(wpool[i][:].rearrange("p (a b) -> p a b", a=a), f"wp{i}")

    state = {"wi": 0, "pbi": 0}

    def next_w():
        i = state["wi"]
        state["wi"] = (i + 1) % 2
        return i

    def next_pb(lo=4, n=4):
        i = state["pbi"]
        state["pbi"] = (i + 1) % n
        return pb[lo + i]

    def linear_fm(wdram, col0, ncols, in_fn, nk, ntoks, evac):
        for g in range(ncols // 512):
            wi = next_w()
            wv = wview(wi, nk, 512)
            S.dma("pool", wv, V(wdram.ap[:, col0 + g * 512: col0 + (g + 1) * 512].rearrange("(k p) n -> p k n", p=128), wdram.keys))
            for ocl in range(4):
                oc = g * 4 + ocl
                for ti, nt in enumerate(ntoks):
                    ps = next_pb()
                    for k in range(nk):
                        S.mm(ps[:, 0:nt], wv[:, k, ocl * 128:(ocl + 1) * 128], in_fn(k, ti), start=(k == 0), stop=(k == nk - 1))
                    evac(oc, ti, ps[:, 0:nt])

    with contextlib.ExitStack() as esA:
        wpool[:] = [_alloc(esA, nc, f"wpA{i}", [128, 8192], BF16) for i in range(2)]
        mixT = _alloc(esA, nc, "mixT", [128, 16, NT], BF16)
        if mix_select:
            stage = [V(_alloc(esA, nc, f"mstg{q}", [128, 1024], BF16)[:], f"mstg{q}") for q in range(4)]
            sel = V(_alloc(esA, nc, "sel", [128, 4], F32)[:], "sel")
            S.dma("sp", sel, t["sel"])
        for k in range(16):
            if mix_select:
                mk = V(mixT[:, k, :], f"mixT_{k}")
                for q in range(4):
                    S.dma("sp", stage[q], V(t["mixT"].ap[k * 128:(k + 1) * 128, q * 1024:(q + 1) * 1024], t["mixT"].keys))
                S.ts("dve", mk, stage[0], sel[:, 0:1], ALU.mult)
                for q in range(1, 4):
                    S.stt(mk, stage[q], sel[:, q:q + 1], mk, ALU.mult, ALU.add)
            else:
                S.dma("sp", V(mixT[:, k, :], f"mixT_{k}"), V(t["mixT"].ap[k * 128:(k + 1) * 128, :], t["mixT"].keys))
            for half in range(2):
                S.dma("sp", hv(k, half), V(t["xT2"].ap[k * 128:(k + 1) * 128, half * 512:(half + 1) * 512], t["xT2"].keys))

        def evacA(oc, ti, ps):
            S.tt("dve", hv(oc, ti), ps, hv(oc, ti), ALU.add)

        linear_fm(t["w_out"], 0, 2048, lambda k, ti: V(mixT[:, k, ti * 512:(ti + 1) * 512], f"mixT_{k}"), 16, [512, 512], evacA)
    S.barrier()

    with contextlib.ExitStack() as esB:
        wpool[:] = [_alloc(esB, nc, f"wpB{i}", [128, 8192], BF16) for i in range(2)]
        kT = _alloc(esB, nc, "kT", [128, 16, 256], BF16)
        v_sb = _alloc(esB, nc, "v_sb", [128, 2, 2048], BF16)
        with contextlib.ExitStack() as esB1:
            memT = _alloc(esB1, nc, "memT", [128, 16, 256], F32)
            memnT = _alloc(esB1, nc, "memnT", [128, 16, 256], BF16)
            kraw = _alloc(esB1, nc, "kraw", [128, 16, 256], F32)
            for k in range(16):
                S.dma("sp", V(memT[:, k, :], f"memT_{k}"), V(t["memT"].ap[k * 128:(k + 1) * 128, :], t["memT"].keys))
            rmsnorm_fm(S, lambda k: V(memT[:, k, :], f"memT_{k}"), lambda k: gvec[:, 16 + k:17 + k],
                       lambda k: V(memnT[:, k, :], f"memnT_{k}"), 16, 256, 2048.0, tmp, pb[0], ones_bf, eps_col)

            def evacK(oc, ti, ps):
                S.copy("act", V(kraw[:, oc, :], f"kraw_{oc}"), ps)

            linear_fm(t["w_ckv"], 0, 2048, lambda k, ti: V(memnT[:, k, :], f"memnT_{k}"), 16, [256], evacK)
            for h in range(4):
                rmsnorm_fm(S, lambda dc: V(kraw[:, h * 4 + dc, :], f"kraw_{h * 4 + dc}"), lambda dc: cg[:, 4 + dc:5 + dc],
                           lambda dc: V(kT[:, h * 4 + dc, :], f"kT_{h * 4 + dc}"), 4, 256, 512.0, tmp, pb[0], ones_bf, eps_col)
            for g in range(4):
                wi = next_w()
                wv = wview(wi, 16, 512)
                S.dma("pool", wv, V(t["w_ckv"].ap[:, 2048 + g * 512: 2048 + (g + 1) * 512].rearrange("(k p) n -> p k n", p=128), t["w_ckv"].keys))
                for mc in range(2):
                    ps = next_pb()
                    for k in range(16):
                        S.mm(ps, V(memnT[:, k, mc * 128:(mc + 1) * 128], f"memnT_{k}"), wv[:, k, :], start=(k == 0), stop=(k == 15))
                    S.copy("act", V(v_sb[:, mc, g * 512:(g + 1) * 512], f"v_sb_{mc}_{g}"), ps)
        S.barrier()
        xnh = _alloc(esB, nc, "xnh", [128, 16, 512], BF16)
        oTh = _alloc(esB, nc, "oTh", [128, 16, 512], BF16)
        qraw = _alloc(esB, nc, "qraw", [128, 4, 512], F32)
        qT = _alloc(esB, nc, "qT", [128, 4, 512], BF16)
        pT = _alloc(esB, nc, "pT", [128, 2, 512], BF16)
        rden = V(_alloc(esB, nc, "rden", [128, 512], F32)[:], "rden")
        for half in range(2):
            rmsnorm_fm(S, lambda k: hv(k, half), lambda k: gvec[:, k:k + 1],
                       lambda k: V(xnh[:, k, :], f"xnh_{k}"), 16, 512, 2048.0, tmp, pb[0], ones_bf, eps_col)
            for h in range(4):
                wi = next_w()
                wv = wview(wi, 16, 512)
                S.dma("pool", wv, V(t["w_cq"].ap[:, h * 512:(h + 1) * 512].rearrange("(k p) n -> p k n", p=128), t["w_cq"].keys))
                for dc in range(4):
                    ps = next_pb()
                    for k in range(16):
                        S.mm(ps, wv[:, k, dc * 128:(dc + 1) * 128], V(xnh[:, k, :], f"xnh_{k}"), start=(k == 0), stop=(k == 15))
                    S.copy("act", V(qraw[:, dc, :], f"qraw_{dc}"), ps)
                rmsnorm_fm(S, lambda dc: V(qraw[:, dc, :], f"qraw_{dc}"), lambda dc: cg[:, dc:dc + 1],
                           lambda dc: V(qT[:, dc, :], f"qT_{dc}"), 4, 512, 512.0, tmp, pb[0], ones_bf, eps_col)
                for mc in range(2):
                    ps = next_pb()
                    for dc in range(4):
                        S.mm(ps, V(kT[:, h * 4 + dc, mc * 128:(mc + 1) * 128], f"kT_{h * 4 + dc}"), V(qT[:, dc, :], f"qT_{dc}"),
                             start=(dc == 0), stop=(dc == 3))
                    S.act(V(pT[:, mc, :], f"pT_{mc}"), ps, AF.Exp, scale=512.0 ** -0.5)
                ps = pb[1]
                for mc in range(2):
                    S.mm(ps, ones_bf, V(pT[:, mc, :], f"pT_{mc}"), start=(mc == 0), stop=(mc == 1))
                S.recip(rden, ps)
                for dvc in range(4):
                    ps = next_pb()
                    for mc in range(2):
                        S.mm(ps, V(v_sb[:, mc, h * 512 + dvc * 128: h * 512 + (dvc + 1) * 128], f"v_sb_{mc}_{h}"), V(pT[:, mc, :], f"pT_{mc}"),
                             start=(mc == 0), stop=(mc == 1))
                    S.tt("dve", V(oTh[:, h * 4 + dvc, :], f"oTh_{h * 4 + dvc}"), ps, rden, ALU.mult)

            def evacO(oc, ti, ps):
                S.tt("dve", hv(oc, half), ps, hv(oc, half), ALU.add)

            linear_fm(t["w_co"], 0, 2048, lambda k, ti: V(oTh[:, k, :], f"oTh_{k}"), 16, [512], evacO)
    S.barrier()

    with contextlib.ExitStack() as esC:
        wpool[:] = [_alloc(esC, nc, f"wpC{i}", [128, 8192], BF16) for i in range(5)]
        xnT = _alloc(esC, nc, "xnT", [128, 16, NT], BF16)
        hid = _alloc(esC, nc, "hid", [128, 4, NT], BF16)
        xf = [V(_alloc(esC, nc, f"xf{i}", [128, 512], F32)[:], f"xf{i}") for i in range(2)]
        w_r = V(_alloc(esC, nc, "w_r", [128, 16, 36], F32)[:], "w_r")
        b_r = V(_alloc(esC, nc, "b_r", [1, 36], F32)[:], "b_r")
        GT = _alloc(esC, nc, "GT", [32, NT], F32)
        L = V(_alloc(esC, nc, "L", [128, 36], F32)[:], "L")
        sm = V(_alloc(esC, nc, "sm", [128, 16], F32)[:], "sm")
        gm = V(_alloc(esC, nc, "gm", [128, 4], F32)[:], "gm")
        pen = V(_alloc(esC, nc, "pen", [128, 4], F32)[:], "pen")
        ge = V(_alloc(esC, nc, "ge", [128, 4], F32)[:], "ge")
        elm = V(_alloc(esC, nc, "elm", [128, 32], F32)[:], "elm")
        elm2 = V(_alloc(esC, nc, "elm2", [128, 32], F32)[:], "elm2")
        mk1 = V(_alloc(esC, nc, "mk1", [128, 32], F32)[:], "mk1")
        mk2 = V(_alloc(esC, nc, "mk2", [128, 32], F32)[:], "mk2")
        G = V(_alloc(esC, nc, "G", [128, 32], F32)[:], "G")
        sg_t = xf
        gbc = [tmp["rt"], tmp["rb"]]
        S.dma("sp", w_r, V(t["w_r"].ap.rearrange("(k p) n -> p k n", p=128), t["w_r"].keys))
        S.dma("sp", b_r, t["b_r"])
        BIG = 1.0e30
        for half in range(2):
            def f32_fn(k, rb, half=half):
                x = xf[k % 2]
                S.stt(x, hv(k, half), gvec[:, 32 + k:33 + k], rb, ALU.mult, ALU.mult)
                for tb in range(4):
                    S.mm(pb[4 + tb][:, 0:36], x[:, tb * 128:(tb + 1) * 128], w_r[:, k, :], start=(k == 0), stop=False)
                S.copy("act", V(xnT[:, k, half * 512:(half + 1) * 512], f"xnT_{k}_{half}"), x)

            rmsnorm_fm(S, lambda k: hv(k, half), None, None, 16, 512, 2048.0, tmp, pb[0], ones_bf, eps_col, f32_fn=f32_fn)
            for tb in range(4):
                S.mm(pb[4 + tb][:, 0:36], ones_f[0:1, 0:128], b_r, start=False, stop=True)
                S.copy("dve", L, pb[4 + tb][:, 0:36])
                gmax, gsum, grw, m1, m2, d12, sgm, g1, g2, ngmax = [sm[:, i:i + 1] for i in range(10)]
                S.add("dve", lambda e, o=gmax, i=L: e.reduce_max(o.ap, i.ap[:, 0:4], AX.X), [L], [gmax])
                S.ts("dve", gm, L[:, 0:4], gmax, ALU.is_equal)
                S.ts("dve", ngmax, gmax, -1.0, ALU.mult)
                S.act(ge, L[:, 0:4], AF.Exp, bias=ngmax, accum=gsum)
                S.recip(grw, gsum)
                S.ts("dve", pen, gm, BIG, ALU.mult, -BIG, ALU.add)
                for g in range(4):
                    S.ts("dve", elm[:, g * 8:(g + 1) * 8], L[:, 4 + g * 8: 4 + (g + 1) * 8], pen[:, g:g + 1], ALU.add)
                S.add("dve", lambda e, o=m1, i=elm: e.reduce_max(o.ap, i.ap, AX.X), [elm], [m1])
                S.ts("dve", mk1, elm, m1, ALU.is_equal)
                S.stt(elm2, mk1, -BIG, elm, ALU.mult, ALU.add)
                S.add("dve", lambda e, o=m2, i=elm2: e.reduce_max(o.ap, i.ap, AX.X), [elm2], [m2])
                S.ts("dve", mk2, elm2, m2, ALU.is_equal)
                S.tt("dve", d12, m1, m2, ALU.subtract)
                S.act(sgm, d12, AF.Sigmoid)
                S.tt("dve", g1, sgm, grw, ALU.mult)
                S.tt("dve", g2, grw, g1, ALU.subtract)
                S.ts("dve", G, mk1, g1, ALU.mult)
                S.stt(G, mk2, g2, G, ALU.mult, ALU.add)
                S.tr(pb[3][0:32, 0:128], G, ident)
                col = half * 512 + tb * 128
                S.copy("dve", V(GT[:, col:col + 128], f"GT_{half}"), pb[3][0:32, 0:128])
        for e in range(32):
            wg = V(wpool[e % 2][:].rearrange("p (a b) -> p a b", a=16), f"wp{e % 2}")
            wu = V(wpool[2 + e % 2][:].rearrange("p (a b) -> p a b", a=16), f"wp{2 + e % 2}")
            wd = V(wpool[4][:].rearrange("p (a b) -> p a b", a=4), "wp4")
            S.dma("pool", wg, V(t["w_gate"].ap[e].rearrange("(k p) n -> p k n", p=128), t["w_gate"].keys))
            S.dma("pool", wu, V(t["w_up"].ap[e].rearrange("(k p) n -> p k n", p=128), t["w_up"].keys))
            S.dma("pool", wd, V(t["w_down"].ap[e].rearrange("(k p) n -> p k n", p=128), t["w_down"].keys))
            for half in range(2):
                gb = gbc[half]
                S.mm(pb[1], V(ident.ap[0:32, e:e + 1].broadcast_to([32, 128]), ident.keys), V(GT[:, half * 512:(half + 1) * 512], f"GT_{half}"))
                S.copy("act", gb, pb[1])
                for fc in range(4):
                    pg, pu = pb[2], pb[3]
                    for k in range(16):
                        S.mm(pg, wg[:, k, fc * 128:(fc + 1) * 128], V(xnT[:, k, half * 512:(half + 1) * 512], f"xnT_{k}_{half}"),
                             start=(k == 0), stop=(k == 15))
                    for k in range(16):
                        S.mm(pu, wu[:, k, fc * 128:(fc + 1) * 128], V(xnT[:, k, half * 512:(half + 1) * 512], f"xnT_{k}_{half}"),
                             start=(k == 0), stop=(k == 15))
                    sg = sg_t[fc % 2]
                    S.act(sg, pg, AF.Silu)
                    S.tt("pool", sg, sg, gb, ALU.mult)
                    S.tt("dve", V(hid[:, fc, half * 512:(half + 1) * 512], f"hid_{fc}_{half}"), pu, sg, ALU.mult)
                for oc in range(16):
                    ps = next_pb()
                    for fc in range(4):
                        S.mm(ps, wd[:, fc, oc * 128:(oc + 1) * 128], V(hid[:, fc, half * 512:(half + 1) * 512], f"hid_{fc}_{half}"),
                             start=(fc == 0), stop=(fc == 3))
                    S.tt("dve", hv(oc, half), ps, hv(oc, half), ALU.add)
        for k in range(16):
            for half in range(2):
                S.dma("sp", V(t["outT"].ap[k * 128:(k + 1) * 128, half * 512:(half + 1) * 512], t["outT"].keys), hv(k, half))


P2_INPUTS = [
    ("mixT", [2048, 1024], BF16), ("xT2", [2048, 1024], F32), ("memT", [2048, 256], F32),
    ("w_out", [2048, 2048], F32), ("w_cq", [2048, 2048], F32), ("w_co", [2048, 2048], F32),
    ("w_ckv", [2048, 4096], F32), ("gvec", [128, 48], F32), ("cg", [128, 8], F32), ("ident", [128, 128], F32),
    ("w_r", [2048, 36], F32), ("b_r", [1, 36], F32),
    ("w_gate", [32, 2048, 512], F32), ("w_up", [32, 2048, 512], F32), ("w_down", [32, 512, 2048], F32),
]


def host_phase2_inputs(inp, c):
    b, j = c // 4, c % 4
    tok = own_tokens(c)

    def pcol(v):
        return np.ascontiguousarray(v.reshape(-1, 128).T)

    gvec = np.concatenate([pcol(inp["g_cross"][0]), pcol(inp["g_mem"][0]), pcol(inp["g_ffn"][0])], axis=1)
    cg = np.concatenate([pcol(inp["cq_norm_g"][0]), pcol(inp["ck_norm_g"][0])], axis=1)
    return {
        "xT2": np.ascontiguousarray(inp["x"][b, tok, :].T),
        "memT": np.ascontiguousarray(inp["mem"][b].T),
        "w_out": inp["w_out"][0], "w_cq": inp["w_cq"][0], "w_co": inp["w_co"][0], "w_ckv": inp["w_ckv"][0],
        "gvec": np.ascontiguousarray(gvec, dtype=np.float32), "cg": np.ascontiguousarray(cg, dtype=np.float32),
        "ident": np.eye(128, dtype=np.float32),
        "w_r": np.ascontiguousarray(np.concatenate([inp["w_router_grp"][0], inp["w_router_exp"][0]], axis=1)),
        "b_r": np.ascontiguousarray(np.concatenate([inp["b_router_grp"][0], inp["b_router_exp"][0]])[None, :]),
        "w_gate": inp["w_gate"][0], "w_up": inp["w_up"][0], "w_down": inp["w_down"][0],
    }


def build_nc_phase2_only():
    import contextlib
    nc = bass.Bass("TRN2", target_bir_lowering=False)
    t = {}
    for name, shape, dt in P2_INPUTS:
        t[name] = V(nc.dram_tensor(name, shape, dt, kind="ExternalInput").ap(), "dram_" + name)
    t["outT"] = V(nc.dram_tensor("outT", [2048, 1024], F32, kind="ExternalOutput").ap(), "dram_outT")
    S = Sched(nc)
    with contextlib.ExitStack() as es:
        build_phase2(nc, S, t, es)
        S.emit()
    return nc


P1_INPUTS = [
    ("xT1", [2048, 4096], F32), ("w1", [1, 2048, 1552], F32), ("pp1", [128, 8], F32), ("pos", [1, 4096], I32),
    ("g1", [128, 16], F32), ("lam4", [1, 256], F32), ("subg", [1, 128], F32), ("gog", [1, 256], F32),
    ("wa2", [1, 16, 128], F32), ("ba", [1, 1, 128], F32), ("ident1", [128, 128], F32), ("mstrict", [128, 128], F32),
    ("chunkind", [128, 2], F32), ("protT", [128, 128], F32), ("sel4", [128, 4], F32), ("maskT", [128, 512], F32),
]
_DBG_STOP = [99]
C_QA, C_QB, C_KA, C_KB, C_GQ, C_LR, C_TM1, C_TM2, NW1 = 0, 128, 256, 384, 512, 640, 656, 1168, 1552


def host_w1(inp, j):
    w = inp["w_in"][0]
    hA, hB = 2 * j, 2 * j + 1
    cols = []
    cols += [w[:, hA * 128:(hA + 1) * 128], w[:, hB * 128:(hB + 1) * 128]]
    cols += [w[:, 1024 + hA * 128:1024 + (hA + 1) * 128], w[:, 1024 + hB * 128:1024 + (hB + 1) * 128]]
    cols += [w[:, 3072 + j * 128:3072 + (j + 1) * 128]]
    cols += [w[:, 5120:5136]]
    cols += [w[:, 2048 + hA * 128:2048 + (hA + 1) * 128], w[:, 2048 + hB * 128:2048 + (hB + 1) * 128]]
    cols += [w[:, 5136 + j * 256:5136 + (j + 1) * 256]]
    cols += [w[:, 3584 + j * 128:3584 + (j + 1) * 128]]
    cols += [w[:, 4096 + j * 256:4096 + (j + 1) * 256]]
    w1 = np.ascontiguousarray(np.concatenate(cols, axis=1))
    assert w1.shape == (2048, NW1)
    return w1


def host_phase1_inputs(inp, c, groups=None):
    b, j = c // 4, c % 4
    groups = [j] if groups is None else groups
    w1 = np.stack([host_w1(inp, g) for g in groups], axis=0)
    pp1 = np.zeros((128, 8), np.float32)
    pp1[:, 0] = np.tile(inp["q_norm_g"][0], 2)
    pp1[:, 2] = np.tile(inp["k_norm_g"][0], 2)
    half = 32
    freq = (np.float32(10000.0) ** (-np.arange(half, dtype=np.float32) / np.float32(half))).astype(np.float32)
    pp1[:, 4] = np.tile(freq, 4)
    lam4 = np.concatenate([inp["lambda_q1"][0], inp["lambda_k1"][0], inp["lambda_q2"][0], inp["lambda_k2"][0]])[None, :]
    l = np.arange(128)
    mstrict = ((l[:, None] > l[None, :]) & ((l[:, None] // 64) == (l[None, :] // 64))).astype(np.float32)
    chunkind = np.stack([(l < 64), (l >= 64)], axis=1).astype(np.float32)
    protT = np.zeros((128, 128), np.float32)
    for m in range(128):
        if (m % 64) < 32:
            protT[m + 32, m] = -1.0
        else:
            protT[m - 32, m] = 1.0
    return {
        "xT1": np.ascontiguousarray(inp["x"][b].T), "w1": w1, "pp1": pp1,
        "pos": np.ascontiguousarray(inp["positions"][b][None, :].astype(np.int32)),
        "g1": np.ascontiguousarray(inp["g_attn"][0].reshape(16, 128).T),
        "lam4": np.ascontiguousarray(lam4.astype(np.float32)),
        "subg": np.ascontiguousarray(inp["diff_subln_g"][0][None, :]),
        "gog": np.ascontiguousarray(inp["gla_out_g"][0][None, :]),
        "wa2": np.ascontiguousarray(np.stack([inp["gla_w_a2"][0][:, g * 128:(g + 1) * 128] for g in groups], axis=0)),
        "ba": np.ascontiguousarray(np.stack([inp["gla_b_a"][0][None, g * 128:(g + 1) * 128] for g in groups], axis=0)),
        "ident1": np.eye(128, dtype=np.float32), "mstrict": mstrict, "chunkind": chunkind, "protT": protT,
        "sel4": own_sel(c), "maskT": own_mask(c),
    }


def own_sel(c):
    sel = np.zeros((128, 4), np.float32)
    sel[:, c % 4] = 1.0
    return sel


def own_mask(c):
    j = c % 4
    M = np.zeros((128, 4, 128), np.float32)
    for m in range(4):
        if m < j:
            M[:, m, :] = 1.0
        elif m == j:
            M[:, m, :] = 1.0
            M[64:128, m, 0:64] = 0.0
    return np.ascontiguousarray(M.reshape(128, 512))


def own_tokens(c):
    j = c % 4
    return np.concatenate([np.arange((4 * i + j) * 128, (4 * i + j + 1) * 128) for i in range(8)])


def build_phase1(nc, S, t, es, ngroups=8, npass=1, row_of=None):
    import math
    NTOK = 4096
    LAM_INIT = 0.8 - 0.6 * math.exp(-0.3 * 0)
    PI = math.pi

    def A(name, shape, dt):
        return _alloc(es, nc, "p1" + name, shape, dt)

    w1s = A("w1s", [128, 16, NW1], BF16)
    KT = A("KT", [128, 2, NTOK], BF16)
    QTo = A("QTo", [128, 2, 128], BF16)
    Vaug = A("Vaug", [128, 32, 2, 130], BF16)
    cosT = A("cosT", [128, NTOK], BF16)
    sinT = A("sinT", [128, NTOK], BF16)
    gqo = V(A("gqo", [128, 128], BF16)[:], "gqo")
    xno = A("xno", [128, 16, 128], BF16)
    cs2all = A("cs2all", [128, 8, 256], BF16)
    sn2all = A("sn2all", [128, 8, 256], BF16)
    maskT = V(A("maskT", [128, 512], BF16)[:], "maskT")
    sel4 = V(A("sel4", [128, 4], F32)[:], "sel4")
    osel = V(A("osel", [128, 256], F32)[:], "osel")
    xg = A("xg", [128, 16, 512], F32)
    xn = A("xn", [128, 16, 512], BF16)
    ones_bf = V(A("ones_bf", [128, 128], BF16)[:], "ones_bf")
    onesblk = V(A("onesblk", [128, 128], BF16)[:], "onesblk")
    protT = V(A("protT", [128, 128], BF16)[:], "protT")
    ident = V(A("ident", [128, 128], F32)[:], "ident")
    mstrict = V(A("mstrict", [128, 128], F32)[:], "mstrict")
    chunkind = V(A("chunkind", [128, 2], F32)[:], "chunkind")
    ones_f = V(A("ones_f", [128, 128], F32)[:], "ones_f")
    eps_col = V(A("eps_col", [128, 1], F32)[:], "eps_col")
    pp1 = V(A("pp1", [128, 8], F32)[:], "pp1")
    g1 = V(A("g1", [128, 16], F32)[:], "g1")
    lamt = V(A("lamt", [128, 256], F32)[:], "lamt")
    lamv = V(A("lamv", [128, 8], F32)[:], "lamv")
    subg_bc = V(A("subg_bc", [128, 128], F32)[:], "subg_bc")
    gog_bc = V(A("gog_bc", [128, 256], F32)[:], "gog_bc")
    wa2 = V(A("wa2", [16, 128], F32)[:], "wa2")
    ba = V(A("ba", [1, 128], F32)[:], "ba")
    tmp = {
        "sq": [V(A(f"sq{i}", [128, 512], BF16)[:], f"sq{i}") for i in range(2)],
        "rt": V(A("rt", [128, 512], F32)[:], "rt"),
        "rb": V(A("rb", [128, 512], F32)[:], "rb"),
    }
    qn_bf = V(A("qn_bf", [128, 512], BF16)[:], "qn_bf")
    t1 = V(A("t1", [128, 512], F32)[:], "t1")
    t2 = V(A("t2", [128, 512], F32)[:], "t2")
    rtB = V(A("rtB", [128, 512], F32)[:], "rtB")
    rbB = V(A("rbB", [128, 512], F32)[:], "rbB")
    qnB = V(A("qnB", [128, 512], BF16)[:], "qnB")
    t1B = V(A("t1B", [128, 512], F32)[:], "t1B")
    t2B = V(A("t2B", [128, 512], F32)[:], "t2B")
    pexp = [V(A(f"pexp{i}", [128, 512], BF16)[:], f"pexp{i}") for i in range(2)]
    glrT = V(A("glrT", [16, 512], F32)[:], "glrT")
    pexp = pexp + [V(A(f"pexp{i}", [128, 512], BF16)[:], f"pexp{i}") for i in (2, 3)]
    ez = [V(t1.ap[:, i * 128:(i + 1) * 128], "t1") for i in range(4)]
    wexp = ez
    lsp = [V(t2.ap[:, i * 128:(i + 1) * 128], "t2") for i in range(4)]
    k_sb = [V(A(f"k_sb{i}", [128, 128], F32)[:], f"k_sb{i}") for i in range(2)] + \
           [V(rtB.ap[:, i * 128:(i + 1) * 128], "rtB") for i in range(2)]
    kdec = [V(tmp["sq"][0].ap[:, i * 128:(i + 1) * 128], "sq0") for i in range(4)]
    vg = [V(A(f"vg{i}", [128, 256], BF16)[:], f"vg{i}") for i in range(2)] + \
         [V(qnB.ap[:, 0:256], "qnB"), V(qnB.ap[:, 256:512], "qnB")]
    go = [V(tmp["rb"].ap[:, 0:256], "rb"), V(tmp["rb"].ap[:, 256:512], "rb")]
    sgr = V(A("sgr", [128, 256], F32)[:], "sgr")
    Sst2 = [V(A(f"Sst{i}", [128, 256], F32)[:], f"Sst{i}") for i in range(2)]
    Sst = Sst2[0]
    Sbf = [V(A(f"Sbf{i}", [128, 256], BF16)[:], f"Sbf{i}") for i in range(8)]
    dec = V(A("dec", [128, 8], F32)[:], "dec")
    junk = V(t2.ap[:, 0:256], "t2")
    junk2 = V(A("junk2", [128, 128], F32)[:], "junk2")
    gsm = V(A("gsm", [128, 16], F32)[:], "gsm")
    o1 = V(A("o1", [128, 128], F32)[:], "o1")
    dd = V(A("dd", [128, 128], F32)[:], "dd")
    dn = [V(A(f"dn{i}", [128, 128], F32)[:], f"dn{i}") for i in range(2)]
    asm = V(A("asm", [128, 8], F32)[:], "asm")
    mstage = [V(A(f"mstage{r}", [128, 512], BF16)[:], f"mstage{r}") for r in range(4)]
    pb = [V(es.enter_context(nc.psum_tensor(f"p1pb{i}", [128, 512], F32))[:], f"pb{i}") for i in range(8)]

    if row_of is None:
        row_of = lambda hg, r: r * 128
    S.dma("pool", protT, t["protT"])
    S.dma("pool", maskT, t["maskT"])
    for dst, src in [(ident, "ident1"), (mstrict, "mstrict"), (chunkind, "chunkind"), (pp1, "pp1"), (g1, "g1"), (sel4, "sel4")]:
        S.dma("sp", dst, t[src])
    S.dma("sp", lamt, V(t["lam4"].ap.partition_broadcast(128), t["lam4"].keys))
    S.dma("sp", subg_bc, V(t["subg"].ap.partition_broadcast(128), t["subg"].keys))
    S.dma("sp", gog_bc, V(t["gog"].ap.partition_broadcast(128), t["gog"].keys))
    S.memset("dve", ones_bf, 1.0)
    S.memset("dve", ones_f, 1.0)
    S.memset("dve", eps_col, EPS)
    S.memset("dve", onesblk, 0.0)
    S.memset("dve", onesblk[0:64, 0:64], 1.0)
    S.memset("dve", onesblk[64:128, 64:128], 1.0)
    S.memset("pool", V(Vaug[:], [f"Vaug_{g}" for g in range(8)]), 1.0)
    for i in range(2):
        S.tt("dve", junk2[:, 0:64], lamt[:, i * 128:i * 128 + 64], lamt[:, i * 128 + 64:i * 128 + 128], ALU.mult)
        S.add("dve", lambda e, o=lamv[:, i:i + 1], x=junk2: e.reduce_sum(o.ap, x.ap[:, 0:64], AX.X), [junk2], [lamv])
        S.act(lamv[:, 2 + i:3 + i], lamv[:, i:i + 1], AF.Exp)
    S.tt("dve", lamv[:, 4:5], lamv[:, 2:3], lamv[:, 3:4], ALU.subtract)
    S.ts("dve", lamv[:, 5:6], lamv[:, 4:5], LAM_INIT, ALU.add, -1.0, ALU.mult)
    neg_lam = lamv[:, 5:6]
    S.ts("dve", subg_bc, subg_bc, 1.0 - LAM_INIT, ALU.mult)
    xgf = xg[:].rearrange("p a b -> p (a b)")
    posi = V(xgf[:, 0:4096].bitcast(I32), "xg_tab0")
    angf = V(xgf[:, 4096:8192], "xg_tab1")
    kf = V(xgf[:, 0:4096], "xg_tab0")
    ki = V(xgf[:, 0:4096].bitcast(I32), "xg_tab0")
    S.dma("sp", posi, V(t["pos"].ap.partition_broadcast(128), t["pos"].keys))
    S.copy("dve", angf, posi)
    S.ts("dve", angf, angf, pp1[:, 4:5], ALU.mult)
    S.ts("dve", t1.k("xg_tab0")[:, 0:1], angf[:, 0:1], 1.0, ALU.mult)
    for c0 in range(0, 4096, 2048):
        sl = slice(c0, c0 + 2048)
        S.ts("dve", kf[:, sl], angf[:, sl], 1.0 / (2 * PI), ALU.mult)
        S.copy("dve", ki[:, sl], kf[:, sl])
        S.copy("dve", kf[:, sl], ki[:, sl])
        C1 = 6.28125
        C2 = 2 * PI - C1
        S.stt(angf[:, sl], kf[:, sl], -C1, angf[:, sl], ALU.mult, ALU.add)
        S.stt(angf[:, sl], kf[:, sl], -C2, angf[:, sl], ALU.mult, ALU.add)
        msk = V(xn[:].rearrange("p a b -> p (a b)").bitcast(F32)[:, 0:2048], "xn_tab")
        PIS = 3.1415925

        def wrap(dst, src, shift):
            S.ts("dve", dst, src, shift, ALU.add)
            S.ts("dve", msk, dst, PI, ALU.is_gt)
            S.stt(dst, msk, -2 * PI, dst, ALU.mult, ALU.add)
            S.ts("dve", msk, dst, -PI, ALU.is_lt)
            S.stt(dst, msk, 2 * PI, dst, ALU.mult, ALU.add)
            S.ts("dve", dst, dst, PIS, ALU.min, -PIS, ALU.max)

        wrap(kf[:, sl], angf[:, sl], 0.0)
        S.act(V(sinT[:, sl], "sinT"), kf[:, sl], AF.Sin)
        wrap(kf[:, sl], angf[:, sl], PI / 2)
        S.act(V(cosT[:, sl], "cosT"), kf[:, sl], AF.Sin)
    S.barrier()

    loaded = set()

    def issue_loads(hg, tg):
        if tg >= ngroups:
            hg, tg = hg + 1, 0
        if hg >= npass or (hg, tg) in loaded:
            return
        loaded.add((hg, tg))
        c_ = slice(tg * 512, (tg + 1) * 512)
        if hg == 0:
            for k in range(16):
                S.dma("sp", V(xg[:, k, :], f"xg_{k}"), V(t["xT1"].ap[k * 128:(k + 1) * 128, c_], t["xT1"].keys))
        else:
            S.dma("sp", V(xno[:], "xno"), V(t["xnoscr"].ap[:, tg], f"xnoscr_{tg}"))
            for k in range(16):
                S.dma("sp", V(xn[:, k, :], f"xn_{k}"), V(t["xnscr"].ap[k * 128:(k + 1) * 128, c_], f"xnscr_{tg}"))

    for hg, tg in [(a, b_) for a in range(npass) for b_ in range(ngroups)]:
        cols = slice(tg * 512, (tg + 1) * 512)
        if tg == 0:
            for k in range(16):
                S.dma("pool", V(w1s[:, k, :], f"w1s_{k}"), V(t["w1"].ap[hg, k * 128:(k + 1) * 128, :], t["w1"].keys))
            S.dma("sp", wa2, V(t["wa2"].ap[hg], t["wa2"].keys))
            S.dma("sp", ba, V(t["ba"].ap[hg], t["ba"].keys))
            S.memset("dve", Sst, 0.0)
        def pick(dst, srcs):
            S.ts("dve", dst, srcs[0], sel4[:, 0:1], ALU.mult)
            for q in range(1, 4):
                S.stt(dst, srcs[q], sel4[:, q:q + 1], dst, ALU.mult, ALU.add)

        xn_all = [f"xn_{k}" for k in range(16)]
        gc0 = tg * 512
        cs2 = V(cs2all[:, tg, :], f"cs2_{tg}")
        sn2 = V(sn2all[:, tg, :], f"sn2_{tg}")
        issue_loads(hg, tg)
        if hg == 0:
            rmsnorm_fm(S, lambda k: V(xg[:, k, :], f"xg_{k}"), lambda k: g1[:, k:k + 1],
                       lambda k: V(xn[:, k, :], f"xn_{k}"), 16, 512, 2048.0, tmp, pb[0], ones_bf, eps_col)
            if tg + 1 < ngroups:
                issue_loads(0, tg + 1)
            pick(V(xno[:], "xno"), [V(xn[:, :, q * 128:(q + 1) * 128], xn_all) for q in range(4)])
            for tab, dst2 in ((cosT, cs2), (sinT, sn2)):
                nm = "cosT" if tab is cosT else "sinT"
                pick(dst2[:, 0:128], [V(tab[:, gc0 + q * 128:gc0 + (q + 1) * 128], nm) for q in range(4)])
                S.copy("pool", dst2[:, 128:256], dst2[:, 0:128])
            if npass > 1:
                for k in range(16):
                    S.dma("sp", V(t["xnscr"].ap[k * 128:(k + 1) * 128, cols], f"xnscr_{tg}"), V(xn[:, k, :], f"xn_{k}"))
                S.dma("sp", V(t["xnoscr"].ap[:, tg], f"xnoscr_{tg}"), V(xno[:], "xno"))

        def qk_post_multi(items, fillers=()):
            fillers = list(fillers)

            def fill():
                if fillers:
                    fillers.pop(0)()

            for ps, gcol, dst, cos_v, sin_v, w, T, nb in items:
                S.act(T["sq"][:, 0:w], ps, AF.Square)
            fill()
            for ps, gcol, dst, cos_v, sin_v, w, T, nb in items:
                S.mm(nb[:, 0:w], onesblk, T["sq"][:, 0:w])
            for ps, gcol, dst, cos_v, sin_v, w, T, nb in items:
                S.act(T["rt"][:, 0:w], nb[:, 0:w], AF.Sqrt, scale=1.0 / 64, bias=eps_col)
            for ps, gcol, dst, cos_v, sin_v, w, T, nb in items:
                S.recip(T["rb"][:, 0:w], T["rt"][:, 0:w])
            for ps, gcol, dst, cos_v, sin_v, w, T, nb in items:
                S.stt(T["qn"][:, 0:w], ps, gcol, T["rb"][:, 0:w], ALU.mult, ALU.mult)
            fill()
            for ps, gcol, dst, cos_v, sin_v, w, T, nb in items:
                S.mm(nb[:, 0:w], protT, T["qn"][:, 0:w])
            for ps, gcol, dst, cos_v, sin_v, w, T, nb in items:
                S.tt("dve", T["t2"][:, 0:w], nb[:, 0:w], sin_v, ALU.mult)
                S.tt("pool", T["t1"][:, 0:w], T["qn"][:, 0:w], cos_v, ALU.mult)
            for ps, gcol, dst, cos_v, sin_v, w, T, nb in items:
                S.tt("pool", dst, T["t1"][:, 0:w], T["t2"][:, 0:w], ALU.add)

        def tm_block(tb):
            blk = tg * 4 + tb
            tsl = slice(tb * 128, (tb + 1) * 128)
            ps = pb[3]
            for k in range(16):
                S.mm(ps[:, 0:256], V(xn[:, k, tsl], f"xn_{k}"), V(w1s[:, k, C_TM1:C_TM1 + 256], f"w1s_{k}"), start=(k == 0), stop=(k == 15))
            for h2 in range(2):
                S.copy("act", V(Vaug[:, blk, h2, 0:128], f"Vaug_{tg}"), ps[:, h2 * 128:(h2 + 1) * 128])
            ps = pb[7]
            for k in range(16):
                S.mm(ps[:, 0:384], V(xn[:, k, tsl], f"xn_{k}"), V(w1s[:, k, C_TM2:C_TM2 + 384], f"w1s_{k}"), start=(k == 0), stop=(k == 15))
            S.copy("dve", k_sb[tb], ps[:, 0:128])
            S.copy("dve", vg[tb], ps[:, 128:384])

        TA = {"sq": tmp["sq"][0], "rt": tmp["rt"], "rb": tmp["rb"], "qn": qn_bf, "t1": t1, "t2": t2}
        TB = {"sq": tmp["sq"][1], "rt": rtB, "rb": rbB, "qn": qnB, "t1": t1B, "t2": t2B}
        items = []
        for i, (c0, hd) in enumerate([(C_KA, 0), (C_KB, 1)]):
            ps = pb[1 + (i % 2)]
            for k in range(16):
                S.mm(ps, V(w1s[:, k, c0:c0 + 128], f"w1s_{k}"), V(xn[:, k, :], f"xn_{k}"), start=(k == 0), stop=(k == 15))
            items.append((ps, pp1[:, 2:3], V(KT[:, hd, cols], f"KT_{hd}_{tg}"), V(cosT[:, cols], "cosT"), V(sinT[:, cols], "sinT"), 512,
                          TA if i == 0 else TB, pb[0] if i == 0 else pb[4]))
        qk_post_multi(items, fillers=[lambda: tm_block(0), lambda: tm_block(1)])
        ps = pb[1]
        for hd, c0 in enumerate((C_QA, C_QB)):
            for k in range(16):
                S.mm(ps[:, hd * 128:(hd + 1) * 128], V(w1s[:, k, c0:c0 + 128], f"w1s_{k}"), V(xno[:, k, :], "xno"), start=(k == 0), stop=(k == 15))
        qk_post_multi([(ps[:, 0:256], pp1[:, 0:1], V(QTo[:].rearrange("p a b -> p (a b)"), "QTo"), cs2, sn2, 256, TA, pb[0])],
                      fillers=[lambda: tm_block(2), lambda: tm_block(3)])
        ps = pb[2]
        for k in range(16):
            S.mm(ps[:, 0:128], V(w1s[:, k, C_GQ:C_GQ + 128], f"w1s_{k}"), V(xno[:, k, :], "xno"), start=(k == 0), stop=(k == 15))
        S.act(gqo, ps[:, 0:128], AF.Copy, scale=128.0 ** -0.5)
        for k in range(16):
            S.mm(ps[0:16, :], V(w1s[:, k, C_LR:C_LR + 16], f"w1s_{k}"), V(xn[:, k, :], f"xn_{k}"), start=(k == 0), stop=(k == 15))
        S.copy("dve", glrT, ps[0:16, :])
        ps = pb[3]
        for k in range(16):
            S.mm(ps[:, 0:256], V(xno[:, k, :], "xno"), V(w1s[:, k, C_TM1 + 256:C_TM1 + 512], f"w1s_{k}"), start=(k == 0), stop=(k == 15))
        S.act(sgr, ps[:, 0:256], AF.Silu)
        if hg >= 1 or tg == ngroups - 1:
            issue_loads(hg, tg + 1)
        sbk = [pb[0], pb[2]]
        acc = [pb[1], pb[6]]
        rounds = [(hd, list(range(r0, r0 + 4))) for hd in range(2) for r0 in range(0, 4 * tg + 4, 4)]

        def emit_qk_exp(n):
            hd, kbs = rounds[n]
            pe2 = [pexp[(n % 2) * 2], pexp[(n % 2) * 2 + 1]]
            for i, kb in enumerate(kbs):
                for c in range(2):
                    csl = slice(64 * c, 64 * c + 64)
                    S.mm(sbk[c][:, i * 128:(i + 1) * 128], V(KT[csl, hd, kb * 128:(kb + 1) * 128], f"KT_{hd}_{kb // 4}"),
                         V(QTo[csl, hd, :], "QTo"))
            for c in range(2):
                S.act(pe2[c], sbk[c], AF.Exp, scale=0.125)
            if kbs[-1] == 4 * tg + 3:
                for c in range(2):
                    S.tt("pool", pe2[c], pe2[c], maskT, ALU.mult)

        def emit_pv(n):
            hd, kbs = rounds[n]
            pe2 = [pexp[(n % 2) * 2], pexp[(n % 2) * 2 + 1]]
            for i, kb in enumerate(kbs):
                for c in range(2):
                    S.mm(acc[c][:, 0:129], pe2[c][:, i * 128:(i + 1) * 128], V(Vaug[:, kb, hd, 0:129], f"Vaug_{kb // 4}"),
                         start=(kb == 0), stop=(kb == 4 * tg + 3))
            if kbs[-1] == 4 * tg + 3:
                dnb = dn[hd]
                S.recip(asm[:, 0:1], acc[0][:, 128:129])
                S.recip(asm[:, 1:2], acc[1][:, 128:129])
                S.tt("dve", asm[:, 2:3], asm[:, 1:2], neg_lam, ALU.mult)
                S.ts("dve", o1, acc[0][:, 0:128], asm[:, 0:1], ALU.mult)
                S.stt(dd, acc[1][:, 0:128], asm[:, 2:3], o1, ALU.mult, ALU.add)
                S.act(junk2, dd, AF.Square, accum=asm[:, 3:4])
                S.act(asm[:, 4:5], asm[:, 3:4], AF.Sqrt, scale=1.0 / 128, bias=eps_col)
                S.recip(asm[:, 5:6], asm[:, 4:5])
                S.stt(dnb, dd, asm[:, 5:6], subg_bc, ALU.mult, ALU.mult)

        todo = [0]

        def att_unit(slots_left=1):
            left = len(rounds) + 1 - todo[0]
            k = -(-left // max(slots_left, 1))
            for _ in range(min(k, left)):
                s_ = todo[0]
                todo[0] += 1
                if s_ >= 1:
                    emit_pv(s_ - 1)
                if s_ < len(rounds):
                    emit_qk_exp(s_)

        zb = [pb[7][:, tb * 128:(tb + 1) * 128] for tb in range(4)]
        rvb = [pb[1][:, tb * 128:(tb + 1) * 128] for tb in range(4)]
        for tb in range(4):
            tsl = slice(tb * 128, (tb + 1) * 128)
            S.mm(zb[tb], glrT[0:16, tsl], wa2, start=True, stop=False)
            S.mm(zb[tb], ones_f[0:1, 0:128], ba, start=False, stop=True)
        for tb in range(4):
            S.act(ez[tb], zb[tb], AF.Exp, scale=-1.0)
        for tb in range(4):
            S.act(lsp[tb], ez[tb], AF.Ln, bias=ones_f[:, 0:1])
        for tb in range(4):
            S.mm(rvb[tb], mstrict, lsp[tb])
        for tb in range(4):
            S.mm(pb[2][:, tb * 2:tb * 2 + 2], lsp[tb], chunkind)
        for tb in range(4):
            S.act(wexp[tb], rvb[tb], AF.Exp, scale=-1.0 / 16)
        S.act(dec, pb[2][:, 0:8], AF.Exp, scale=-1.0 / 16)
        for tb in range(4):
            S.tt("dve", kdec[tb], k_sb[tb], wexp[tb], ALU.mult)
        dslot = [V(pb[7].ap[:, 0:256], ["pb7a", "pb7"]), V(pb[3].ap[:, 0:256], ["pb3a", "pb3"]),
                 V(pb[7].ap[:, 256:512], ["pb7b", "pb7"]), V(pb[3].ap[:, 256:512], ["pb3b", "pb3"])]
        opsb = [pb[4][:, 0:256], pb[4][:, 256:512], pb[5][:, 0:256], pb[5][:, 256:512]]
        for c8 in range(8):
            tb, cc = c8 // 2, c8 % 2
            psl = slice(64 * cc, 64 * cc + 64)
            ds_ps = dslot[c8 % 4]
            S.mm(ds_ps, kdec[tb][psl, :], vg[tb][psl, :])
            S.stt(Sst2[(c8 + 1) % 2], Sst2[c8 % 2], dec[:, c8:c8 + 1], ds_ps, ALU.mult, ALU.add)
            S.copy("pool", Sbf[c8], Sst2[(c8 + 1) % 2])
            S.mm(opsb[tb][psl, :], gqo[:, 64 * cc:64 * cc + 64], Sbf[c8])
            att_unit(13 - c8)
        pick(osel, opsb)
        att_unit(5)
        S.act(junk, osel, AF.Square, accum=gsm[:, 0:1])
        att_unit(4)
        S.act(gsm[:, 1:2], gsm[:, 0:1], AF.Sqrt, scale=1.0 / 256, bias=eps_col)
        S.recip(gsm[:, 2:3], gsm[:, 1:2])
        att_unit(3)
        S.stt(osel, osel, gsm[:, 2:3], gog_bc, ALU.mult, ALU.mult)
        att_unit(2)
        S.tt("pool", osel, osel, sgr, ALU.mult)
        att_unit(1)
        for r in range(2):
            S.tr(pb[7][:, r * 128:(r + 1) * 128], osel[:, r * 128:(r + 1) * 128], ident)
            S.copy("act", mstage[2 + r][:, 0:128], pb[7][:, r * 128:(r + 1) * 128])
        while todo[0] <= len(rounds):
            att_unit()
        for hd in range(2):
            S.tr(sbk[0][:, hd * 128:(hd + 1) * 128], dn[hd], ident)
            S.copy("act", mstage[hd][:, 0:128], sbk[0][:, hd * 128:(hd + 1) * 128])
        ocols = slice(tg * 128, (tg + 1) * 128)
        for r in range(4):
            r0 = row_of(hg, r)
            S.dma("sp", V(t["mixT1"].ap[r0:r0 + 128, ocols], t["mixT1"].keys), mstage[r][:, 0:128])


def build_nc_phase1_only(ngroups=8):
    import contextlib
    nc = bass.Bass("TRN2", target_bir_lowering=False)
    t = {}
    for name, shape, dt in P1_INPUTS:
        t[name] = V(nc.dram_tensor(name, shape, dt, kind="ExternalInput").ap(), "dram_" + name)
    t["mixT1"] = V(nc.dram_tensor("mixT1", [512, 1024], BF16, kind="ExternalOutput").ap(), "dram_mixT1")
    S = Sched(nc)
    with contextlib.ExitStack() as es:
        build_phase1(nc, S, t, es, ngroups=ngroups)
        S.emit()
    return nc


_NC_CACHE = {}


def build_nc_fused(ngroups=8):
    import contextlib
    nc = bass.Bass("TRN2", target_bir_lowering=False)
    t1, t2 = {}, {}
    for name, shape, dt in P1_INPUTS:
        shape = list(shape)
        if name in ("w1", "wa2", "ba"):
            shape[0] = 4
        t1[name] = V(nc.dram_tensor(name, shape, dt, kind="ExternalInput").ap(), "dram_" + name)
    mixscr = V(nc.dram_tensor("mixscr", [2048, 1024], BF16, kind="Internal").ap(), "dram_mixscr")
    t1["mixT1"] = mixscr
    t1["xnscr"] = V(nc.dram_tensor("xnscr", [2048, 4096], BF16, kind="Internal").ap(), "dram_xnscr")
    t1["xnoscr"] = V(nc.dram_tensor("xnoscr", [128, 8, 16, 128], BF16, kind="Internal").ap(), "dram_xnoscr")
    for name, shape, dt in P2_INPUTS:
        if name == "mixT":
            continue
        t2[name] = V(nc.dram_tensor(name, shape, dt, kind="ExternalInput").ap(), "dram_" + name)
    t2["mixT"] = mixscr
    t2["outT"] = V(nc.dram_tensor("outT", [2048, 1024], F32, kind="ExternalOutput").ap(), "dram_outT")
    S = Sched(nc)

    def row_of(hg, r):
        return hg * 256 + r * 128 if r < 2 else 1024 + hg * 256 + (r - 2) * 128

    with contextlib.ExitStack() as es1:
        build_phase1(nc, S, t1, es1, ngroups=ngroups, npass=4, row_of=row_of)
    S.barrier()
    with contextlib.ExitStack() as es2:
        build_phase2(nc, S, t2, es2, mix_select=False)
    S.emit()
    return nc


def host_fused_inputs(inp, c):
    m = host_phase1_inputs(inp, c, groups=[0, 1, 2, 3])
    m.update(host_phase2_inputs(inp, c))
    return m


def kernel(**inp):
    inp = {k: np.asarray(v) for k, v in inp.items()}
    if "fused" not in _NC_CACHE:
        _NC_CACHE["fused"] = build_nc_fused()
    cores = list(range(8))
    res = run_bass_kernel_spmd(_NC_CACHE["fused"], [host_fused_inputs(inp, c) for c in cores], core_ids=cores)
    out = np.empty((2, 4096, 2048), np.float32)
    for c in cores:
        b, j = c // 4, c % 4
        out[b, own_tokens(c), :] = np.asarray(res.results[c]["outT"]).T
    return out
```

```python
import numpy as np
import ml_dtypes
import concourse.bass as bass
import concourse.mybir as mybir
from concourse.bass_utils import run_bass_kernel_spmd

F32 = mybir.dt.float32
BF16 = mybir.dt.bfloat16
I32 = mybir.dt.int32
AF = mybir.ActivationFunctionType
ALU = mybir.AluOpType
AX = mybir.AxisListType

EPS = 1e-6
NDMA_SLOTS = 16


class V:
    def __init__(self, ap, keys):
        self.ap = ap
        self.keys = tuple(keys) if isinstance(keys, (list, tuple)) else (keys,)

    def __getitem__(self, idx):
        return V(self.ap[idx], self.keys)

    def k(self, *keys):
        return V(self.ap, keys)


class Sched:
    ENG = ("pe", "act", "dve", "pool", "sp")

    def __init__(self, nc):
        self.nc = nc
        self.ops = []
        self.dma_rr = {"sp": 0, "pool": 0, "act": 0}

    def add(self, issue, fn, reads, writes, dma=False):
        if dma:
            s = self.dma_rr[issue]
            self.dma_rr[issue] = (s + 1) % NDMA_SLOTS
            stream = f"dma_{issue}_{s}"
        else:
            stream = issue
        rk, wk = [], []
        for v in reads:
            rk.extend(v.keys)
        for v in writes:
            wk.extend(v.keys)
        self.ops.append(dict(stream=stream, issue=issue, fn=fn, reads=rk, writes=wk, dma=dma))

    def barrier(self):
        self.ops.append(dict(barrier=True))

    def mm(self, out, lhsT, rhs, start=True, stop=True, extra_reads=()):
        self.add("pe", lambda e: e.matmul(out.ap, lhsT.ap, rhs.ap, start=start, stop=stop),
                 [lhsT, rhs, *extra_reads], [out])

    def tr(self, out, in_, ident):
        self.add("pe", lambda e: e.transpose(out.ap, in_.ap, ident.ap), [in_, ident], [out])

    def act(self, out, in_, func, scale=1.0, bias=0.0, accum=None):
        reads = [in_]
        if isinstance(scale, V):
            reads.append(scale)
        if isinstance(bias, V):
            reads.append(bias)
        writes = [out] + ([accum] if accum is not None else [])
        sc = scale.ap if isinstance(scale, V) else scale
        bi = bias.ap if isinstance(bias, V) else bias
        if accum is None:
            self.add("act", lambda e: e.activation(out.ap, in_.ap, func, bias=bi, scale=sc), reads, writes)
        else:
            self.add("act", lambda e: e.activation(out.ap, in_.ap, func, bias=bi, scale=sc, accum_out=accum.ap),
                     reads, writes)

    def tt(self, eng, out, a, b, op):
        self.add(eng, lambda e: e.tensor_tensor(out.ap, a.ap, b.ap, op), [a, b], [out])

    def ts(self, eng, out, a, s1, op0, s2=None, op1=None):
        reads = [a] + [s for s in (s1, s2) if isinstance(s, V)]
        x1 = s1.ap if isinstance(s1, V) else s1
        x2 = s2.ap if isinstance(s2, V) else s2
        if op1 is None:
            self.add(eng, lambda e: e.tensor_scalar(out.ap, a.ap, x1, None, op0), reads, [out])
        else:
            self.add(eng, lambda e: e.tensor_scalar(out.ap, a.ap, x1, x2, op0, op1), reads, [out])

    def stt(self, out, a, s, b, op0, op1):
        reads = [a, b] + ([s] if isinstance(s, V) else [])
        x = s.ap if isinstance(s, V) else s
        self.add("dve", lambda e: e.scalar_tensor_tensor(out.ap, a.ap, x, b.ap, op0, op1), reads, [out])

    def copy(self, eng, out, in_):
        if eng == "act":
            self.add("act", lambda e: e.copy(out.ap, in_.ap), [in_], [out])
        else:
            self.add(eng, lambda e: e.tensor_copy(out.ap, in_.ap), [in_], [out])

    def recip(self, out, in_):
        self.add("dve", lambda e: e.reciprocal(out.ap, in_.ap), [in_], [out])

    def memset(self, eng, out, val):
        self.add(eng, lambda e: e.memset(out.ap, val), [], [out])

    def dma(self, issue, out, in_):
        self.add(issue, lambda e: e.dma_start(out=out.ap, in_=in_.ap), [in_], [out], dma=True)

    def emit(self):
        nc = self.nc
        stream_pos = {}
        fence = {}
        ops = []
        for op in self.ops:
            if op.get("barrier"):
                fence = dict(stream_pos)
                continue
            p = stream_pos.get(op["stream"], 0) + 1
            stream_pos[op["stream"]] = p
            op["pos"] = p
            op["fence"] = fence
            ops.append(op)
        n = len(ops)
        last_w = {}
        readers = {}
        seen = {e: {} for e in self.ENG}
        for i, op in enumerate(ops):
            need = {}

            def want(j):
                o = ops[j]
                st = o["stream"]
                if st == "pe" and op["stream"] == "pe":
                    return
                if o["pos"] > need.get(st, 0):
                    need[st] = o["pos"]

            for k in op["reads"]:
                if k in last_w:
                    want(last_w[k])
            for k in op["writes"]:
                if k in last_w:
                    want(last_w[k])
                for r in readers.get(k, ()):
                    if r != i:
                        want(r)
            for st, p in op["fence"].items():
                if st == "pe" and op["stream"] == "pe":
                    continue
                if p > need.get(st, 0):
                    need[st] = p
            if op["dma"] and op["pos"] > 1:
                if op["pos"] - 1 > need.get(op["stream"], 0):
                    need[op["stream"]] = op["pos"] - 1
            sn = seen[op["issue"]]
            waits = []
            for st, p in need.items():
                if sn.get(st, 0) < p:
                    sn[st] = p
                    waits.append((st, p))
            op["waits"] = waits
            for k in op["reads"]:
                readers.setdefault(k, []).append(i)
            for k in op["writes"]:
                last_w[k] = i
                readers[k] = []
        by_stream = {}
        for i, op in enumerate(ops):
            by_stream.setdefault(op["stream"], []).append(i)
        signal = set()
        for op in ops:
            for st, p in op["waits"]:
                signal.add((st, p))
            if op["dma"]:
                signal.add((op["stream"], op["pos"]))
        for st, lst in by_stream.items():
            signal.add((st, ops[lst[-1]]["pos"]))
        rank = {}
        for st, lst in by_stream.items():
            r = 0
            for i in lst:
                if (st, ops[i]["pos"]) in signal:
                    r += 1
                    rank[(st, ops[i]["pos"])] = r
        finals = {st: max(v for (s, _), v in rank.items() if s == st) for st in by_stream}
        import contextlib
        with contextlib.ExitStack() as es:
            sems = {st: es.enter_context(nc.semaphore(f"s_{st}")) for st in by_stream}
            block = es.enter_context(nc.Block())
            handles = {"pe": nc.tensor, "act": nc.scalar, "dve": nc.vector, "pool": nc.gpsimd, "sp": nc.sync}

            def run_engine(ename):
                def body(_e):
                    eng = handles[ename]
                    for op in ops:
                        if op["issue"] != ename:
                            continue
                        for st, p in op["waits"]:
                            mult = 16 if st.startswith("dma_") else 1
                            eng.wait_ge(sems[st], rank[(st, p)] * mult)
                        ins = op["fn"](eng)
                        key = (op["stream"], op["pos"])
                        if key in rank:
                            ins.then_inc(sems[op["stream"]], 16 if op["dma"] else 1)
                    if ename == "sp":
                        for st, f in finals.items():
                            mult = 16 if st.startswith("dma_") else 1
                            eng.wait_ge(sems[st], f * mult)
                return body

            block.tensor(run_engine("pe"))
            block.scalar(run_engine("act"))
            block.vector(run_engine("dve"))
            block.gpsimd(run_engine("pool"))
            block.sync(run_engine("sp"))


def _alloc(es, nc, name, shape, dt):
    return es.enter_context(nc.sbuf_tensor("sb_" + name, list(shape), dt))


def rmsnorm_fm(S, src_fn, gcol_fn, dst_fn, nk, ntok, D, tmp, pbank, ones_bf, eps_col, f32_fn=None):
    for k in range(nk):
        sq = tmp["sq"][k % 2][:, 0:ntok]
        S.act(sq, src_fn(k), AF.Square)
        S.mm(pbank[:, 0:ntok], ones_bf, sq, start=(k == 0), stop=(k == nk - 1))
    rt = tmp["rt"][:, 0:ntok]
    S.act(rt, pbank[:, 0:ntok], AF.Sqrt, scale=1.0 / D, bias=eps_col)
    rb = tmp["rb"][:, 0:ntok]
    S.recip(rb, rt)
    for k in range(nk):
        if f32_fn is None:
            S.stt(dst_fn(k), src_fn(k), gcol_fn(k), rb, ALU.mult, ALU.mult)
        else:
            f32_fn(k, rb)


def build_phase2(nc, S, t, es_outer, mix_select=False):
    import contextlib
    es = es_outer
    NT = 1024
    hT = _alloc(es, nc, "hT", [128, 16, NT], F32)
    ones_bf = V(_alloc(es, nc, "ones_bf", [128, 128], BF16)[:], "ones_bf")
    ones_f = V(_alloc(es, nc, "ones_f", [128, 128], F32)[:], "ones_f")
    ident = V(_alloc(es, nc, "ident", [128, 128], F32)[:], "ident")
    eps_col = V(_alloc(es, nc, "eps_col", [128, 1], F32)[:], "eps_col")
    gvec = V(_alloc(es, nc, "gvec", [128, 48], F32)[:], "gvec")
    cg = V(_alloc(es, nc, "cg", [128, 8], F32)[:], "cg")
    tmp = {
        "sq": [V(_alloc(es, nc, f"sq{i}", [128, 512], BF16)[:], f"sq{i}") for i in range(2)],
        "rt": V(_alloc(es, nc, "rt", [128, 512], F32)[:], "rt"),
        "rb": V(_alloc(es, nc, "rb", [128, 512], F32)[:], "rb"),
    }
    pb = [V(es.enter_context(nc.psum_tensor(f"pb{i}", [128, 512], F32))[:], f"pb{i}") for i in range(8)]
    wpool = []

    def hv(k, half):
        return V(hT[:, k, half * 512:(half + 1) * 512], f"hT_{k}_{half}")

    S.memset("dve", ones_bf, 1.0)
    S.memset("dve", ones_f, 1.0)
    S.memset("dve", eps_col, EPS)
    S.dma("sp", ident, t["ident"])
    S.dma("sp", gvec, t["gvec"])
    S.dma("sp", cg, t["cg"])

    def wview(i, a, b):
        return V(wpool[i][:].rearrange("p (a b) -> p a b", a=a), f"wp{i}")

    state = {"wi": 0, "pbi": 0}

    def next_w():
        i = state["wi"]
        state["wi"] = (i + 1) % 2
        return i

    def next_pb(lo=4, n=4):
        i = state["pbi"]
        state["pbi"] = (i + 1) % n
        return pb[lo + i]

    def linear_fm(wdram, col0, ncols, in_fn, nk, ntoks, evac):
        for g in range(ncols // 512):
            wi = next_w()
            wv = wview(wi, nk, 512)
            S.dma("pool", wv, V(wdram.ap[:, col0 + g * 512: col0 + (g + 1) * 512].rearrange("(k p) n -> p k n", p=128), wdram.keys))
            for ocl in range(4):
                oc = g * 4 + ocl
                for ti, nt in enumerate(ntoks):
                    ps = next_pb()
                    for k in range(nk):
                        S.mm(ps[:, 0:nt], wv[:, k, ocl * 128:(ocl + 1) * 128], in_fn(k, ti), start=(k == 0), stop=(k == nk - 1))
                    evac(oc, ti, ps[:, 0:nt])

    with contextlib.ExitStack() as esA:
        wpool[:] = [_alloc(esA, nc, f"wpA{i}", [128, 8192], BF16) for i in range(2)]
        mixT = _alloc(esA, nc, "mixT", [128, 16, NT], BF16)
        if mix_select:
            stage = [V(_alloc(esA, nc, f"mstg{q}", [128, 1024], BF16)[:], f"mstg{q}") for q in range(4)]
            sel = V(_alloc(esA, nc, "sel", [128, 4], F32)[:], "sel")
            S.dma("sp", sel, t["sel"])
        for k in range(16):
            if mix_select:
                mk = V(mixT[:, k, :], f"mixT_{k}")
                for q in range(4):
                    S.dma("sp", stage[q], V(t["mixT"].ap[k * 128:(k + 1) * 128, q * 1024:(q + 1) * 1024], t["mixT"].keys))
                S.ts("dve", mk, stage[0], sel[:, 0:1], ALU.mult)
                for q in range(1, 4):
                    S.stt(mk, stage[q], sel[:, q:q + 1], mk, ALU.mult, ALU.add)
            else:
                S.dma("sp", V(mixT[:, k, :], f"mixT_{k}"), V(t["mixT"].ap[k * 128:(k + 1) * 128, :], t["mixT"].keys))
            for half in range(2):
                S.dma("sp", hv(k, half), V(t["xT2"].ap[k * 128:(k + 1) * 128, half * 512:(half + 1) * 512], t["xT2"].keys))

        def evacA(oc, ti, ps):
            S.tt("dve", hv(oc, ti), ps, hv(oc, ti), ALU.add)

        linear_fm(t["w_out"], 0, 2048, lambda k, ti: V(mixT[:, k, ti * 512:(ti + 1) * 512], f"mixT_{k}"), 16, [512, 512], evacA)
    S.barrier()

    with contextlib.ExitStack() as esB:
        wpool[:] = [_alloc(esB, nc, f"wpB{i}", [128, 8192], BF16) for i in range(2)]
        kT = _alloc(esB, nc, "kT", [128, 16, 256], BF16)
        v_sb = _alloc(esB, nc, "v_sb", [128, 2, 2048], BF16)
        with contextlib.ExitStack() as esB1:
            memT = _alloc(esB1, nc, "memT", [128, 16, 256], F32)
            memnT = _alloc(esB1, nc, "memnT", [128, 16, 256], BF16)
            kraw = _alloc(esB1, nc, "kraw", [128, 16, 256], F32)
            for k in range(16):
                S.dma("sp", V(memT[:, k, :], f"memT_{k}"), V(t["memT"].ap[k * 128:(k + 1) * 128, :], t["memT"].keys))
            rmsnorm_fm(S, lambda k: V(memT[:, k, :], f"memT_{k}"), lambda k: gvec[:, 16 + k:17 + k],
                       lambda k: V(memnT[:, k, :], f"memnT_{k}"), 16, 256, 2048.0, tmp, pb[0], ones_bf, eps_col)

            def evacK(oc, ti, ps):
                S.copy("act", V(kraw[:, oc, :], f"kraw_{oc}"), ps)

            linear_fm(t["w_ckv"], 0, 2048, lambda k, ti: V(memnT[:, k, :], f"memnT_{k}"), 16, [256], evacK)
            for h in range(4):
                rmsnorm_fm(S, lambda dc: V(kraw[:, h * 4 + dc, :], f"kraw_{h * 4 + dc}"), lambda dc: cg[:, 4 + dc:5 + dc],
                           lambda dc: V(kT[:, h * 4 + dc, :], f"kT_{h * 4 + dc}"), 4, 256, 512.0, tmp, pb[0], ones_bf, eps_col)
            for g in range(4):
                wi = next_w()
                wv = wview(wi, 16, 512)
                S.dma("pool", wv, V(t["w_ckv"].ap[:, 2048 + g * 512: 2048 + (g + 1) * 512].rearrange("(k p) n -> p k n", p=128), t["w_ckv"].keys))
                for mc in range(2):
                    ps = next_pb()
                    for k in range(16):
                        S.mm(ps, V(memnT[:, k, mc * 128:(mc + 1) * 128], f"memnT_{k}"), wv[:, k, :], start=(k == 0), stop=(k == 15))
                    S.copy("act", V(v_sb[:, mc, g * 512:(g + 1) * 512], f"v_sb_{mc}_{g}"), ps)
        S.barrier()
        xnh = _alloc(esB, nc, "xnh", [128, 16, 512], BF16)
        oTh = _alloc(esB, nc, "oTh", [128, 16, 512], BF16)
        qraw = _alloc(esB, nc, "qraw", [128, 4, 512], F32)
        qT = _alloc(esB, nc, "qT", [128, 4, 512], BF16)
        pT = _alloc(esB, nc, "pT", [128, 2, 512], BF16)
        rden = V(_alloc(esB, nc, "rden", [128, 512], F32)[:], "rden")
        for half in range(2):
            rmsnorm_fm(S, lambda k: hv(k, half), lambda k: gvec[:, k:k + 1],
                       lambda k: V(xnh[:, k, :], f"xnh_{k}"), 16, 512, 2048.0, tmp, pb[0], ones_bf, eps_col)
            for h in range(4):
                wi = next_w()
                wv = wview(wi, 16, 512)
                S.dma("pool", wv, V(t["w_cq"].ap[:, h * 512:(h + 1) * 512].rearrange("(k p) n -> p k n", p=128), t["w_cq"].keys))
                for dc in range(4):
                    ps = next_pb()
                    for k in range(16):
                        S.mm(ps, wv[:, k, dc * 128:(dc + 1) * 128], V(xnh[:, k, :], f"xnh_{k}"), start=(k == 0), stop=(k == 15))
                    S.copy("act", V(qraw[:, dc, :], f"qraw_{dc}"), ps)
                rmsnorm_fm(S, lambda dc: V(qraw[:, dc, :], f"qraw_{dc}"), lambda dc: cg[:, dc:dc + 1],
                           lambda dc: V(qT[:, dc, :], f"qT_{dc}"), 4, 512, 512.0, tmp, pb[0], ones_bf, eps_col)
                for mc in range(2):
                    ps = next_pb()
                    for dc in range(4):
                        S.mm(ps, V(kT[:, h * 4 + dc, mc * 128:(mc + 1) * 128], f"kT_{h * 4 + dc}"), V(qT[:, dc, :], f"qT_{dc}"),
                             start=(dc == 0), stop=(dc == 3))
                    S.act(V(pT[:, mc, :], f"pT_{mc}"), ps, AF.Exp, scale=512.0 ** -0.5)
                ps = pb[1]
                for mc in range(2):
                    S.mm(ps, ones_bf, V(pT[:, mc, :], f"pT_{mc}"), start=(mc == 0), stop=(mc == 1))
                S.recip(rden, ps)
                for dvc in range(4):
                    ps = next_pb()
                    for mc in range(2):
                        S.mm(ps, V(v_sb[:, mc, h * 512 + dvc * 128: h * 512 + (dvc + 1) * 128], f"v_sb_{mc}_{h}"), V(pT[:, mc, :], f"pT_{mc}"),
                             start=(mc == 0), stop=(mc == 1))
                    S.tt("dve", V(oTh[:, h * 4 + dvc, :], f"oTh_{h * 4 + dvc}"), ps, rden, ALU.mult)

            def evacO(oc, ti, ps):
                S.tt("dve", hv(oc, half), ps, hv(oc, half), ALU.add)

            linear_fm(t["w_co"], 0, 2048, lambda k, ti: V(oTh[:, k, :], f"oTh_{k}"), 16, [512], evacO)
    S.barrier()

    with contextlib.ExitStack() as esC:
        wpool[:] = [_alloc(esC, nc, f"wpC{i}", [128, 8192], BF16) for i in range(5)]
        xnT = _alloc(esC, nc, "xnT", [128, 16, NT], BF16)
        hid = _alloc(esC, nc, "hid", [128, 4, NT], BF16)
        xf = [V(_alloc(esC, nc, f"xf{i}", [128, 512], F32)[:], f"xf{i}") for i in range(2)]
        w_r = V(_alloc(esC, nc, "w_r", [128, 16, 36], F32)[:], "w_r")
        b_r = V(_alloc(esC, nc, "b_r", [1, 36], F32)[:], "b_r")
        GT = _alloc(esC, nc, "GT", [32, NT], F32)
        L = V(_alloc(esC, nc, "L", [128, 36], F32)[:], "L")
        sm = V(_alloc(esC, nc, "sm", [128, 16], F32)[:], "sm")
        gm = V(_alloc(esC, nc, "gm", [128, 4], F32)[:], "gm")
        pen = V(_alloc(esC, nc, "pen", [128, 4], F32)[:], "pen")
        ge = V(_alloc(esC, nc, "ge", [128, 4], F32)[:], "ge")
        elm = V(_alloc(esC, nc, "elm", [128, 32], F32)[:], "elm")
        elm2 = V(_alloc(esC, nc, "elm2", [128, 32], F32)[:], "elm2")
        mk1 = V(_alloc(esC, nc, "mk1", [128, 32], F32)[:], "mk1")
        mk2 = V(_alloc(esC, nc, "mk2", [128, 32], F32)[:], "mk2")
        G = V(_alloc(esC, nc, "G", [128, 32], F32)[:], "G")
        sg_t = xf
        gbc = [tmp["rt"], tmp["rb"]]
        S.dma("sp", w_r, V(t["w_r"].ap.rearrange("(k p) n -> p k n", p=128), t["w_r"].keys))
        S.dma("sp", b_r, t["b_r"])
        BIG = 1.0e30
        for half in range(2):
            def f32_fn(k, rb, half=half):
                x = xf[k % 2]
                S.stt(x, hv(k, half), gvec[:, 32 + k:33 + k], rb, ALU.mult, ALU.mult)
                for tb in range(4):
                    S.mm(pb[4 + tb][:, 0:36], x[:, tb * 128:(tb + 1) * 128], w_r[:, k, :], start=(k == 0), stop=False)
                S.copy("act", V(xnT[:, k, half * 512:(half + 1) * 512], f"xnT_{k}_{half}"), x)

            rmsnorm_fm(S, lambda k: hv(k, half), None, None, 16, 512, 2048.0, tmp, pb[0], ones_bf, eps_col, f32_fn=f32_fn)
            for tb in range(4):
                S.mm(pb[4 + tb][:, 0:36], ones_f[0:1, 0:128], b_r, start=False, stop=True)
                S.copy("dve", L, pb[4 + tb][:, 0:36])
                gmax, gsum, grw, m1, m2, d12, sgm, g1, g2, ngmax = [sm[:, i:i + 1] for i in range(10)]
                S.add("dve", lambda e, o=gmax, i=L: e.reduce_max(o.ap, i.ap[:, 0:4], AX.X), [L], [gmax])
                S.ts("dve", gm, L[:, 0:4], gmax, ALU.is_equal)
                S.ts("dve", ngmax, gmax, -1.0, ALU.mult)
                S.act(ge, L[:, 0:4], AF.Exp, bias=ngmax, accum=gsum)
                S.recip(grw, gsum)
                S.ts("dve", pen, gm, BIG, ALU.mult, -BIG, ALU.add)
                for g in range(4):
                    S.ts("dve", elm[:, g * 8:(g + 1) * 8], L[:, 4 + g * 8: 4 + (g + 1) * 8], pen[:, g:g + 1], ALU.add)
                S.add("dve", lambda e, o=m1, i=elm: e.reduce_max(o.ap, i.ap, AX.X), [elm], [m1])
                S.ts("dve", mk1, elm, m1, ALU.is_equal)
                S.stt(elm2, mk1, -BIG, elm, ALU.mult, ALU.add)
                S.add("dve", lambda e, o=m2, i=elm2: e.reduce_max(o.ap, i.ap, AX.X), [elm2], [m2])
                S.ts("dve", mk2, elm2, m2, ALU.is_equal)
                S.tt("dve", d12, m1, m2, ALU.subtract)
                S.act(sgm, d12, AF.Sigmoid)
                S.tt("dve", g1, sgm, grw, ALU.mult)
                S.tt("dve", g2, grw, g1, ALU.subtract)
                S.ts("dve", G, mk1, g1, ALU.mult)
                S.stt(G, mk2, g2, G, ALU.mult, ALU.add)
                S.tr(pb[3][0:32, 0:128], G, ident)
                col = half * 512 + tb * 128
                S.copy("dve", V(GT[:, col:col + 128], f"GT_{half}"), pb[3][0:32, 0:128])
        for e in range(32):
            wg = V(wpool[e % 2][:].rearrange("p (a b) -> p a b", a=16), f"wp{e % 2}")
            wu = V(wpool[2 + e % 2][:].rearrange("p (a b) -> p a b", a=16), f"wp{2 + e % 2}")
            wd = V(wpool[4][:].rearrange("p (a b) -> p a b", a=4), "wp4")
            S.dma("pool", wg, V(t["w_gate"].ap[e].rearrange("(k p) n -> p k n", p=128), t["w_gate"].keys))
            S.dma("pool", wu, V(t["w_up"].ap[e].rearrange("(k p) n -> p k n", p=128), t["w_up"].keys))
            S.dma("pool", wd, V(t["w_down"].ap[e].rearrange("(k p) n -> p k n", p=128), t["w_down"].keys))
            for half in range(2):
                gb = gbc[half]
                S.mm(pb[1], V(ident.ap[0:32, e:e + 1].broadcast_to([32, 128]), ident.keys), V(GT[:, half * 512:(half + 1) * 512], f"GT_{half}"))
                S.copy("act", gb, pb[1])
                for fc in range(4):
                    pg, pu = pb[2], pb[3]
                    for k in range(16):
                        S.mm(pg, wg[:, k, fc * 128:(fc + 1) * 128], V(xnT[:, k, half * 512:(half + 1) * 512], f"xnT_{k}_{half}"),
                             start=(k == 0), stop=(k == 15))
                    for k in range(16):
                        S.mm(pu, wu[:, k, fc * 128:(fc + 1) * 128], V(xnT[:, k, half * 512:(half + 1) * 512], f"xnT_{k}_{half}"),
                             start=(k == 0), stop=(k == 15))
                    sg = sg_t[fc % 2]
                    S.act(sg, pg, AF.Silu)
                    S.tt("pool", sg, sg, gb, ALU.mult)
                    S.tt("dve", V(hid[:, fc, half * 512:(half + 1) * 512], f"hid_{fc}_{half}"), pu, sg, ALU.mult)
                for oc in range(16):
                    ps = next_pb()
                    for fc in range(4):
                        S.mm(ps, wd[:, fc, oc * 128:(oc + 1) * 128], V(hid[:, fc, half * 512:(half + 1) * 512], f"hid_{fc}_{half}"),
                             start=(fc == 0), stop=(fc == 3))
                    S.tt("dve", hv(oc, half), ps, hv(oc, half), ALU.add)
        for k in range(16):
            for half in range(2):
                S.dma("sp", V(t["outT"].ap[k * 128:(k + 1) * 128, half * 512:(half + 1) * 512], t["outT"].keys), hv(k, half))


P2_INPUTS = [
    ("mixT", [2048, 1024], BF16), ("xT2", [2048, 1024], F32), ("memT", [2048, 256], F32),
    ("w_out", [2048, 2048], F32), ("w_cq", [2048, 2048], F32), ("w_co", [2048, 2048], F32),
    ("w_ckv", [2048, 4096], F32), ("gvec", [128, 48], F32), ("cg", [128, 8], F32), ("ident", [128, 128], F32),
    ("w_r", [2048, 36], F32), ("b_r", [1, 36], F32),
    ("w_gate", [32, 2048, 512], F32), ("w_up", [32, 2048, 512], F32), ("w_down", [32, 512, 2048], F32),
]


def host_phase2_inputs(inp, c):
    b, j = c // 4, c % 4
    tok = own_tokens(c)

    def pcol(v):
        return np.ascontiguousarray(v.reshape(-1, 128).T)

    gvec = np.concatenate([pcol(inp["g_cross"][0]), pcol(inp["g_mem"][0]), pcol(inp["g_ffn"][0])], axis=1)
    cg = np.concatenate([pcol(inp["cq_norm_g"][0]), pcol(inp["ck_norm_g"][0])], axis=1)
    return {
        "xT2": np.ascontiguousarray(inp["x"][b, tok, :].T),
        "memT": np.ascontiguousarray(inp["mem"][b].T),
        "w_out": inp["w_out"][0], "w_cq": inp["w_cq"][0], "w_co": inp["w_co"][0], "w_ckv": inp["w_ckv"][0],
        "gvec": np.ascontiguousarray(gvec, dtype=np.float32), "cg": np.ascontiguousarray(cg, dtype=np.float32),
        "ident": np.eye(128, dtype=np.float32),
        "w_r": np.ascontiguousarray(np.concatenate([inp["w_router_grp"][0], inp["w_router_exp"][0]], axis=1)),
        "b_r": np.ascontiguousarray(np.concatenate([inp["b_router_grp"][0], inp["b_router_exp"][0]])[None, :]),
        "w_gate": inp["w_gate"][0], "w_up": inp["w_up"][0], "w_down": inp["w_down"][0],
    }


def build_nc_phase2_only():
    import contextlib
    nc = bass.Bass("TRN2", target_bir_lowering=False)
    t = {}
    for name, shape, dt in P2_INPUTS:
        t[name] = V(nc.dram_tensor(name, shape, dt, kind="ExternalInput").ap(), "dram_" + name)
    t["outT"] = V(nc.dram_tensor("outT", [2048, 1024], F32, kind="ExternalOutput").ap(), "dram_outT")
    S = Sched(nc)
    with contextlib.ExitStack() as es:
        build_phase2(nc, S, t, es)
        S.emit()
    return nc


P1_INPUTS = [
    ("xT1", [2048, 4096], F32), ("w1", [1, 2048, 1552], F32), ("pp1", [128, 8], F32), ("pos", [1, 4096], I32),
    ("g1", [128, 16], F32), ("lam4", [1, 256], F32), ("subg", [1, 128], F32), ("gog", [1, 256], F32),
    ("wa2", [1, 16, 128], F32), ("ba", [1, 1, 128], F32), ("ident1", [128, 128], F32), ("mstrict", [128, 128], F32),
    ("chunkind", [128, 2], F32), ("protT", [128, 128], F32), ("sel4", [128, 4], F32), ("maskT", [128, 512], F32),
]
_DBG_STOP = [99]
C_QA, C_QB, C_KA, C_KB, C_GQ, C_LR, C_TM1, C_TM2, NW1 = 0, 128, 256, 384, 512, 640, 656, 1168, 1552


def host_w1(inp, j):
    w = inp["w_in"][0]
    hA, hB = 2 * j, 2 * j + 1
    cols = []
    cols += [w[:, hA * 128:(hA + 1) * 128], w[:, hB * 128:(hB + 1) * 128]]
    cols += [w[:, 1024 + hA * 128:1024 + (hA + 1) * 128], w[:, 1024 + hB * 128:1024 + (hB + 1) * 128]]
    cols += [w[:, 3072 + j * 128:3072 + (j + 1) * 128]]
    cols += [w[:, 5120:5136]]
    cols += [w[:, 2048 + hA * 128:2048 + (hA + 1) * 128], w[:, 2048 + hB * 128:2048 + (hB + 1) * 128]]
    cols += [w[:, 5136 + j * 256:5136 + (j + 1) * 256]]
    cols += [w[:, 3584 + j * 128:3584 + (j + 1) * 128]]
    cols += [w[:, 4096 + j * 256:4096 + (j + 1) * 256]]
    w1 = np.ascontiguousarray(np.concatenate(cols, axis=1))
    assert w1.shape == (2048, NW1)
    return w1


def host_phase1_inputs(inp, c, groups=None):
    b, j = c // 4, c % 4
    groups = [j] if groups is None else groups
    w1 = np.stack([host_w1(inp, g) for g in groups], axis=0)
    pp1 = np.zeros((128, 8), np.float32)
    pp1[:, 0] = np.tile(inp["q_norm_g"][0], 2)
    pp1[:, 2] = np.tile(inp["k_norm_g"][0], 2)
    half = 32
    freq = (np.float32(10000.0) ** (-np.arange(half, dtype=np.float32) / np.float32(half))).astype(np.float32)
    pp1[:, 4] = np.tile(freq, 4)
    lam4 = np.concatenate([inp["lambda_q1"][0], inp["lambda_k1"][0], inp["lambda_q2"][0], inp["lambda_k2"][0]])[None, :]
    l = np.arange(128)
    mstrict = ((l[:, None] > l[None, :]) & ((l[:, None] // 64) == (l[None, :] // 64))).astype(np.float32)
    chunkind = np.stack([(l < 64), (l >= 64)], axis=1).astype(np.float32)
    protT = np.zeros((128, 128), np.float32)
    for m in range(128):
        if (m % 64) < 32:
            protT[m + 32, m] = -1.0
        else:
            protT[m - 32, m] = 1.0
    return {
        "xT1": np.ascontiguousarray(inp["x"][b].T), "w1": w1, "pp1": pp1,
        "pos": np.ascontiguousarray(inp["positions"][b][None, :].astype(np.int32)),
        "g1": np.ascontiguousarray(inp["g_attn"][0].reshape(16, 128).T),
        "lam4": np.ascontiguousarray(lam4.astype(np.float32)),
        "subg": np.ascontiguousarray(inp["diff_subln_g"][0][None, :]),
        "gog": np.ascontiguousarray(inp["gla_out_g"][0][None, :]),
        "wa2": np.ascontiguousarray(np.stack([inp["gla_w_a2"][0][:, g * 128:(g + 1) * 128] for g in groups], axis=0)),
        "ba": np.ascontiguousarray(np.stack([inp["gla_b_a"][0][None, g * 128:(g + 1) * 128] for g in groups], axis=0)),
        "ident1": np.eye(128, dtype=np.float32), "mstrict": mstrict, "chunkind": chunkind, "protT": protT,
        "sel4": own_sel(c), "maskT": own_mask(c),
    }


def own_sel(c):
    sel = np.zeros((128, 4), np.float32)
    sel[:, c % 4] = 1.0
    return sel


def own_mask(c):
    j = c % 4
    M = np.zeros((128, 4, 128), np.float32)
    for m in range(4):
        if m < j:
            M[:, m, :] = 1.0
        elif m == j:
            M[:, m, :] = 1.0
            M[64:128, m, 0:64] = 0.0
    return np.ascontiguousarray(M.reshape(128, 512))


def own_tokens(c):
    j = c % 4
    return np.concatenate([np.arange((4 * i + j) * 128, (4 * i + j + 1) * 128) for i in range(8)])


def build_phase1(nc, S, t, es, ngroups=8, npass=1, row_of=None):
    import math
    NTOK = 4096
    LAM_INIT = 0.8 - 0.6 * math.exp(-0.3 * 0)
    PI = math.pi

    def A(name, shape, dt):
        return _alloc(es, nc, "p1" + name, shape, dt)

    w1s = A("w1s", [128, 16, NW1], BF16)
    KT = A("KT", [128, 2, NTOK], BF16)
    QTo = A("QTo", [128, 2, 128], BF16)
    Vaug = A("Vaug", [128, 32, 2, 130], BF16)
    cosT = A("cosT", [128, NTOK], BF16)
    sinT = A("sinT", [128, NTOK], BF16)
    gqo = V(A("gqo", [128, 128], BF16)[:], "gqo")
    xno = A("xno", [128, 16, 128], BF16)
    cs2all = A("cs2all", [128, 8, 256], BF16)
    sn2all = A("sn2all", [128, 8, 256], BF16)
    maskT = V(A("maskT", [128, 512], BF16)[:], "maskT")
    sel4 = V(A("sel4", [128, 4], F32)[:], "sel4")
    osel = V(A("osel", [128, 256], F32)[:], "osel")
    xg = A("xg", [128, 16, 512], F32)
    xn = A("xn", [128, 16, 512], BF16)
    ones_bf = V(A("ones_bf", [128, 128], BF16)[:], "ones_bf")
    onesblk = V(A("onesblk", [128, 128], BF16)[:], "onesblk")
    protT = V(A("protT", [128, 128], BF16)[:], "protT")
    ident = V(A("ident", [128, 128], F32)[:], "ident")
    mstrict = V(A("mstrict", [128, 128], F32)[:], "mstrict")
    chunkind = V(A("chunkind", [128, 2], F32)[:], "chunkind")
    ones_f = V(A("ones_f", [128, 128], F32)[:], "ones_f")
    eps_col = V(A("eps_col", [128, 1], F32)[:], "eps_col")
    pp1 = V(A("pp1", [128, 8], F32)[:], "pp1")
    g1 = V(A("g1", [128, 16], F32)[:], "g1")
    lamt = V(A("lamt", [128, 256], F32)[:], "lamt")
    lamv = V(A("lamv", [128, 8], F32)[:], "lamv")
    subg_bc = V(A("subg_bc", [128, 128], F32)[:], "subg_bc")
    gog_bc = V(A("gog_bc", [128, 256], F32)[:], "gog_bc")
    wa2 = V(A("wa2", [16, 128], F32)[:], "wa2")
    ba = V(A("ba", [1, 128], F32)[:], "ba")
    tmp = {
        "sq": [V(A(f"sq{i}", [128, 512], BF16)[:], f"sq{i}") for i in range(2)],
        "rt": V(A("rt", [128, 512], F32)[:], "rt"),
        "rb": V(A("rb", [128, 512], F32)[:], "rb"),
    }
    qn_bf = V(A("qn_bf", [128, 512], BF16)[:], "qn_bf")
    t1 = V(A("t1", [128, 512], F32)[:], "t1")
    t2 = V(A("t2", [128, 512], F32)[:], "t2")
    rtB = V(A("rtB", [128, 512], F32)[:], "rtB")
    rbB = V(A("rbB", [128, 512], F32)[:], "rbB")
    qnB = V(A("qnB", [128, 512], BF16)[:], "qnB")
    t1B = V(A("t1B", [128, 512], F32)[:], "t1B")
    t2B = V(A("t2B", [128, 512], F32)[:], "t2B")
    pexp = [V(A(f"pexp{i}", [128, 512], BF16)[:], f"pexp{i}") for i in range(2)]
    glrT = V(A("glrT", [16, 512], F32)[:], "glrT")
    pexp = pexp + [V(A(f"pexp{i}", [128, 512], BF16)[:], f"pexp{i}") for i in (2, 3)]
    ez = [V(t1.ap[:, i * 128:(i + 1) * 128], "t1") for i in range(4)]
    wexp = ez
    lsp = [V(t2.ap[:, i * 128:(i + 1) * 128], "t2") for i in range(4)]
    k_sb = [V(A(f"k_sb{i}", [128, 128], F32)[:], f"k_sb{i}") for i in range(2)] + \
           [V(rtB.ap[:, i * 128:(i + 1) * 128], "rtB") for i in range(2)]
    kdec = [V(tmp["sq"][0].ap[:, i * 128:(i + 1) * 128], "sq0") for i in range(4)]
    vg = [V(A(f"vg{i}", [128, 256], BF16)[:], f"vg{i}") for i in range(2)] + \
         [V(qnB.ap[:, 0:256], "qnB"), V(qnB.ap[:, 256:512], "qnB")]
    go = [V(tmp["rb"].ap[:, 0:256], "rb"), V(tmp["rb"].ap[:, 256:512], "rb")]
    sgr = V(A("sgr", [128, 256], F32)[:], "sgr")
    Sst2 = [V(A(f"Sst{i}", [128, 256], F32)[:], f"Sst{i}") for i in range(2)]
    Sst = Sst2[0]
    Sbf = [V(A(f"Sbf{i}", [128, 256], BF16)[:], f"Sbf{i}") for i in range(8)]
    dec = V(A("dec", [128, 8], F32)[:], "dec")
    junk = V(t2.ap[:, 0:256], "t2")
    junk2 = V(A("junk2", [128, 128], F32)[:], "junk2")
    gsm = V(A("gsm", [128, 16], F32)[:], "gsm")
    o1 = V(A("o1", [128, 128], F32)[:], "o1")
    dd = V(A("dd", [128, 128], F32)[:], "dd")
    dn = [V(A(f"dn{i}", [128, 128], F32)[:], f"dn{i}") for i in range(2)]
    asm = V(A("asm", [128, 8], F32)[:], "asm")
    mstage = [V(A(f"mstage{r}", [128, 512], BF16)[:], f"mstage{r}") for r in range(4)]
    pb = [V(es.enter_context(nc.psum_tensor(f"p1pb{i}", [128, 512], F32))[:], f"pb{i}") for i in range(8)]

    if row_of is None:
        row_of = lambda hg, r: r * 128
    S.dma("pool", protT, t["protT"])
    S.dma("pool", maskT, t["maskT"])
    for dst, src in [(ident, "ident1"), (mstrict, "mstrict"), (chunkind, "chunkind"), (pp1, "pp1"), (g1, "g1"), (sel4, "sel4")]:
        S.dma("sp", dst, t[src])
    S.dma("sp", lamt, V(t["lam4"].ap.partition_broadcast(128), t["lam4"].keys))
    S.dma("sp", subg_bc, V(t["subg"].ap.partition_broadcast(128), t["subg"].keys))
    S.dma("sp", gog_bc, V(t["gog"].ap.partition_broadcast(128), t["gog"].keys))
    S.memset("dve", ones_bf, 1.0)
    S.memset("dve", ones_f, 1.0)
    S.memset("dve", eps_col, EPS)
    S.memset("dve", onesblk, 0.0)
    S.memset("dve", onesblk[0:64, 0:64], 1.0)
    S.memset("dve", onesblk[64:128, 64:128], 1.0)
    S.memset("pool", V(Vaug[:], [f"Vaug_{g}" for g in range(8)]), 1.0)
    for i in range(2):
        S.tt("dve", junk2[:, 0:64], lamt[:, i * 128:i * 128 + 64], lamt[:, i * 128 + 64:i * 128 + 128], ALU.mult)
        S.add("dve", lambda e, o=lamv[:, i:i + 1], x=junk2: e.reduce_sum(o.ap, x.ap[:, 0:64], AX.X), [junk2], [lamv])
        S.act(lamv[:, 2 + i:3 + i], lamv[:, i:i + 1], AF.Exp)
    S.tt("dve", lamv[:, 4:5], lamv[:, 2:3], lamv[:, 3:4], ALU.subtract)
    S.ts("dve", lamv[:, 5:6], lamv[:, 4:5], LAM_INIT, ALU.add, -1.0, ALU.mult)
    neg_lam = lamv[:, 5:6]
    S.ts("dve", subg_bc, subg_bc, 1.0 - LAM_INIT, ALU.mult)
    xgf = xg[:].rearrange("p a b -> p (a b)")
    posi = V(xgf[:, 0:4096].bitcast(I32), "xg_tab0")
    angf = V(xgf[:, 4096:8192], "xg_tab1")
    kf = V(xgf[:, 0:4096], "xg_tab0")
    ki = V(xgf[:, 0:4096].bitcast(I32), "xg_tab0")
    S.dma("sp", posi, V(t["pos"].ap.partition_broadcast(128), t["pos"].keys))
    S.copy("dve", angf, posi)
    S.ts("dve", angf, angf, pp1[:, 4:5], ALU.mult)
    S.ts("dve", t1.k("xg_tab0")[:, 0:1], angf[:, 0:1], 1.0, ALU.mult)
    for c0 in range(0, 4096, 2048):
        sl = slice(c0, c0 + 2048)
        S.ts("dve", kf[:, sl], angf[:, sl], 1.0 / (2 * PI), ALU.mult)
        S.copy("dve", ki[:, sl], kf[:, sl])
        S.copy("dve", kf[:, sl], ki[:, sl])
        C1 = 6.28125
        C2 = 2 * PI - C1
        S.stt(angf[:, sl], kf[:, sl], -C1, angf[:, sl], ALU.mult, ALU.add)
        S.stt(angf[:, sl], kf[:, sl], -C2, angf[:, sl], ALU.mult, ALU.add)
        msk = V(xn[:].rearrange("p a b -> p (a b)").bitcast(F32)[:, 0:2048], "xn_tab")
        PIS = 3.1415925

        def wrap(dst, src, shift):
            S.ts("dve", dst, src, shift, ALU.add)
            S.ts("dve", msk, dst, PI, ALU.is_gt)
            S.stt(dst, msk, -2 * PI, dst, ALU.mult, ALU.add)
            S.ts("dve", msk, dst, -PI, ALU.is_lt)
            S.stt(dst, msk, 2 * PI, dst, ALU.mult, ALU.add)
            S.ts("dve", dst, dst, PIS, ALU.min, -PIS, ALU.max)

        wrap(kf[:, sl], angf[:, sl], 0.0)
        S.act(V(sinT[:, sl], "sinT"), kf[:, sl], AF.Sin)
        wrap(kf[:, sl], angf[:, sl], PI / 2)
        S.act(V(cosT[:, sl], "cosT"), kf[:, sl], AF.Sin)
    S.barrier()

    loaded = set()

    def issue_loads(hg, tg):
        if tg >= ngroups:
            hg, tg = hg + 1, 0
        if hg >= npass or (hg, tg) in loaded:
            return
        loaded.add((hg, tg))
        c_ = slice(tg * 512, (tg + 1) * 512)
        if hg == 0:
            for k in range(16):
                S.dma("sp", V(xg[:, k, :], f"xg_{k}"), V(t["xT1"].ap[k * 128:(k + 1) * 128, c_], t["xT1"].keys))
        else:
            S.dma("sp", V(xno[:], "xno"), V(t["xnoscr"].ap[:, tg], f"xnoscr_{tg}"))
            for k in range(16):
                S.dma("sp", V(xn[:, k, :], f"xn_{k}"), V(t["xnscr"].ap[k * 128:(k + 1) * 128, c_], f"xnscr_{tg}"))

    for hg, tg in [(a, b_) for a in range(npass) for b_ in range(ngroups)]:
        cols = slice(tg * 512, (tg + 1) * 512)
        if tg == 0:
            for k in range(16):
                S.dma("pool", V(w1s[:, k, :], f"w1s_{k}"), V(t["w1"].ap[hg, k * 128:(k + 1) * 128, :], t["w1"].keys))
            S.dma("sp", wa2, V(t["wa2"].ap[hg], t["wa2"].keys))
            S.dma("sp", ba, V(t["ba"].ap[hg], t["ba"].keys))
            S.memset("dve", Sst, 0.0)
        def pick(dst, srcs):
            S.ts("dve", dst, srcs[0], sel4[:, 0:1], ALU.mult)
            for q in range(1, 4):
                S.stt(dst, srcs[q], sel4[:, q:q + 1], dst, ALU.mult, ALU.add)

        xn_all = [f"xn_{k}" for k in range(16)]
        gc0 = tg * 512
        cs2 = V(cs2all[:, tg, :], f"cs2_{tg}")
        sn2 = V(sn2all[:, tg, :], f"sn2_{tg}")
        issue_loads(hg, tg)
        if hg == 0:
            rmsnorm_fm(S, lambda k: V(xg[:, k, :], f"xg_{k}"), lambda k: g1[:, k:k + 1],
                       lambda k: V(xn[:, k, :], f"xn_{k}"), 16, 512, 2048.0, tmp, pb[0], ones_bf, eps_col)
            if tg + 1 < ngroups:
                issue_loads(0, tg + 1)
            pick(V(xno[:], "xno"), [V(xn[:, :, q * 128:(q + 1) * 128], xn_all) for q in range(4)])
            for tab, dst2 in ((cosT, cs2), (sinT, sn2)):
                nm = "cosT" if tab is cosT else "sinT"
                pick(dst2[:, 0:128], [V(tab[:, gc0 + q * 128:gc0 + (q + 1) * 128], nm) for q in range(4)])
                S.copy("pool", dst2[:, 128:256], dst2[:, 0:128])
            if npass > 1:
                for k in range(16):
                    S.dma("sp", V(t["xnscr"].ap[k * 128:(k + 1) * 128, cols], f"xnscr_{tg}"), V(xn[:, k, :], f"xn_{k}"))
                S.dma("sp", V(t["xnoscr"].ap[:, tg], f"xnoscr_{tg}"), V(xno[:], "xno"))

        def qk_post_multi(items, fillers=()):
            fillers = list(fillers)

            def fill():
                if fillers:
                    fillers.pop(0)()

            for ps, gcol, dst, cos_v, sin_v, w, T, nb in items:
                S.act(T["sq"][:, 0:w], ps, AF.Square)
            fill()
            for ps, gcol, dst, cos_v, sin_v, w, T, nb in items:
                S.mm(nb[:, 0:w], onesblk, T["sq"][:, 0:w])
            for ps, gcol, dst, cos_v, sin_v, w, T, nb in items:
                S.act(T["rt"][:, 0:w], nb[:, 0:w], AF.Sqrt, scale=1.0 / 64, bias=eps_col)
            for ps, gcol, dst, cos_v, sin_v, w, T, nb in items:
                S.recip(T["rb"][:, 0:w], T["rt"][:, 0:w])
            for ps, gcol, dst, cos_v, sin_v, w, T, nb in items:
                S.stt(T["qn"][:, 0:w], ps, gcol, T["rb"][:, 0:w], ALU.mult, ALU.mult)
            fill()
            for ps, gcol, dst, cos_v, sin_v, w, T, nb in items:
                S.mm(nb[:, 0:w], protT, T["qn"][:, 0:w])
            for ps, gcol, dst, cos_v, sin_v, w, T, nb in items:
                S.tt("dve", T["t2"][:, 0:w], nb[:, 0:w], sin_v, ALU.mult)
                S.tt("pool", T["t1"][:, 0:w], T["qn"][:, 0:w], cos_v, ALU.mult)
            for ps, gcol, dst, cos_v, sin_v, w, T, nb in items:
                S.tt("pool", dst, T["t1"][:, 0:w], T["t2"][:, 0:w], ALU.add)

        def tm_block(tb):
            blk = tg * 4 + tb
            tsl = slice(tb * 128, (tb + 1) * 128)
            ps = pb[3]
            for k in range(16):
                S.mm(ps[:, 0:256], V(xn[:, k, tsl], f"xn_{k}"), V(w1s[:, k, C_TM1:C_TM1 + 256], f"w1s_{k}"), start=(k == 0), stop=(k == 15))
            for h2 in range(2):
                S.copy("act", V(Vaug[:, blk, h2, 0:128], f"Vaug_{tg}"), ps[:, h2 * 128:(h2 + 1) * 128])
            ps = pb[7]
            for k in range(16):
                S.mm(ps[:, 0:384], V(xn[:, k, tsl], f"xn_{k}"), V(w1s[:, k, C_TM2:C_TM2 + 384], f"w1s_{k}"), start=(k == 0), stop=(k == 15))
            S.copy("dve", k_sb[tb], ps[:, 0:128])
            S.copy("dve", vg[tb], ps[:, 128:384])

        TA = {"sq": tmp["sq"][0], "rt": tmp["rt"], "rb": tmp["rb"], "qn": qn_bf, "t1": t1, "t2": t2}
        TB = {"sq": tmp["sq"][1], "rt": rtB, "rb": rbB, "qn": qnB, "t1": t1B, "t2": t2B}
        items = []
        for i, (c0, hd) in enumerate([(C_KA, 0), (C_KB, 1)]):
            ps = pb[1 + (i % 2)]
            for k in range(16):
                S.mm(ps, V(w1s[:, k, c0:c0 + 128], f"w1s_{k}"), V(xn[:, k, :], f"xn_{k}"), start=(k == 0), stop=(k == 15))
            items.append((ps, pp1[:, 2:3], V(KT[:, hd, cols], f"KT_{hd}_{tg}"), V(cosT[:, cols], "cosT"), V(sinT[:, cols], "sinT"), 512,
                          TA if i == 0 else TB, pb[0] if i == 0 else pb[4]))
        qk_post_multi(items, fillers=[lambda: tm_block(0), lambda: tm_block(1)])
        ps = pb[1]
        for hd, c0 in enumerate((C_QA, C_QB)):
            for k in range(16):
                S.mm(ps[:, hd * 128:(hd + 1) * 128], V(w1s[:, k, c0:c0 + 128], f"w1s_{k}"), V(xno[:, k, :], "xno"), start=(k == 0), stop=(k == 15))
        qk_post_multi([(ps[:, 0:256], pp1[:, 0:1], V(QTo[:].rearrange("p a b -> p (a b)"), "QTo"), cs2, sn2, 256, TA, pb[0])],
                      fillers=[lambda: tm_block(2), lambda: tm_block(3)])
        ps = pb[2]
        for k in range(16):
            S.mm(ps[:, 0:128], V(w1s[:, k, C_GQ:C_GQ + 128], f"w1s_{k}"), V(xno[:, k, :], "xno"), start=(k == 0), stop=(k == 15))
        S.act(gqo, ps[:, 0:128], AF.Copy, scale=128.0 ** -0.5)
        for k in range(16):
            S.mm(ps[0:16, :], V(w1s[:, k, C_LR:C_LR + 16], f"w1s_{k}"), V(xn[:, k, :], f"xn_{k}"), start=(k == 0), stop=(k == 15))
        S.copy("dve", glrT, ps[0:16, :])
        ps = pb[3]
        for k in range(16):
            S.mm(ps[:, 0:256], V(xno[:, k, :], "xno"), V(w1s[:, k, C_TM1 + 256:C_TM1 + 512], f"w1s_{k}"), start=(k == 0), stop=(k == 15))
        S.act(sgr, ps[:, 0:256], AF.Silu)
        if hg >= 1 or tg == ngroups - 1:
            issue_loads(hg, tg + 1)
        sbk = [pb[0], pb[2]]
        acc = [pb[1], pb[6]]
        rounds = [(hd, list(range(r0, r0 + 4))) for hd in range(2) for r0 in range(0, 4 * tg + 4, 4)]

        def emit_qk_exp(n):
            hd, kbs = rounds[n]
            pe2 = [pexp[(n % 2) * 2], pexp[(n % 2) * 2 + 1]]
            for i, kb in enumerate(kbs):
                for c in range(2):
                    csl = slice(64 * c, 64 * c + 64)
                    S.mm(sbk[c][:, i * 128:(i + 1) * 128], V(KT[csl, hd, kb * 128:(kb + 1) * 128], f"KT_{hd}_{kb // 4}"),
                         V(QTo[csl, hd, :], "QTo"))
            for c in range(2):
                S.act(pe2[c], sbk[c], AF.Exp, scale=0.125)
            if kbs[-1] == 4 * tg + 3:
                for c in range(2):
                    S.tt("pool", pe2[c], pe2[c], maskT, ALU.mult)

        def emit_pv(n):
            hd, kbs = rounds[n]
            pe2 = [pexp[(n % 2) * 2], pexp[(n % 2) * 2 + 1]]
            for i, kb in enumerate(kbs):
                for c in range(2):
                    S.mm(acc[c][:, 0:129], pe2[c][:, i * 128:(i + 1) * 128], V(Vaug[:, kb, hd, 0:129], f"Vaug_{kb // 4}"),
                         start=(kb == 0), stop=(kb == 4 * tg + 3))
            if kbs[-1] == 4 * tg + 3:
                dnb = dn[hd]
                S.recip(asm[:, 0:1], acc[0][:, 128:129])
                S.recip(asm[:, 1:2], acc[1][:, 128:129])
                S.tt("dve", asm[:, 2:3], asm[:, 1:2], neg_lam, ALU.mult)
                S.ts("dve", o1, acc[0][:, 0:128], asm[:, 0:1], ALU.mult)
                S.stt(dd, acc[1][:, 0:128], asm[:, 2:3], o1, ALU.mult, ALU.add)
                S.act(junk2, dd, AF.Square, accum=asm[:, 3:4])
                S.act(asm[:, 4:5], asm[:, 3:4], AF.Sqrt, scale=1.0 / 128, bias=eps_col)
                S.recip(asm[:, 5:6], asm[:, 4:5])
                S.stt(dnb, dd, asm[:, 5:6], subg_bc, ALU.mult, ALU.mult)

        todo = [0]

        def att_unit(slots_left=1):
            left = len(rounds) + 1 - todo[0]
            k = -(-left // max(slots_left, 1))
            for _ in range(min(k, left)):
                s_ = todo[0]
                todo[0] += 1
                if s_ >= 1:
                    emit_pv(s_ - 1)
                if s_ < len(rounds):
                    emit_qk_exp(s_)

        zb = [pb[7][:, tb * 128:(tb + 1) * 128] for tb in range(4)]
        rvb = [pb[1][:, tb * 128:(tb + 1) * 128] for tb in range(4)]
        for tb in range(4):
            tsl = slice(tb * 128, (tb + 1) * 128)
            S.mm(zb[tb], glrT[0:16, tsl], wa2, start=True, stop=False)
            S.mm(zb[tb], ones_f[0:1, 0:128], ba, start=False, stop=True)
        for tb in range(4):
            S.act(ez[tb], zb[tb], AF.Exp, scale=-1.0)
        for tb in range(4):
            S.act(lsp[tb], ez[tb], AF.Ln, bias=ones_f[:, 0:1])
        for tb in range(4):
            S.mm(rvb[tb], mstrict, lsp[tb])
        for tb in range(4):
            S.mm(pb[2][:, tb * 2:tb * 2 + 2], lsp[tb], chunkind)
        for tb in range(4):
            S.act(wexp[tb], rvb[tb], AF.Exp, scale=-1.0 / 16)
        S.act(dec, pb[2][:, 0:8], AF.Exp, scale=-1.0 / 16)
        for tb in range(4):
            S.tt("dve", kdec[tb], k_sb[tb], wexp[tb], ALU.mult)
        dslot = [V(pb[7].ap[:, 0:256], ["pb7a", "pb7"]), V(pb[3].ap[:, 0:256], ["pb3a", "pb3"]),
                 V(pb[7].ap[:, 256:512], ["pb7b", "pb7"]), V(pb[3].ap[:, 256:512], ["pb3b", "pb3"])]
        opsb = [pb[4][:, 0:256], pb[4][:, 256:512], pb[5][:, 0:256], pb[5][:, 256:512]]
        for c8 in range(8):
            tb, cc = c8 // 2, c8 % 2
            psl = slice(64 * cc, 64 * cc + 64)
            ds_ps = dslot[c8 % 4]
            S.mm(ds_ps, kdec[tb][psl, :], vg[tb][psl, :])
            S.stt(Sst2[(c8 + 1) % 2], Sst2[c8 % 2], dec[:, c8:c8 + 1], ds_ps, ALU.mult, ALU.add)
            S.copy("pool", Sbf[c8], Sst2[(c8 + 1) % 2])
            if c8 >= 1:
                ptb, pcc = (c8 - 1) // 2, (c8 - 1) % 2
                S.mm(opsb[ptb][slice(64 * pcc, 64 * pcc + 64), :], gqo[:, 64 * pcc:64 * pcc + 64], Sbf[c8 - 1])
            att_unit(13 - c8)
        S.mm(opsb[3][64:128, :], gqo[:, 64:128], Sbf[7])
        pick(osel, opsb)
        att_unit(5)
        S.act(junk, osel, AF.Square, accum=gsm[:, 0:1])
        att_unit(4)
        S.act(gsm[:, 1:2], gsm[:, 0:1], AF.Sqrt, scale=1.0 / 256, bias=eps_col)
        S.recip(gsm[:, 2:3], gsm[:, 1:2])
        att_unit(3)
        S.stt(osel, osel, gsm[:, 2:3], gog_bc, ALU.mult, ALU.mult)
        att_unit(2)
        S.tt("pool", osel, osel, sgr, ALU.mult)
        att_unit(1)
        for r in range(2):
            S.tr(pb[7][:, r * 128:(r + 1) * 128], osel[:, r * 128:(r + 1) * 128], ident)
            S.copy("act", mstage[2 + r][:, 0:128], pb[7][:, r * 128:(r + 1) * 128])
        while todo[0] <= len(rounds):
            att_unit()
        for hd in range(2):
            S.tr(sbk[0][:, hd * 128:(hd + 1) * 128], dn[hd], ident)
            S.copy("act", mstage[hd][:, 0:128], sbk[0][:, hd * 128:(hd + 1) * 128])
        ocols = slice(tg * 128, (tg + 1) * 128)
        for r in range(4):
            r0 = row_of(hg, r)
            S.dma("sp", V(t["mixT1"].ap[r0:r0 + 128, ocols], t["mixT1"].keys), mstage[r][:, 0:128])


def build_nc_phase1_only(ngroups=8):
    import contextlib
    nc = bass.Bass("TRN2", target_bir_lowering=False)
    t = {}
    for name, shape, dt in P1_INPUTS:
        t[name] = V(nc.dram_tensor(name, shape, dt, kind="ExternalInput").ap(), "dram_" + name)
    t["mixT1"] = V(nc.dram_tensor("mixT1", [512, 1024], BF16, kind="ExternalOutput").ap(), "dram_mixT1")
    S = Sched(nc)
    with contextlib.ExitStack() as es:
        build_phase1(nc, S, t, es, ngroups=ngroups)
        S.emit()
    return nc


_NC_CACHE = {}


def build_nc_fused(ngroups=8):
    import contextlib
    nc = bass.Bass("TRN2", target_bir_lowering=False)
    t1, t2 = {}, {}
    for name, shape, dt in P1_INPUTS:
        shape = list(shape)
        if name in ("w1", "wa2", "ba"):
            shape[0] = 4
        t1[name] = V(nc.dram_tensor(name, shape, dt, kind="ExternalInput").ap(), "dram_" + name)
    mixscr = V(nc.dram_tensor("mixscr", [2048, 1024], BF16, kind="Internal").ap(), "dram_mixscr")
    t1["mixT1"] = mixscr
    t1["xnscr"] = V(nc.dram_tensor("xnscr", [2048, 4096], BF16, kind="Internal").ap(), "dram_xnscr")
    t1["xnoscr"] = V(nc.dram_tensor("xnoscr", [128, 8, 16, 128], BF16, kind="Internal").ap(), "dram_xnoscr")
    for name, shape, dt in P2_INPUTS:
        if name == "mixT":
            continue
        t2[name] = V(nc.dram_tensor(name, shape, dt, kind="ExternalInput").ap(), "dram_" + name)
    t2["mixT"] = mixscr
    t2["outT"] = V(nc.dram_tensor("outT", [2048, 1024], F32, kind="ExternalOutput").ap(), "dram_outT")
    S = Sched(nc)

    def row_of(hg, r):
        return hg * 256 + r * 128 if r < 2 else 1024 + hg * 256 + (r - 2) * 128

    with contextlib.ExitStack() as es1:
        build_phase1(nc, S, t1, es1, ngroups=ngroups, npass=4, row_of=row_of)
    S.barrier()
    with contextlib.ExitStack() as es2:
        build_phase2(nc, S, t2, es2, mix_select=False)
    S.emit()
    return nc


def host_fused_inputs(inp, c):
    m = host_phase1_inputs(inp, c, groups=[0, 1, 2, 3])
    m.update(host_phase2_inputs(inp, c))
    return m


def kernel(**inp):
    inp = {k: np.asarray(v) for k, v in inp.items()}
    if "fused" not in _NC_CACHE:
        _NC_CACHE["fused"] = build_nc_fused()
    cores = list(range(8))
    res = run_bass_kernel_spmd(_NC_CACHE["fused"], [host_fused_inputs(inp, c) for c in cores], core_ids=cores)
    out = np.empty((2, 4096, 2048), np.float32)
    for c in cores:
        b, j = c // 4, c % 4
        out[b, own_tokens(c), :] = np.asarray(res.results[c]["outT"]).T
    return out
```

```python
import numpy as np
import ml_dtypes
import concourse.bass as bass
import concourse.mybir as mybir
from concourse.bass_utils import run_bass_kernel_spmd

F32 = mybir.dt.float32
BF16 = mybir.dt.bfloat16
I32 = mybir.dt.int32
AF = mybir.ActivationFunctionType
ALU = mybir.AluOpType
AX = mybir.AxisListType

EPS = 1e-6
NDMA_SLOTS = 16


class V:
    def __init__(self, ap, keys):
        self.ap = ap
        self.keys = tuple(keys) if isinstance(keys, (list, tuple)) else (keys,)

    def __getitem__(self, idx):
        return V(self.ap[idx], self.keys)

    def k(self, *keys):
        return V(self.ap, keys)


class Sched:
    ENG = ("pe", "act", "dve", "pool", "sp")

    def __init__(self, nc):
        self.nc = nc
        self.ops = []
        self.dma_rr = {"sp": 0, "pool": 0, "act": 0}

    def add(self, issue, fn, reads, writes, dma=False):
        if dma:
            s = self.dma_rr[issue]
            self.dma_rr[issue] = (s + 1) % NDMA_SLOTS
            stream = f"dma_{issue}_{s}"
        else:
            stream = issue
        rk, wk = [], []
        for v in reads:
            rk.extend(v.keys)
        for v in writes:
            wk.extend(v.keys)
        self.ops.append(dict(stream=stream, issue=issue, fn=fn, reads=rk, writes=wk, dma=dma))

    def barrier(self):
        self.ops.append(dict(barrier=True))

    def mm(self, out, lhsT, rhs, start=True, stop=True, extra_reads=()):
        self.add("pe", lambda e: e.matmul(out.ap, lhsT.ap, rhs.ap, start=start, stop=stop),
                 [lhsT, rhs, *extra_reads], [out])

    def tr(self, out, in_, ident):
        self.add("pe", lambda e: e.transpose(out.ap, in_.ap, ident.ap), [in_, ident], [out])

    def act(self, out, in_, func, scale=1.0, bias=0.0, accum=None):
        reads = [in_]
        if isinstance(scale, V):
            reads.append(scale)
        if isinstance(bias, V):
            reads.append(bias)
        writes = [out] + ([accum] if accum is not None else [])
        sc = scale.ap if isinstance(scale, V) else scale
        bi = bias.ap if isinstance(bias, V) else bias
        if accum is None:
            self.add("act", lambda e: e.activation(out.ap, in_.ap, func, bias=bi, scale=sc), reads, writes)
        else:
            self.add("act", lambda e: e.activation(out.ap, in_.ap, func, bias=bi, scale=sc, accum_out=accum.ap),
                     reads, writes)

    def tt(self, eng, out, a, b, op):
        self.add(eng, lambda e: e.tensor_tensor(out.ap, a.ap, b.ap, op), [a, b], [out])

    def ts(self, eng, out, a, s1, op0, s2=None, op1=None):
        reads = [a] + [s for s in (s1, s2) if isinstance(s, V)]
        x1 = s1.ap if isinstance(s1, V) else s1
        x2 = s2.ap if isinstance(s2, V) else s2
        if op1 is None:
            self.add(eng, lambda e: e.tensor_scalar(out.ap, a.ap, x1, None, op0), reads, [out])
        else:
            self.add(eng, lambda e: e.tensor_scalar(out.ap, a.ap, x1, x2, op0, op1), reads, [out])

    def stt(self, out, a, s, b, op0, op1):
        reads = [a, b] + ([s] if isinstance(s, V) else [])
        x = s.ap if isinstance(s, V) else s
        self.add("dve", lambda e: e.scalar_tensor_tensor(out.ap, a.ap, x, b.ap, op0, op1), reads, [out])

    def copy(self, eng, out, in_):
        if eng == "act":
            self.add("act", lambda e: e.copy(out.ap, in_.ap), [in_], [out])
        else:
            self.add(eng, lambda e: e.tensor_copy(out.ap, in_.ap), [in_], [out])

    def recip(self, out, in_):
        self.add("dve", lambda e: e.reciprocal(out.ap, in_.ap), [in_], [out])

    def memset(self, eng, out, val):
        self.add(eng, lambda e: e.memset(out.ap, val), [], [out])

    def dma(self, issue, out, in_):
        self.add(issue, lambda e: e.dma_start(out=out.ap, in_=in_.ap), [in_], [out], dma=True)

    def emit(self):
        nc = self.nc
        stream_pos = {}
        fence = {}
        ops = []
        for op in self.ops:
            if op.get("barrier"):
                fence = dict(stream_pos)
                continue
            p = stream_pos.get(op["stream"], 0) + 1
            stream_pos[op["stream"]] = p
            op["pos"] = p
            op["fence"] = fence
            ops.append(op)
        n = len(ops)
        last_w = {}
        readers = {}
        seen = {e: {} for e in self.ENG}
        for i, op in enumerate(ops):
            need = {}

            def want(j):
                o = ops[j]
                st = o["stream"]
                if st == "pe" and op["stream"] == "pe":
                    return
                if o["pos"] > need.get(st, 0):
                    need[st] = o["pos"]

            for k in op["reads"]:
                if k in last_w:
                    want(last_w[k])
            for k in op["writes"]:
                if k in last_w:
                    want(last_w[k])
                for r in readers.get(k, ()):
                    if r != i:
                        want(r)
            for st, p in op["fence"].items():
                if st == "pe" and op["stream"] == "pe":
                    continue
                if p > need.get(st, 0):
                    need[st] = p
            if op["dma"] and op["pos"] > 1:
                if op["pos"] - 1 > need.get(op["stream"], 0):
                    need[op["stream"]] = op["pos"] - 1
            sn = seen[op["issue"]]
            waits = []
            for st, p in need.items():
                if sn.get(st, 0) < p:
                    sn[st] = p
                    waits.append((st, p))
            op["waits"] = waits
            for k in op["reads"]:
                readers.setdefault(k, []).append(i)
            for k in op["writes"]:
                last_w[k] = i
                readers[k] = []
        by_stream = {}
        for i, op in enumerate(ops):
            by_stream.setdefault(op["stream"], []).append(i)
        signal = set()
        for op in ops:
            for st, p in op["waits"]:
                signal.add((st, p))
            if op["dma"]:
                signal.add((op["stream"], op["pos"]))
        for st, lst in by_stream.items():
            signal.add((st, ops[lst[-1]]["pos"]))
        rank = {}
        for st, lst in by_stream.items():
            r = 0
            for i in lst:
                if (st, ops[i]["pos"]) in signal:
                    r += 1
                    rank[(st, ops[i]["pos"])] = r
        finals = {st: max(v for (s, _), v in rank.items() if s == st) for st in by_stream}
        import contextlib
        with contextlib.ExitStack() as es:
            sems = {st: es.enter_context(nc.semaphore(f"s_{st}")) for st in by_stream}
            block = es.enter_context(nc.Block())
            handles = {"pe": nc.tensor, "act": nc.scalar, "dve": nc.vector, "pool": nc.gpsimd, "sp": nc.sync}

            def run_engine(ename):
                def body(_e):
                    eng = handles[ename]
                    for op in ops:
                        if op["issue"] != ename:
                            continue
                        for st, p in op["waits"]:
                            mult = 16 if st.startswith("dma_") else 1
                            eng.wait_ge(sems[st], rank[(st, p)] * mult)
                        ins = op["fn"](eng)
                        key = (op["stream"], op["pos"])
                        if key in rank:
                            ins.then_inc(sems[op["stream"]], 16 if op["dma"] else 1)
                    if ename == "sp":
                        for st, f in finals.items():
                            mult = 16 if st.startswith("dma_") else 1
                            eng.wait_ge(sems[st], f * mult)
                return body

            block.tensor(run_engine("pe"))
            block.scalar(run_engine("act"))
            block.vector(run_engine("dve"))
            block.gpsimd(run_engine("pool"))
            block.sync(run_engine("sp"))


def _alloc(es, nc, name, shape, dt):
    return es.enter_context(nc.sbuf_tensor("sb_" + name, list(shape), dt))


def rmsnorm_fm(S, src_fn, gcol_fn, dst_fn, nk, ntok, D, tmp, pbank, ones_bf, eps_col, f32_fn=None):
    for k in range(nk):
        sq = tmp["sq"][k % 2][:, 0:ntok]
        S.act(sq, src_fn(k), AF.Square)
        S.mm(pbank[:, 0:ntok], ones_bf, sq, start=(k == 0), stop=(k == nk - 1))
    rt = tmp["rt"][:, 0:ntok]
    S.act(rt, pbank[:, 0:ntok], AF.Sqrt, scale=1.0 / D, bias=eps_col)
    rb = tmp["rb"][:, 0:ntok]
    S.recip(rb, rt)
    for k in range(nk):
        if f32_fn is None:
            S.stt(dst_fn(k), src_fn(k), gcol_fn(k), rb, ALU.mult, ALU.mult)
        else:
            f32_fn(k, rb)


def build_phase2(nc, S, t, es_outer, mix_select=False):
    import contextlib
    es = es_outer
    NT = 1024
    hT = _alloc(es, nc, "hT", [128, 16, NT], F32)
    ones_bf = V(_alloc(es, nc, "ones_bf", [128, 128], BF16)[:], "ones_bf")
    ones_f = V(_alloc(es, nc, "ones_f", [128, 128], F32)[:], "ones_f")
    ident = V(_alloc(es, nc, "ident", [128, 128], F32)[:], "ident")
    eps_col = V(_alloc(es, nc, "eps_col", [128, 1], F32)[:], "eps_col")
    gvec = V(_alloc(es, nc, "gvec", [128, 48], F32)[:], "gvec")
    cg = V(_alloc(es, nc, "cg", [128, 8], F32)[:], "cg")
    tmp = {
        "sq": [V(_alloc(es, nc, f"sq{i}", [128, 512], BF16)[:], f"sq{i}") for i in range(2)],
        "rt": V(_alloc(es, nc, "rt", [128, 512], F32)[:], "rt"),
        "rb": V(_alloc(es, nc, "rb", [128, 512], F32)[:], "rb"),
    }
    pb = [V(es.enter_context(nc.psum_tensor(f"pb{i}", [128, 512], F32))[:], f"pb{i}") for i in range(8)]
    wpool = []

    def hv(k, half):
        return V(hT[:, k, half * 512:(half + 1) * 512], f"hT_{k}_{half}")

    S.memset("dve", ones_bf, 1.0)
    S.memset("dve", ones_f, 1.0)
    S.memset("dve", eps_col, EPS)
    S.dma("sp", ident, t["ident"])
    S.dma("sp", gvec, t["gvec"])
    S.dma("sp", cg, t["cg"])

    def wview(i, a, b):
        return V(wpool[i][:].rearrange("p (a b) -> p a b", a=a), f"wp{i}")

    state = {"wi": 0, "pbi": 0}

    def next_w():
        i = state["wi"]
        state["wi"] = (i + 1) % 2
        return i

    def next_pb(lo=4, n=4):
        i = state["pbi"]
        state["pbi"] = (i + 1) % n
        return pb[lo + i]

    def linear_fm(wdram, col0, ncols, in_fn, nk, ntoks, evac):
        for g in range(ncols // 512):
            wi = next_w()
            wv = wview(wi, nk, 512)
            S.dma("pool", wv, V(wdram.ap[:, col0 + g * 512: col0 + (g + 1) * 512].rearrange("(k p) n -> p k n", p=128), wdram.keys))
            for ocl in range(4):
                oc = g * 4 + ocl
                for ti, nt in enumerate(ntoks):
                    ps = next_pb()
                    for k in range(nk):
                        S.mm(ps[:, 0:nt], wv[:, k, ocl * 128:(ocl + 1) * 128], in_fn(k, ti), start=(k == 0), stop=(k == nk - 1))
                    evac(oc, ti, ps[:, 0:nt])

    with contextlib.ExitStack() as esA:
        wpool[:] = [_alloc(esA, nc, f"wpA{i}", [128, 8192], BF16) for i in range(2)]
        mixT = _alloc(esA, nc, "mixT", [128, 16, NT], BF16)
        if mix_select:
            stage = [V(_alloc(esA, nc, f"mstg{q}", [128, 1024], BF16)[:], f"mstg{q}") for q in range(4)]
            sel = V(_alloc(esA, nc, "sel", [128, 4], F32)[:], "sel")
            S.dma("sp", sel, t["sel"])
        for k in range(16):
            if mix_select:
                mk = V(mixT[:, k, :], f"mixT_{k}")
                for q in range(4):
                    S.dma("sp", stage[q], V(t["mixT"].ap[k * 128:(k + 1) * 128, q * 1024:(q + 1) * 1024], t["mixT"].keys))
                S.ts("dve", mk, stage[0], sel[:, 0:1], ALU.mult)
                for q in range(1, 4):
                    S.stt(mk, stage[q], sel[:, q:q + 1], mk, ALU.mult, ALU.add)
            else:
                S.dma("sp", V(mixT[:, k, :], f"mixT_{k}"), V(t["mixT"].ap[k * 128:(k + 1) * 128, :], t["mixT"].keys))
            for half in range(2):
                S.dma("sp", hv(k, half), V(t["xT2"].ap[k * 128:(k + 1) * 128, half * 512:(half + 1) * 512], t["xT2"].keys))

        def evacA(oc, ti, ps):
            S.tt("dve", hv(oc, ti), ps, hv(oc, ti), ALU.add)

        linear_fm(t["w_out"], 0, 2048, lambda k, ti: V(mixT[:, k, ti * 512:(ti + 1) * 512], f"mixT_{k}"), 16, [512, 512], evacA)
    S.barrier()

    with contextlib.ExitStack() as esB:
        wpool[:] = [_alloc(esB, nc, f"wpB{i}", [128, 8192], BF16) for i in range(2)]
        kT = _alloc(esB, nc, "kT", [128, 16, 256], BF16)
        v_sb = _alloc(esB, nc, "v_sb", [128, 2, 2048], BF16)
        with contextlib.ExitStack() as esB1:
            memT = _alloc(esB1, nc, "memT", [128, 16, 256], F32)
            memnT = _alloc(esB1, nc, "memnT", [128, 16, 256], BF16)
            kraw = _alloc(esB1, nc, "kraw", [128, 16, 256], F32)
            for k in range(16):
                S.dma("sp", V(memT[:, k, :], f"memT_{k}"), V(t["memT"].ap[k * 128:(k + 1) * 128, :], t["memT"].keys))
            rmsnorm_fm(S, lambda k: V(memT[:, k, :], f"memT_{k}"), lambda k: gvec[:, 16 + k:17 + k],
                       lambda k: V(memnT[:, k, :], f"memnT_{k}"), 16, 256, 2048.0, tmp, pb[0], ones_bf, eps_col)

            def evacK(oc, ti, ps):
                S.copy("act", V(kraw[:, oc, :], f"kraw_{oc}"), ps)

            linear_fm(t["w_ckv"], 0, 2048, lambda k, ti: V(memnT[:, k, :], f"memnT_{k}"), 16, [256], evacK)
            for h in range(4):
                rmsnorm_fm(S, lambda dc: V(kraw[:, h * 4 + dc, :], f"kraw_{h * 4 + dc}"), lambda dc: cg[:, 4 + dc:5 + dc],
                           lambda dc: V(kT[:, h * 4 + dc, :], f"kT_{h * 4 + dc}"), 4, 256, 512.0, tmp, pb[0], ones_bf, eps_col)
            for g in range(4):
                wi = next_w()
                wv = wview(wi, 16, 512)
                S.dma("pool", wv, V(t["w_ckv"].ap[:, 2048 + g * 512: 2048 + (g + 1) * 512].rearrange("(k p) n -> p k n", p=128), t["w_ckv"].keys))
                for mc in range(2):
                    ps = next_pb()
                    for k in range(16):
                        S.mm(ps, V(memnT[:, k, mc * 128:(mc + 1) * 128], f"memnT_{k}"), wv[:, k, :], start=(k == 0), stop=(k == 15))
                    S.copy("act", V(v_sb[:, mc, g * 512:(g + 1) * 512], f"v_sb_{mc}_{g}"), ps)
        S.barrier()
        xnh = _alloc(esB, nc, "xnh", [128, 16, 512], BF16)
        oTh = _alloc(esB, nc, "oTh", [128, 16, 512], BF16)
        qraw = _alloc(esB, nc, "qraw", [128, 4, 512], F32)
        qT = _alloc(esB, nc, "qT", [128, 4, 512], BF16)
        pT = _alloc(esB, nc, "pT", [128, 2, 512], BF16)
        rden = V(_alloc(esB, nc, "rden", [128, 512], F32)[:], "rden")
        for half in range(2):
            rmsnorm_fm(S, lambda k: hv(k, half), lambda k: gvec[:, k:k + 1],
                       lambda k: V(xnh[:, k, :], f"xnh_{k}"), 16, 512, 2048.0, tmp, pb[0], ones_bf, eps_col)
            for h in range(4):
                wi = next_w()
                wv = wview(wi, 16, 512)
                S.dma("pool", wv, V(t["w_cq"].ap[:, h * 512:(h + 1) * 512].rearrange("(k p) n -> p k n", p=128), t["w_cq"].keys))
                for dc in range(4):
                    ps = next_pb()
                    for k in range(16):
                        S.mm(ps, wv[:, k, dc * 128:(dc + 1) * 128], V(xnh[:, k, :], f"xnh_{k}"), start=(k == 0), stop=(k == 15))
                    S.copy("act", V(qraw[:, dc, :], f"qraw_{dc}"), ps)
                rmsnorm_fm(S, lambda dc: V(qraw[:, dc, :], f"qraw_{dc}"), lambda dc: cg[:, dc:dc + 1],
                           lambda dc: V(qT[:, dc, :], f"qT_{dc}"), 4, 512, 512.0, tmp, pb[0], ones_bf, eps_col)
                for mc in range(2):
                    ps = next_pb()
                    for dc in range(4):
                        S.mm(ps, V(kT[:, h * 4 + dc, mc * 128:(mc + 1) * 128], f"kT_{h * 4 + dc}"), V(qT[:, dc, :], f"qT_{dc}"),
                             start=(dc == 0), stop=(dc == 3))
                    S.act(V(pT[:, mc, :], f"pT_{mc}"), ps, AF.Exp, scale=512.0 ** -0.5)
                ps = pb[1]
                for mc in range(2):
                    S.mm(ps, ones_bf, V(pT[:, mc, :], f"pT_{mc}"), start=(mc == 0), stop=(mc == 1))
                S.recip(rden, ps)
                for dvc in range(4):
                    ps = next_pb()
                    for mc in range(2):
                        S.mm(ps, V(v_sb[:, mc, h * 512 + dvc * 128: h * 512 + (dvc + 1) * 128], f"v_sb_{mc}_{h}"), V(pT[:, mc, :], f"pT_{mc}"),
                             start=(mc == 0), stop=(mc == 1))
                    S.tt("dve", V(oTh[:, h * 4 + dvc, :], f"oTh_{h * 4 + dvc}"), ps, rden, ALU.mult)

            def evacO(oc, ti, ps):
                S.tt("dve", hv(oc, half), ps, hv(oc, half), ALU.add)

            linear_fm(t["w_co"], 0, 2048, lambda k, ti: V(oTh[:, k, :], f"oTh_{k}"), 16, [512], evacO)
    S.barrier()

    with contextlib.ExitStack() as esC:
        wpool[:] = [_alloc(esC, nc, f"wpC{i}", [128, 8192], BF16) for i in range(5)]
        xnT = _alloc(esC, nc, "xnT", [128, 16, NT], BF16)
        hid = _alloc(esC, nc, "hid", [128, 4, NT], BF16)
        xf = [V(_alloc(esC, nc, f"xf{i}", [128, 512], F32)[:], f"xf{i}") for i in range(2)]
        w_r = V(_alloc(esC, nc, "w_r", [128, 16, 36], F32)[:], "w_r")
        b_r = V(_alloc(esC, nc, "b_r", [1, 36], F32)[:], "b_r")
        GT = _alloc(esC, nc, "GT", [32, NT], F32)
        L = V(_alloc(esC, nc, "L", [128, 36], F32)[:], "L")
        sm = V(_alloc(esC, nc, "sm", [128, 16], F32)[:], "sm")
        gm = V(_alloc(esC, nc, "gm", [128, 4], F32)[:], "gm")
        pen = V(_alloc(esC, nc, "pen", [128, 4], F32)[:], "pen")
        ge = V(_alloc(esC, nc, "ge", [128, 4], F32)[:], "ge")
        elm = V(_alloc(esC, nc, "elm", [128, 32], F32)[:], "elm")
        elm2 = V(_alloc(esC, nc, "elm2", [128, 32], F32)[:], "elm2")
        mk1 = V(_alloc(esC, nc, "mk1", [128, 32], F32)[:], "mk1")
        mk2 = V(_alloc(esC, nc, "mk2", [128, 32], F32)[:], "mk2")
        G = V(_alloc(esC, nc, "G", [128, 32], F32)[:], "G")
        sg_t = xf
        gbc = [tmp["rt"], tmp["rb"]]
        S.dma("sp", w_r, V(t["w_r"].ap.rearrange("(k p) n -> p k n", p=128), t["w_r"].keys))
        S.dma("sp", b_r, t["b_r"])
        BIG = 1.0e30
        for half in range(2):
            def f32_fn(k, rb, half=half):
                x = xf[k % 2]
                S.stt(x, hv(k, half), gvec[:, 32 + k:33 + k], rb, ALU.mult, ALU.mult)
                for tb in range(4):
                    S.mm(pb[4 + tb][:, 0:36], x[:, tb * 128:(tb + 1) * 128], w_r[:, k, :], start=(k == 0), stop=False)
                S.copy("act", V(xnT[:, k, half * 512:(half + 1) * 512], f"xnT_{k}_{half}"), x)

            rmsnorm_fm(S, lambda k: hv(k, half), None, None, 16, 512, 2048.0, tmp, pb[0], ones_bf, eps_col, f32_fn=f32_fn)
            for tb in range(4):
                S.mm(pb[4 + tb][:, 0:36], ones_f[0:1, 0:128], b_r, start=False, stop=True)
                S.copy("dve", L, pb[4 + tb][:, 0:36])
                gmax, gsum, grw, m1, m2, d12, sgm, g1, g2, ngmax = [sm[:, i:i + 1] for i in range(10)]
                S.add("dve", lambda e, o=gmax, i=L: e.reduce_max(o.ap, i.ap[:, 0:4], AX.X), [L], [gmax])
                S.ts("dve", gm, L[:, 0:4], gmax, ALU.is_equal)
                S.ts("dve", ngmax, gmax, -1.0, ALU.mult)
                S.act(ge, L[:, 0:4], AF.Exp, bias=ngmax, accum=gsum)
                S.recip(grw, gsum)
                S.ts("dve", pen, gm, BIG, ALU.mult, -BIG, ALU.add)
                for g in range(4):
                    S.ts("dve", elm[:, g * 8:(g + 1) * 8], L[:, 4 + g * 8: 4 + (g + 1) * 8], pen[:, g:g + 1], ALU.add)
                S.add("dve", lambda e, o=m1, i=elm: e.reduce_max(o.ap, i.ap, AX.X), [elm], [m1])
                S.ts("dve", mk1, elm, m1, ALU.is_equal)
                S.stt(elm2, mk1, -BIG, elm, ALU.mult, ALU.add)
                S.add("dve", lambda e, o=m2, i=elm2: e.reduce_max(o.ap, i.ap, AX.X), [elm2], [m2])
                S.ts("dve", mk2, elm2, m2, ALU.is_equal)
                S.tt("dve", d12, m1, m2, ALU.subtract)
                S.act(sgm, d12, AF.Sigmoid)
                S.tt("dve", g1, sgm, grw, ALU.mult)
                S.tt("dve", g2, grw, g1, ALU.subtract)
                S.ts("dve", G, mk1, g1, ALU.mult)
                S.stt(G, mk2, g2, G, ALU.mult, ALU.add)
                S.tr(pb[3][0:32, 0:128], G, ident)
                col = half * 512 + tb * 128
                S.copy("dve", V(GT[:, col:col + 128], f"GT_{half}"), pb[3][0:32, 0:128])
        for e in range(32):
            wg = V(wpool[e % 2][:].rearrange("p (a b) -> p a b", a=16), f"wp{e % 2}")
            wu = V(wpool[2 + e % 2][:].rearrange("p (a b) -> p a b", a=16), f"wp{2 + e % 2}")
            wd = V(wpool[4][:].rearrange("p (a b) -> p a b", a=4), "wp4")
            S.dma("pool", wg, V(t["w_gate"].ap[e].rearrange("(k p) n -> p k n", p=128), t["w_gate"].keys))
            S.dma("pool", wu, V(t["w_up"].ap[e].rearrange("(k p) n -> p k n", p=128), t["w_up"].keys))
            S.dma("pool", wd, V(t["w_down"].ap[e].rearrange("(k p) n -> p k n", p=128), t["w_down"].keys))
            for half in range(2):
                gb = gbc[half]
                S.mm(pb[1], V(ident.ap[0:32, e:e + 1].broadcast_to([32, 128]), ident.keys), V(GT[:, half * 512:(half + 1) * 512], f"GT_{half}"))
                S.copy("act", gb, pb[1])
                for fc in range(4):
                    pg, pu = pb[2], pb[3]
                    for k in range(16):
                        S.mm(pg, wg[:, k, fc * 128:(fc + 1) * 128], V(xnT[:, k, half * 512:(half + 1) * 512], f"xnT_{k}_{half}"),
                             start=(k == 0), stop=(k == 15))
                    for k in range(16):
                        S.mm(pu, wu[:, k, fc * 128:(fc + 1) * 128], V(xnT[:, k, half * 512:(half + 1) * 512], f"xnT_{k}_{half}"),
                             start=(k == 0), stop=(k == 15))
                    sg = sg_t[fc % 2]
                    S.act(sg, pg, AF.Silu)
                    S.tt("pool", sg, sg, gb, ALU.mult)
                    S.tt("dve", V(hid[:, fc, half * 512:(half + 1) * 512], f"hid_{fc}_{half}"), pu, sg, ALU.mult)
                for oc in range(16):
                    ps = next_pb()
                    for fc in range(4):
                        S.mm(ps, wd[:, fc, oc * 128:(oc + 1) * 128], V(hid[:, fc, half * 512:(half + 1) * 512], f"hid_{fc}_{half}"),
                             start=(fc == 0), stop=(fc == 3))
                    S.tt("dve", hv(oc, half), ps, hv(oc, half), ALU.add)
        for k in range(16):
            for half in range(2):
                S.dma("sp", V(t["outT"].ap[k * 128:(k + 1) * 128, half * 512:(half + 1) * 512], t["outT"].keys), hv(k, half))


P2_INPUTS = [
    ("mixT", [2048, 1024], BF16), ("xT2", [2048, 1024], F32), ("memT", [2048, 256], F32),
    ("w_out", [2048, 2048], F32), ("w_cq", [2048, 2048], F32), ("w_co", [2048, 2048], F32),
    ("w_ckv", [2048, 4096], F32), ("gvec", [128, 48], F32), ("cg", [128, 8], F32), ("ident", [128, 128], F32),
    ("w_r", [2048, 36], F32), ("b_r", [1, 36], F32),
    ("w_gate", [32, 2048, 512], F32), ("w_up", [32, 2048, 512], F32), ("w_down", [32, 512, 2048], F32),
]


def host_phase2_inputs(inp, c):
    b, j = c // 4, c % 4
    tok = own_tokens(c)

    def pcol(v):
        return np.ascontiguousarray(v.reshape(-1, 128).T)

    gvec = np.concatenate([pcol(inp["g_cross"][0]), pcol(inp["g_mem"][0]), pcol(inp["g_ffn"][0])], axis=1)
    cg = np.concatenate([pcol(inp["cq_norm_g"][0]), pcol(inp["ck_norm_g"][0])], axis=1)
    return {
        "xT2": np.ascontiguousarray(inp["x"][b, tok, :].T),
        "memT": np.ascontiguousarray(inp["mem"][b].T),
        "w_out": inp["w_out"][0], "w_cq": inp["w_cq"][0], "w_co": inp["w_co"][0], "w_ckv": inp["w_ckv"][0],
        "gvec": np.ascontiguousarray(gvec, dtype=np.float32), "cg": np.ascontiguousarray(cg, dtype=np.float32),
        "ident": np.eye(128, dtype=np.float32),
        "w_r": np.ascontiguousarray(np.concatenate([inp["w_router_grp"][0], inp["w_router_exp"][0]], axis=1)),
        "b_r": np.ascontiguousarray(np.concatenate([inp["b_router_grp"][0], inp["b_router_exp"][0]])[None, :]),
        "w_gate": inp["w_gate"][0], "w_up": inp["w_up"][0], "w_down": inp["w_down"][0],
    }


def build_nc_phase2_only():
    import contextlib
    nc = bass.Bass("TRN2", target_bir_lowering=False)
    t = {}
    for name, shape, dt in P2_INPUTS:
        t[name] = V(nc.dram_tensor(name, shape, dt, kind="ExternalInput").ap(), "dram_" + name)
    t["outT"] = V(nc.dram_tensor("outT", [2048, 1024], F32, kind="ExternalOutput").ap(), "dram_outT")
    S = Sched(nc)
    with contextlib.ExitStack() as es:
        build_phase2(nc, S, t, es)
        S.emit()
    return nc


P1_INPUTS = [
    ("xT1", [2048, 4096], F32), ("w1", [1, 2048, 1552], F32), ("pp1", [128, 8], F32), ("pos", [1, 4096], I32),
    ("g1", [128, 16], F32), ("lam4", [1, 256], F32), ("subg", [1, 128], F32), ("gog", [1, 256], F32),
    ("wa2", [1, 16, 128], F32), ("ba", [1, 1, 128], F32), ("ident1", [128, 128], F32), ("mstrict", [128, 128], F32),
    ("chunkind", [128, 2], F32), ("protT", [128, 128], F32), ("sel4", [128, 4], F32), ("maskT", [128, 512], F32),
]
_DBG_STOP = [99]
C_QA, C_QB, C_KA, C_KB, C_GQ, C_LR, C_TM1, C_TM2, NW1 = 0, 128, 256, 384, 512, 640, 656, 1168, 1552


def host_w1(inp, j):
    w = inp["w_in"][0]
    hA, hB = 2 * j, 2 * j + 1
    cols = []
    cols += [w[:, hA * 128:(hA + 1) * 128], w[:, hB * 128:(hB + 1) * 128]]
    cols += [w[:, 1024 + hA * 128:1024 + (hA + 1) * 128], w[:, 1024 + hB * 128:1024 + (hB + 1) * 128]]
    cols += [w[:, 3072 + j * 128:3072 + (j + 1) * 128]]
    cols += [w[:, 5120:5136]]
    cols += [w[:, 2048 + hA * 128:2048 + (hA + 1) * 128], w[:, 2048 + hB * 128:2048 + (hB + 1) * 128]]
    cols += [w[:, 5136 + j * 256:5136 + (j + 1) * 256]]
    cols += [w[:, 3584 + j * 128:3584 + (j + 1) * 128]]
    cols += [w[:, 4096 + j * 256:4096 + (j + 1) * 256]]
    w1 = np.ascontiguousarray(np.concatenate(cols, axis=1))
    assert w1.shape == (2048, NW1)
    return w1


def host_phase1_inputs(inp, c, groups=None):
    b, j = c // 4, c % 4
    groups = [j] if groups is None else groups
    w1 = np.stack([host_w1(inp, g) for g in groups], axis=0)
    pp1 = np.zeros((128, 8), np.float32)
    pp1[:, 0] = np.tile(inp["q_norm_g"][0], 2)
    pp1[:, 2] = np.tile(inp["k_norm_g"][0], 2)
    half = 32
    freq = (np.float32(10000.0) ** (-np.arange(half, dtype=np.float32) / np.float32(half))).astype(np.float32)
    pp1[:, 4] = np.tile(freq, 4)
    lam4 = np.concatenate([inp["lambda_q1"][0], inp["lambda_k1"][0], inp["lambda_q2"][0], inp["lambda_k2"][0]])[None, :]
    l = np.arange(128)
    mstrict = ((l[:, None] > l[None, :]) & ((l[:, None] // 64) == (l[None, :] // 64))).astype(np.float32)
    chunkind = np.stack([(l < 64), (l >= 64)], axis=1).astype(np.float32)
    protT = np.zeros((128, 128), np.float32)
    for m in range(128):
        if (m % 64) < 32:
            protT[m + 32, m] = -1.0
        else:
            protT[m - 32, m] = 1.0
    return {
        "xT1": np.ascontiguousarray(inp["x"][b].T), "w1": w1, "pp1": pp1,
        "pos": np.ascontiguousarray(inp["positions"][b][None, :].astype(np.int32)),
        "g1": np.ascontiguousarray(inp["g_attn"][0].reshape(16, 128).T),
        "lam4": np.ascontiguousarray(lam4.astype(np.float32)),
        "subg": np.ascontiguousarray(inp["diff_subln_g"][0][None, :]),
        "gog": np.ascontiguousarray(inp["gla_out_g"][0][None, :]),
        "wa2": np.ascontiguousarray(np.stack([inp["gla_w_a2"][0][:, g * 128:(g + 1) * 128] for g in groups], axis=0)),
        "ba": np.ascontiguousarray(np.stack([inp["gla_b_a"][0][None, g * 128:(g + 1) * 128] for g in groups], axis=0)),
        "ident1": np.eye(128, dtype=np.float32), "mstrict": mstrict, "chunkind": chunkind, "protT": protT,
        "sel4": own_sel(c), "maskT": own_mask(c),
    }


def own_sel(c):
    sel = np.zeros((128, 4), np.float32)
    sel[:, c % 4] = 1.0
    return sel


def own_mask(c):
    j = c % 4
    M = np.zeros((128, 4, 128), np.float32)
    for m in range(4):
        if m < j:
            M[:, m, :] = 1.0
        elif m == j:
            M[:, m, :] = 1.0
            M[64:128, m, 0:64] = 0.0
    return np.ascontiguousarray(M.reshape(128, 512))


def own_tokens(c):
    j = c % 4
    return np.concatenate([np.arange((4 * i + j) * 128, (4 * i + j + 1) * 128) for i in range(8)])


def build_phase1(nc, S, t, es, ngroups=8, npass=1, row_of=None):
    import math
    NTOK = 4096
    LAM_INIT = 0.8 - 0.6 * math.exp(-0.3 * 0)
    PI = math.pi

    def A(name, shape, dt):
        return _alloc(es, nc, "p1" + name, shape, dt)

    w1s = A("w1s", [128, 16, NW1], BF16)
    KT = A("KT", [128, 2, NTOK], BF16)
    QTo = A("QTo", [128, 2, 128], BF16)
    Vaug = A("Vaug", [128, 32, 2, 130], BF16)
    cosT = A("cosT", [128, NTOK], BF16)
    sinT = A("sinT", [128, NTOK], BF16)
    gqo = V(A("gqo", [128, 128], BF16)[:], "gqo")
    xno = A("xno", [128, 16, 128], BF16)
    cs2all = A("cs2all", [128, 8, 256], BF16)
    sn2all = A("sn2all", [128, 8, 256], BF16)
    maskT = V(A("maskT", [128, 512], BF16)[:], "maskT")
    sel4 = V(A("sel4", [128, 4], F32)[:], "sel4")
    osel = V(A("osel", [128, 256], F32)[:], "osel")
    xg = A("xg", [128, 16, 512], F32)
    xn = A("xn", [128, 16, 512], BF16)
    ones_bf = V(A("ones_bf", [128, 128], BF16)[:], "ones_bf")
    onesblk = V(A("onesblk", [128, 128], BF16)[:], "onesblk")
    protT = V(A("protT", [128, 128], BF16)[:], "protT")
    ident = V(A("ident", [128, 128], F32)[:], "ident")
    mstrict = V(A("mstrict", [128, 128], F32)[:], "mstrict")
    chunkind = V(A("chunkind", [128, 2], F32)[:], "chunkind")
    ones_f = V(A("ones_f", [128, 128], F32)[:], "ones_f")
    eps_col = V(A("eps_col", [128, 1], F32)[:], "eps_col")
    pp1 = V(A("pp1", [128, 8], F32)[:], "pp1")
    g1 = V(A("g1", [128, 16], F32)[:], "g1")
    lamt = V(A("lamt", [128, 256], F32)[:], "lamt")
    lamv = V(A("lamv", [128, 8], F32)[:], "lamv")
    subg_bc = V(A("subg_bc", [128, 128], F32)[:], "subg_bc")
    gog_bc = V(A("gog_bc", [128, 256], F32)[:], "gog_bc")
    wa2 = V(A("wa2", [16, 128], F32)[:], "wa2")
    ba = V(A("ba", [1, 128], F32)[:], "ba")
    tmp = {
        "sq": [V(A(f"sq{i}", [128, 512], BF16)[:], f"sq{i}") for i in range(2)],
        "rt": V(A("rt", [128, 512], F32)[:], "rt"),
        "rb": V(A("rb", [128, 512], F32)[:], "rb"),
    }
    qn_bf = V(A("qn_bf", [128, 512], BF16)[:], "qn_bf")
    t1 = V(A("t1", [128, 512], F32)[:], "t1")
    t2 = V(A("t2", [128, 512], F32)[:], "t2")
    rtB = V(A("rtB", [128, 512], F32)[:], "rtB")
    rbB = V(A("rbB", [128, 512], F32)[:], "rbB")
    qnB = V(A("qnB", [128, 512], BF16)[:], "qnB")
    t1B = V(A("t1B", [128, 512], F32)[:], "t1B")
    t2B = V(A("t2B", [128, 512], F32)[:], "t2B")
    pexp = [V(A(f"pexp{i}", [128, 512], BF16)[:], f"pexp{i}") for i in range(2)]
    glrT = V(A("glrT", [16, 512], F32)[:], "glrT")
    pexp = pexp + [V(A(f"pexp{i}", [128, 512], BF16)[:], f"pexp{i}") for i in (2, 3)]
    ez = [V(t1.ap[:, i * 128:(i + 1) * 128], "t1") for i in range(4)]
    wexp = ez
    lsp = [V(t2.ap[:, i * 128:(i + 1) * 128], "t2") for i in range(4)]
    k_sb = [V(A(f"k_sb{i}", [128, 128], F32)[:], f"k_sb{i}") for i in range(2)] + \
           [V(rtB.ap[:, i * 128:(i + 1) * 128], "rtB") for i in range(2)]
    kdec = [V(tmp["sq"][0].ap[:, i * 128:(i + 1) * 128], "sq0") for i in range(4)]
    vg = [V(A(f"vg{i}", [128, 256], BF16)[:], f"vg{i}") for i in range(2)] + \
         [V(qnB.ap[:, 0:256], "qnB"), V(qnB.ap[:, 256:512], "qnB")]
    go = [V(tmp["rb"].ap[:, 0:256], "rb"), V(tmp["rb"].ap[:, 256:512], "rb")]
    sgr = V(A("sgr", [128, 256], F32)[:], "sgr")
    Sst2 = [V(A(f"Sst{i}", [128, 256], F32)[:], f"Sst{i}") for i in range(2)]
    Sst = Sst2[0]
    Sbf = [V(A(f"Sbf{i}", [128, 256], BF16)[:], f"Sbf{i}") for i in range(8)]
    dec = V(A("dec", [128, 8], F32)[:], "dec")
    junk = V(t2.ap[:, 0:256], "t2")
    junk2 = V(A("junk2", [128, 128], F32)[:], "junk2")
    gsm = V(A("gsm", [128, 16], F32)[:], "gsm")
    o1 = V(A("o1", [128, 128], F32)[:], "o1")
    dd = V(A("dd", [128, 128], F32)[:], "dd")
    dn = [V(A(f"dn{i}", [128, 128], F32)[:], f"dn{i}") for i in range(2)]
    asm = V(A("asm", [128, 8], F32)[:], "asm")
    mstage = [V(A(f"mstage{r}", [128, 512], BF16)[:], f"mstage{r}") for r in range(4)]
    pb = [V(es.enter_context(nc.psum_tensor(f"p1pb{i}", [128, 512], F32))[:], f"pb{i}") for i in range(8)]

    if row_of is None:
        row_of = lambda hg, r: r * 128
    S.dma("pool", protT, t["protT"])
    S.dma("pool", maskT, t["maskT"])
    for dst, src in [(ident, "ident1"), (mstrict, "mstrict"), (chunkind, "chunkind"), (pp1, "pp1"), (g1, "g1"), (sel4, "sel4")]:
        S.dma("sp", dst, t[src])
    S.dma("sp", lamt, V(t["lam4"].ap.partition_broadcast(128), t["lam4"].keys))
    S.dma("sp", subg_bc, V(t["subg"].ap.partition_broadcast(128), t["subg"].keys))
    S.dma("sp", gog_bc, V(t["gog"].ap.partition_broadcast(128), t["gog"].keys))
    S.memset("dve", ones_bf, 1.0)
    S.memset("dve", ones_f, 1.0)
    S.memset("dve", eps_col, EPS)
    S.memset("dve", onesblk, 0.0)
    S.memset("dve", onesblk[0:64, 0:64], 1.0)
    S.memset("dve", onesblk[64:128, 64:128], 1.0)
    S.memset("pool", V(Vaug[:], [f"Vaug_{g}" for g in range(8)]), 1.0)
    for i in range(2):
        S.tt("dve", junk2[:, 0:64], lamt[:, i * 128:i * 128 + 64], lamt[:, i * 128 + 64:i * 128 + 128], ALU.mult)
        S.add("dve", lambda e, o=lamv[:, i:i + 1], x=junk2: e.reduce_sum(o.ap, x.ap[:, 0:64], AX.X), [junk2], [lamv])
        S.act(lamv[:, 2 + i:3 + i], lamv[:, i:i + 1], AF.Exp)
    S.tt("dve", lamv[:, 4:5], lamv[:, 2:3], lamv[:, 3:4], ALU.subtract)
    S.ts("dve", lamv[:, 5:6], lamv[:, 4:5], LAM_INIT, ALU.add, -1.0, ALU.mult)
    neg_lam = lamv[:, 5:6]
    S.ts("dve", subg_bc, subg_bc, 1.0 - LAM_INIT, ALU.mult)
    xgf = xg[:].rearrange("p a b -> p (a b)")
    posi = V(xgf[:, 0:4096].bitcast(I32), "xg_tab0")
    angf = V(xgf[:, 4096:8192], "xg_tab1")
    kf = V(xgf[:, 0:4096], "xg_tab0")
    ki = V(xgf[:, 0:4096].bitcast(I32), "xg_tab0")
    S.dma("sp", posi, V(t["pos"].ap.partition_broadcast(128), t["pos"].keys))
    S.copy("dve", angf, posi)
    S.ts("dve", angf, angf, pp1[:, 4:5], ALU.mult)
    S.ts("dve", t1.k("xg_tab0")[:, 0:1], angf[:, 0:1], 1.0, ALU.mult)
    for c0 in range(0, 4096, 2048):
        sl = slice(c0, c0 + 2048)
        S.ts("dve", kf[:, sl], angf[:, sl], 1.0 / (2 * PI), ALU.mult)
        S.copy("dve", ki[:, sl], kf[:, sl])
        S.copy("dve", kf[:, sl], ki[:, sl])
        C1 = 6.28125
        C2 = 2 * PI - C1
        S.stt(angf[:, sl], kf[:, sl], -C1, angf[:, sl], ALU.mult, ALU.add)
        S.stt(angf[:, sl], kf[:, sl], -C2, angf[:, sl], ALU.mult, ALU.add)
        msk = V(xn[:].rearrange("p a b -> p (a b)").bitcast(F32)[:, 0:2048], "xn_tab")
        PIS = 3.1415925

        def wrap(dst, src, shift):
            S.ts("dve", dst, src, shift, ALU.add)
            S.ts("dve", msk, dst, PI, ALU.is_gt)
            S.stt(dst, msk, -2 * PI, dst, ALU.mult, ALU.add)
            S.ts("dve", msk, dst, -PI, ALU.is_lt)
            S.stt(dst, msk, 2 * PI, dst, ALU.mult, ALU.add)
            S.ts("dve", dst, dst, PIS, ALU.min, -PIS, ALU.max)

        wrap(kf[:, sl], angf[:, sl], 0.0)
        S.act(V(sinT[:, sl], "sinT"), kf[:, sl], AF.Sin)
        wrap(kf[:, sl], angf[:, sl], PI / 2)
        S.act(V(cosT[:, sl], "cosT"), kf[:, sl], AF.Sin)
    S.barrier()

    loaded = set()

    def issue_loads(hg, tg):
        if tg >= ngroups:
            hg, tg = hg + 1, 0
        if hg >= npass or (hg, tg) in loaded:
            return
        loaded.add((hg, tg))
        c_ = slice(tg * 512, (tg + 1) * 512)
        if hg == 0:
            for k in range(16):
                S.dma("sp", V(xg[:, k, :], f"xg_{k}"), V(t["xT1"].ap[k * 128:(k + 1) * 128, c_], t["xT1"].keys))
        else:
            S.dma("sp", V(xno[:], "xno"), V(t["xnoscr"].ap[:, tg], f"xnoscr_{tg}"))
            for k in range(16):
                S.dma("sp", V(xn[:, k, :], f"xn_{k}"), V(t["xnscr"].ap[k * 128:(k + 1) * 128, c_], f"xnscr_{tg}"))

    for hg, tg in [(a, b_) for a in range(npass) for b_ in range(ngroups)]:
        cols = slice(tg * 512, (tg + 1) * 512)
        if tg == 0:
            for k in range(16):
                S.dma("pool", V(w1s[:, k, :], f"w1s_{k}"), V(t["w1"].ap[hg, k * 128:(k + 1) * 128, :], t["w1"].keys))
            S.dma("sp", wa2, V(t["wa2"].ap[hg], t["wa2"].keys))
            S.dma("sp", ba, V(t["ba"].ap[hg], t["ba"].keys))
            S.memset("dve", Sst, 0.0)
        def pick(dst, srcs):
            S.ts("dve", dst, srcs[0], sel4[:, 0:1], ALU.mult)
            for q in range(1, 4):
                S.stt(dst, srcs[q], sel4[:, q:q + 1], dst, ALU.mult, ALU.add)

        xn_all = [f"xn_{k}" for k in range(16)]
        gc0 = tg * 512
        cs2 = V(cs2all[:, tg, :], f"cs2_{tg}")
        sn2 = V(sn2all[:, tg, :], f"sn2_{tg}")
        issue_loads(hg, tg)
        if hg == 0:
            rmsnorm_fm(S, lambda k: V(xg[:, k, :], f"xg_{k}"), lambda k: g1[:, k:k + 1],
                       lambda k: V(xn[:, k, :], f"xn_{k}"), 16, 512, 2048.0, tmp, pb[0], ones_bf, eps_col)
            if tg + 1 < ngroups:
                issue_loads(0, tg + 1)
            pick(V(xno[:], "xno"), [V(xn[:, :, q * 128:(q + 1) * 128], xn_all) for q in range(4)])
            for tab, dst2 in ((cosT, cs2), (sinT, sn2)):
                nm = "cosT" if tab is cosT else "sinT"
                pick(dst2[:, 0:128], [V(tab[:, gc0 + q * 128:gc0 + (q + 1) * 128], nm) for q in range(4)])
                S.copy("pool", dst2[:, 128:256], dst2[:, 0:128])
            if npass > 1:
                for k in range(16):
                    S.dma("sp", V(t["xnscr"].ap[k * 128:(k + 1) * 128, cols], f"xnscr_{tg}"), V(xn[:, k, :], f"xn_{k}"))
                S.dma("sp", V(t["xnoscr"].ap[:, tg], f"xnoscr_{tg}"), V(xno[:], "xno"))

        def qk_post_multi(items, fillers=()):
            fillers = list(fillers)

            def fill():
                if fillers:
                    fillers.pop(0)()

            for ps, gcol, dst, cos_v, sin_v, w, T, nb in items:
                S.act(T["sq"][:, 0:w], ps, AF.Square)
            fill()
            for ps, gcol, dst, cos_v, sin_v, w, T, nb in items:
                S.mm(nb[:, 0:w], onesblk, T["sq"][:, 0:w])
            for ps, gcol, dst, cos_v, sin_v, w, T, nb in items:
                S.act(T["rt"][:, 0:w], nb[:, 0:w], AF.Sqrt, scale=1.0 / 64, bias=eps_col)
            for ps, gcol, dst, cos_v, sin_v, w, T, nb in items:
                S.recip(T["rb"][:, 0:w], T["rt"][:, 0:w])
            for ps, gcol, dst, cos_v, sin_v, w, T, nb in items:
                S.stt(T["qn"][:, 0:w], ps, gcol, T["rb"][:, 0:w], ALU.mult, ALU.mult)
            fill()
            for ps, gcol, dst, cos_v, sin_v, w, T, nb in items:
                S.mm(nb[:, 0:w], protT, T["qn"][:, 0:w])
            for ps, gcol, dst, cos_v, sin_v, w, T, nb in items:
                S.tt("dve", T["t2"][:, 0:w], nb[:, 0:w], sin_v, ALU.mult)
                S.tt("pool", T["t1"][:, 0:w], T["qn"][:, 0:w], cos_v, ALU.mult)
            for ps, gcol, dst, cos_v, sin_v, w, T, nb in items:
                S.tt("pool", dst, T["t1"][:, 0:w], T["t2"][:, 0:w], ALU.add)

        def tm_block(tb):
            blk = tg * 4 + tb
            tsl = slice(tb * 128, (tb + 1) * 128)
            ps = pb[3]
            for k in range(16):
                S.mm(ps[:, 0:256], V(xn[:, k, tsl], f"xn_{k}"), V(w1s[:, k, C_TM1:C_TM1 + 256], f"w1s_{k}"), start=(k == 0), stop=(k == 15))
            for h2 in range(2):
                S.copy("act", V(Vaug[:, blk, h2, 0:128], f"Vaug_{tg}"), ps[:, h2 * 128:(h2 + 1) * 128])
            ps = pb[7]
            for k in range(16):
                S.mm(ps[:, 0:384], V(xn[:, k, tsl], f"xn_{k}"), V(w1s[:, k, C_TM2:C_TM2 + 384], f"w1s_{k}"), start=(k == 0), stop=(k == 15))
            S.copy("dve", k_sb[tb], ps[:, 0:128])
            S.copy("dve", vg[tb], ps[:, 128:384])

        TA = {"sq": tmp["sq"][0], "rt": tmp["rt"], "rb": tmp["rb"], "qn": qn_bf, "t1": t1, "t2": t2}
        TB = {"sq": tmp["sq"][1], "rt": rtB, "rb": rbB, "qn": qnB, "t1": t1B, "t2": t2B}
        items = []
        for i, (c0, hd) in enumerate([(C_KA, 0), (C_KB, 1)]):
            ps = pb[1 + (i % 2)]
            for k in range(16):
                S.mm(ps, V(w1s[:, k, c0:c0 + 128], f"w1s_{k}"), V(xn[:, k, :], f"xn_{k}"), start=(k == 0), stop=(k == 15))
            items.append((ps, pp1[:, 2:3], V(KT[:, hd, cols], f"KT_{hd}_{tg}"), V(cosT[:, cols], "cosT"), V(sinT[:, cols], "sinT"), 512,
                          TA if i == 0 else TB, pb[0] if i == 0 else pb[4]))
        qk_post_multi(items, fillers=[lambda: tm_block(0), lambda: tm_block(1)])
        ps = pb[1]
        for hd, c0 in enumerate((C_QA, C_QB)):
            for k in range(16):
                S.mm(ps[:, hd * 128:(hd + 1) * 128], V(w1s[:, k, c0:c0 + 128], f"w1s_{k}"), V(xno[:, k, :], "xno"), start=(k == 0), stop=(k == 15))
        qk_post_multi([(ps[:, 0:256], pp1[:, 0:1], V(QTo[:].rearrange("p a b -> p (a b)"), "QTo"), cs2, sn2, 256, TA, pb[0])],
                      fillers=[lambda: tm_block(2), lambda: tm_block(3)])
        ps = pb[2]
        for k in range(16):
            S.mm(ps[:, 0:128], V(w1s[:, k, C_GQ:C_GQ + 128], f"w1s_{k}"), V(xno[:, k, :], "xno"), start=(k == 0), stop=(k == 15))
        S.act(gqo, ps[:, 0:128], AF.Copy, scale=128.0 ** -0.5)
        for k in range(16):
            S.mm(ps[0:16, :], V(w1s[:, k, C_LR:C_LR + 16], f"w1s_{k}"), V(xn[:, k, :], f"xn_{k}"), start=(k == 0), stop=(k == 15))
        S.copy("dve", glrT, ps[0:16, :])
        ps = pb[3]
        for k in range(16):
            S.mm(ps[:, 0:256], V(xno[:, k, :], "xno"), V(w1s[:, k, C_TM1 + 256:C_TM1 + 512], f"w1s_{k}"), start=(k == 0), stop=(k == 15))
        S.act(sgr, ps[:, 0:256], AF.Silu)
        if hg >= 1 or tg == ngroups - 1:
            issue_loads(hg, tg + 1)
        sbk = [pb[0], pb[2]]
        acc = [pb[1], pb[6]]
        rounds = [(hd, list(range(r0, r0 + 4))) for hd in range(2) for r0 in range(0, 4 * tg + 4, 4)]

        def emit_qk_exp(n):
            hd, kbs = rounds[n]
            pe2 = [pexp[(n % 2) * 2], pexp[(n % 2) * 2 + 1]]
            for i, kb in enumerate(kbs):
                for c in range(2):
                    csl = slice(64 * c, 64 * c + 64)
                    S.mm(sbk[c][:, i * 128:(i + 1) * 128], V(KT[csl, hd, kb * 128:(kb + 1) * 128], f"KT_{hd}_{kb // 4}"),
                         V(QTo[csl, hd, :], "QTo"))
            for c in range(2):
                S.act(pe2[c], sbk[c], AF.Exp, scale=0.125)
            if kbs[-1] == 4 * tg + 3:
                for c in range(2):
                    S.tt("pool", pe2[c], pe2[c], maskT, ALU.mult)

        def emit_pv(n):
            hd, kbs = rounds[n]
            pe2 = [pexp[(n % 2) * 2], pexp[(n % 2) * 2 + 1]]
            for i, kb in enumerate(kbs):
                for c in range(2):
                    S.mm(acc[c][:, 0:129], pe2[c][:, i * 128:(i + 1) * 128], V(Vaug[:, kb, hd, 0:129], f"Vaug_{kb // 4}"),
                         start=(kb == 0), stop=(kb == 4 * tg + 3))
            if kbs[-1] == 4 * tg + 3:
                dnb = dn[hd]
                S.recip(asm[:, 0:1], acc[0][:, 128:129])
                S.recip(asm[:, 1:2], acc[1][:, 128:129])
                S.tt("dve", asm[:, 2:3], asm[:, 1:2], neg_lam, ALU.mult)
                S.ts("dve", o1, acc[0][:, 0:128], asm[:, 0:1], ALU.mult)
                S.stt(dd, acc[1][:, 0:128], asm[:, 2:3], o1, ALU.mult, ALU.add)
                S.act(junk2, dd, AF.Square, accum=asm[:, 3:4])
                S.act(asm[:, 4:5], asm[:, 3:4], AF.Ln, scale=1.0 / 128, bias=eps_col)
                S.act(asm[:, 5:6], asm[:, 4:5], AF.Exp, scale=-0.5)
                S.stt(dnb, dd, asm[:, 5:6], subg_bc, ALU.mult, ALU.mult)

        todo = [0]

        def att_unit(slots_left=1):
            left = len(rounds) + 1 - todo[0]
            k = -(-left // max(slots_left, 1))
            for _ in range(min(k, left)):
                s_ = todo[0]
                todo[0] += 1
                if s_ >= 1:
                    emit_pv(s_ - 1)
                if s_ < len(rounds):
                    emit_qk_exp(s_)

        zb = [pb[7][:, tb * 128:(tb + 1) * 128] for tb in range(4)]
        rvb = [pb[1][:, tb * 128:(tb + 1) * 128] for tb in range(4)]
        for tb in range(4):
            tsl = slice(tb * 128, (tb + 1) * 128)
            S.mm(zb[tb], glrT[0:16, tsl], wa2, start=True, stop=False)
            S.mm(zb[tb], ones_f[0:1, 0:128], ba, start=False, stop=True)
        for tb in range(4):
            S.act(ez[tb], zb[tb], AF.Exp, scale=-1.0)
        for tb in range(4):
            S.act(lsp[tb], ez[tb], AF.Ln, bias=ones_f[:, 0:1])
        for tb in range(4):
            S.mm(rvb[tb], mstrict, lsp[tb])
        for tb in range(4):
            S.mm(pb[2][:, tb * 2:tb * 2 + 2], lsp[tb], chunkind)
        for tb in range(4):
            S.act(wexp[tb], rvb[tb], AF.Exp, scale=-1.0 / 16)
        S.act(dec, pb[2][:, 0:8], AF.Exp, scale=-1.0 / 16)
        for tb in range(4):
            S.tt("dve", kdec[tb], k_sb[tb], wexp[tb], ALU.mult)
        dslot = [V(pb[7].ap[:, 0:256], ["pb7a", "pb7"]), V(pb[3].ap[:, 0:256], ["pb3a", "pb3"]),
                 V(pb[7].ap[:, 256:512], ["pb7b", "pb7"]), V(pb[3].ap[:, 256:512], ["pb3b", "pb3"])]
        opsb = [pb[4][:, 0:256], pb[4][:, 256:512], pb[5][:, 0:256], pb[5][:, 256:512]]
        for c8 in range(8):
            tb, cc = c8 // 2, c8 % 2
            psl = slice(64 * cc, 64 * cc + 64)
            ds_ps = dslot[c8 % 4]
            S.mm(ds_ps, kdec[tb][psl, :], vg[tb][psl, :])
            S.stt(Sst2[(c8 + 1) % 2], Sst2[c8 % 2], dec[:, c8:c8 + 1], ds_ps, ALU.mult, ALU.add)
            S.copy("pool", Sbf[c8], Sst2[(c8 + 1) % 2])
            if c8 >= 1:
                ptb, pcc = (c8 - 1) // 2, (c8 - 1) % 2
                S.mm(opsb[ptb][slice(64 * pcc, 64 * pcc + 64), :], gqo[:, 64 * pcc:64 * pcc + 64], Sbf[c8 - 1])
            att_unit(13 - c8)
        S.mm(opsb[3][64:128, :], gqo[:, 64:128], Sbf[7])
        pick(osel, opsb)
        att_unit(5)
        S.act(junk, osel, AF.Square, accum=gsm[:, 0:1])
        att_unit(4)
        S.act(gsm[:, 1:2], gsm[:, 0:1], AF.Ln, scale=1.0 / 256, bias=eps_col)
        S.act(gsm[:, 2:3], gsm[:, 1:2], AF.Exp, scale=-0.5)
        att_unit(3)
        S.stt(osel, osel, gsm[:, 2:3], gog_bc, ALU.mult, ALU.mult)
        att_unit(2)
        S.tt("pool", osel, osel, sgr, ALU.mult)
        att_unit(1)
        for r in range(2):
            S.tr(pb[7][:, r * 128:(r + 1) * 128], osel[:, r * 128:(r + 1) * 128], ident)
            S.copy("act", mstage[2 + r][:, 0:128], pb[7][:, r * 128:(r + 1) * 128])
        while todo[0] <= len(rounds):
            att_unit()
        for hd in range(2):
            S.tr(sbk[0][:, hd * 128:(hd + 1) * 128], dn[hd], ident)
            S.copy("act", mstage[hd][:, 0:128], sbk[0][:, hd * 128:(hd + 1) * 128])
        ocols = slice(tg * 128, (tg + 1) * 128)
        for r in range(4):
            r0 = row_of(hg, r)
            S.dma("sp", V(t["mixT1"].ap[r0:r0 + 128, ocols], t["mixT1"].keys), mstage[r][:, 0:128])


def build_nc_phase1_only(ngroups=8):
    import contextlib
    nc = bass.Bass("TRN2", target_bir_lowering=False)
    t = {}
    for name, shape, dt in P1_INPUTS:
        t[name] = V(nc.dram_tensor(name, shape, dt, kind="ExternalInput").ap(), "dram_" + name)
    t["mixT1"] = V(nc.dram_tensor("mixT1", [512, 1024], BF16, kind="ExternalOutput").ap(), "dram_mixT1")
    S = Sched(nc)
    with contextlib.ExitStack() as es:
        build_phase1(nc, S, t, es, ngroups=ngroups)
        S.emit()
    return nc


_NC_CACHE = {}


def build_nc_fused(ngroups=8):
    import contextlib
    nc = bass.Bass("TRN2", target_bir_lowering=False)
    t1, t2 = {}, {}
    for name, shape, dt in P1_INPUTS:
        shape = list(shape)
        if name in ("w1", "wa2", "ba"):
            shape[0] = 4
        t1[name] = V(nc.dram_tensor(name, shape, dt, kind="ExternalInput").ap(), "dram_" + name)
    mixscr = V(nc.dram_tensor("mixscr", [2048, 1024], BF16, kind="Internal").ap(), "dram_mixscr")
    t1["mixT1"] = mixscr
    t1["xnscr"] = V(nc.dram_tensor("xnscr", [2048, 4096], BF16, kind="Internal").ap(), "dram_xnscr")
    t1["xnoscr"] = V(nc.dram_tensor("xnoscr", [128, 8, 16, 128], BF16, kind="Internal").ap(), "dram_xnoscr")
    for name, shape, dt in P2_INPUTS:
        if name == "mixT":
            continue
        t2[name] = V(nc.dram_tensor(name, shape, dt, kind="ExternalInput").ap(), "dram_" + name)
    t2["mixT"] = mixscr
    t2["outT"] = V(nc.dram_tensor("outT", [2048, 1024], F32, kind="ExternalOutput").ap(), "dram_outT")
    S = Sched(nc)

    def row_of(hg, r):
        return hg * 256 + r * 128 if r < 2 else 1024 + hg * 256 + (r - 2) * 128

    with contextlib.ExitStack() as es1:
        build_phase1(nc, S, t1, es1, ngroups=ngroups, npass=4, row_of=row_of)
    S.barrier()
    with contextlib.ExitStack() as es2:
        build_phase2(nc, S, t2, es2, mix_select=False)
    S.emit()
    return nc


def host_fused_inputs(inp, c):
    m = host_phase1_inputs(inp, c, groups=[0, 1, 2, 3])
    m.update(host_phase2_inputs(inp, c))
    return m


def kernel(**inp):
    inp = {k: np.asarray(v) for k, v in inp.items()}
    if "fused" not in _NC_CACHE:
        _NC_CACHE["fused"] = build_nc_fused()
    cores = list(range(8))
    res = run_bass_kernel_spmd(_NC_CACHE["fused"], [host_fused_inputs(inp, c) for c in cores], core_ids=cores)
    out = np.empty((2, 4096, 2048), np.float32)
    for c in cores:
        b, j = c // 4, c % 4
        out[b, own_tokens(c), :] = np.asarray(res.results[c]["outT"]).T
    return out
```
